# Optimizing a Trainium2 kernel written in Bass

```python
import jax, jax.numpy as jnp
from jax import lax
import numpy as np


D_MODEL = 2048
BATCH = 2
SEQ = 8192
DEPTH = 1

PLE_DIM = 256
D_SGU = 2048
SGU_GROUPS = 16
CHUNK = 128
D_CONV = 2048
CONV_WIDTH = 31
N_EXPERTS = 32
TOP_K = 4
D_EXPERT = 2048
SWIGLU_ALPHA = 1.702
SWIGLU_LIMIT = 7.0
MOE_BLOCK = 128
LN_EPS = 1e-5
DN_ALPHA = (2.0 * DEPTH) ** 0.25
DN_BETA = (8.0 * DEPTH) ** -0.25
D_IN = 2 * D_SGU + 2 * D_CONV + 2 * D_MODEL
SPLIT_POINTS = (D_SGU, 2 * D_SGU, 2 * D_SGU + D_CONV, 2 * D_SGU + 2 * D_CONV, 2 * D_SGU + 2 * D_CONV + D_MODEL)

kernel_name = 'hybrid_sgu_conformer_moe_deepnorm_block'


def layer_norm(x, g, b):
    xf = x.astype(jnp.float32)
    mu = jnp.mean(xf, axis=-1, keepdims=True)
    var = jnp.mean(jnp.square(xf - mu), axis=-1, keepdims=True)
    return ((xf - mu) * lax.rsqrt(var + LN_EPS)).astype(x.dtype) * g + b


def spatial_gating(u, v, ln_g, ln_b, w_s, b_s):
    bsz, seq, _ = v.shape
    v = layer_norm(v, ln_g, ln_b)
    vc = v.reshape(bsz, seq // CHUNK, CHUNK, SGU_GROUPS, D_SGU // SGU_GROUPS)
    causal = jnp.tril(jnp.ones((CHUNK, CHUNK), dtype=bool))
    w = jnp.where(causal[None], w_s, 0).astype(v.dtype)
    s = jnp.einsum('gts,bcsgd->bctgd', w, vc) + b_s.T[None, None, :, :, None]
    return u * s.reshape(bsz, seq, D_SGU)


def causal_depthwise_conv(h, w, b):
    chans = h.shape[-1]
    y = lax.conv_general_dilated(
        h, w[:, None, :].astype(h.dtype), window_strides=(1,),
        padding=[(CONV_WIDTH - 1, 0)], dimension_numbers=('NWC', 'WIO', 'NWC'),
        feature_group_count=chans)
    return y + b


def clamped_swiglu_expert(xb, w1, b1, w2, b2):
    hid = xb @ w1 + b1
    gate, up = hid[:, :D_EXPERT], hid[:, D_EXPERT:]
    gate = jnp.minimum(gate, SWIGLU_LIMIT)
    up = jnp.clip(up, -SWIGLU_LIMIT, SWIGLU_LIMIT)
    glu = gate * jax.nn.sigmoid(SWIGLU_ALPHA * gate)
    return (glu * (up + 1)) @ w2 + b2


def moe(x, w_router, b_router, w1, b1, w2, b2):
    bsz, seq, d = x.shape
    n_tok = bsz * seq
    xf = x.reshape(n_tok, d)
    logits = (xf @ w_router + b_router).astype(jnp.float32)
    top_val, top_idx = lax.top_k(logits, TOP_K)
    top_w = jax.nn.softmax(top_val, axis=-1)
    n_assign = n_tok * TOP_K
    e_flat = top_idx.reshape(n_assign).astype(jnp.int32)
    t_flat = jnp.arange(n_assign, dtype=jnp.int32) // TOP_K
    w_flat = top_w.reshape(n_assign)
    e_sorted, t_sorted, w_sorted = lax.sort((e_flat, t_flat, w_flat), dimension=0, is_stable=True, num_keys=1)
    counts = jnp.bincount(e_flat, length=N_EXPERTS).astype(jnp.int32)
    padded = (counts + MOE_BLOCK - 1) // MOE_BLOCK * MOE_BLOCK
    starts = jnp.cumsum(counts) - counts
    pad_ends = jnp.cumsum(padded)
    pad_starts = pad_ends - padded
    dest = pad_starts[e_sorted] + jnp.arange(n_assign, dtype=jnp.int32) - starts[e_sorted]
    n_blocks = -(-n_assign // MOE_BLOCK) + N_EXPERTS
    n_slots = n_blocks * MOE_BLOCK
    slot_tok = jnp.full((n_slots,), n_tok, jnp.int32).at[dest].set(t_sorted)
    slot_w = jnp.zeros((n_slots,), jnp.float32).at[dest].set(w_sorted)
    block_exp = jnp.minimum(
        jnp.searchsorted(pad_ends, jnp.arange(n_blocks, dtype=jnp.int32) * MOE_BLOCK, side='right'),
        N_EXPERTS - 1)
    x_pad = jnp.concatenate([xf, jnp.zeros((1, d), xf.dtype)], axis=0)
    xs = x_pad[slot_tok].reshape(n_blocks, MOE_BLOCK, d)

    def run_block(args):
        xb, e = args
        return clamped_swiglu_expert(xb, w1[e], b1[e], w2[e], b2[e])

    ys = lax.map(run_block, (xs, block_exp)).reshape(n_slots, d)
    out = jnp.zeros((n_tok + 1, d), x.dtype).at[slot_tok].add(ys * slot_w[:, None].astype(ys.dtype))
    return out[:n_tok].reshape(bsz, seq, d)


def _normal(k, shape, scale):
    return jax.random.normal(k, shape, jnp.float32) * scale


def setup_inputs(seed: int = 0) -> dict:
    key = jax.random.key(seed)
    ks = jax.random.split(key, 32)
    L = DEPTH
    return {
        'x': _normal(ks[0], (BATCH, SEQ, D_MODEL), 1.0),
        'p': _normal(ks[1], (L, BATCH, SEQ, PLE_DIM), 1.0),
        'ln0_g': 1.0 + _normal(ks[2], (D_MODEL,), 0.02),
        'ln0_b': _normal(ks[3], (D_MODEL,), 0.02),
        'w_in': _normal(ks[4], (L, D_MODEL, D_IN), D_MODEL ** -0.5),
        'b_in': _normal(ks[5], (L, D_IN), 0.02),
        'sgu_ln_g': 1.0 + _normal(ks[6], (L, D_SGU), 0.02),
        'sgu_ln_b': _normal(ks[7], (L, D_SGU), 0.02),
        'w_s': _normal(ks[8], (L, SGU_GROUPS, CHUNK, CHUNK), CHUNK ** -0.5),
        'b_s': 1.0 + _normal(ks[9], (L, SGU_GROUPS, CHUNK), 0.02),
        'w_sgu_out': _normal(ks[10], (L, D_SGU, D_MODEL), D_SGU ** -0.5),
        'b_sgu_out': _normal(ks[11], (L, D_MODEL), 0.02),
        'conv_w': _normal(ks[12], (L, CONV_WIDTH, D_CONV), CONV_WIDTH ** -0.5),
        'conv_b': _normal(ks[13], (L, D_CONV), 0.02),
        'conv_ln_g': 1.0 + _normal(ks[14], (L, D_CONV), 0.02),
        'conv_ln_b': _normal(ks[15], (L, D_CONV), 0.02),
        'w_conv_out': _normal(ks[16], (L, D_CONV, D_MODEL), D_CONV ** -0.5),
        'b_conv_out': _normal(ks[17], (L, D_MODEL), 0.02),
        'w_o': _normal(ks[18], (L, D_MODEL, D_MODEL), DN_BETA * D_MODEL ** -0.5),
        'b_o': _normal(ks[19], (L, D_MODEL), 0.02),
        'ln1_g': 1.0 + _normal(ks[20], (L, D_MODEL), 0.02),
        'ln1_b': _normal(ks[21], (L, D_MODEL), 0.02),
        'w_router': _normal(ks[22], (L, D_MODEL, N_EXPERTS), D_MODEL ** -0.5),
        'b_router': _normal(ks[23], (L, N_EXPERTS), 0.01),
        'w_e1': _normal(ks[24], (L, N_EXPERTS, D_MODEL, 2 * D_EXPERT), D_MODEL ** -0.5),
        'b_e1': _normal(ks[25], (L, N_EXPERTS, 2 * D_EXPERT), 0.02),
        'w_e2': _normal(ks[26], (L, N_EXPERTS, D_EXPERT, D_MODEL), DN_BETA * D_EXPERT ** -0.5),
        'b_e2': _normal(ks[27], (L, N_EXPERTS, D_MODEL), 0.02),
        'w_pg': _normal(ks[28], (L, D_MODEL, D_MODEL), D_MODEL ** -0.5),
        'b_pg': _normal(ks[29], (L, D_MODEL), 0.02),
        'w_ple': _normal(ks[30], (L, PLE_DIM, D_MODEL), DN_BETA * PLE_DIM ** -0.5),
        'ln2_g': 1.0 + _normal(ks[31], (L, D_MODEL), 0.02),
        'ln2_b': _normal(jax.random.fold_in(ks[31], 1), (L, D_MODEL), 0.02),
    }


def reference(x, p, ln0_g, ln0_b, w_in, b_in, sgu_ln_g, sgu_ln_b, w_s, b_s, w_sgu_out, b_sgu_out,
              conv_w, conv_b, conv_ln_g, conv_ln_b, w_conv_out, b_conv_out, w_o, b_o, ln1_g, ln1_b,
              w_router, b_router, w_e1, b_e1, w_e2, b_e2, w_pg, b_pg, w_ple, ln2_g, ln2_b):
    h = layer_norm(x, ln0_g, ln0_b)
    for i in range(DEPTH):
        z = h @ w_in[i] + b_in[i]
        u, v, glu_a, glu_b, gate_a, gate_b = jnp.split(z, SPLIT_POINTS, axis=-1)
        y_a = spatial_gating(jax.nn.gelu(u, approximate=False), jax.nn.gelu(v, approximate=False),
                             sgu_ln_g[i], sgu_ln_b[i], w_s[i], b_s[i])
        y_a = y_a @ w_sgu_out[i] + b_sgu_out[i]
        c = glu_a * jax.nn.sigmoid(glu_b)
        c = causal_depthwise_conv(c, conv_w[i], conv_b[i])
        c = jax.nn.silu(layer_norm(c, conv_ln_g[i], conv_ln_b[i]))
        y_b = c @ w_conv_out[i] + b_conv_out[i]
        mix = (jax.nn.sigmoid(gate_a) * y_a + jax.nn.sigmoid(gate_b) * y_b) @ w_o[i] + b_o[i]
        h = layer_norm(DN_ALPHA * h + mix, ln1_g[i], ln1_b[i])
        ffn = moe(h, w_router[i], b_router[i], w_e1[i], b_e1[i], w_e2[i], b_e2[i])
        ple = jax.nn.sigmoid(h @ w_pg[i] + b_pg[i]) * (p[i] @ w_ple[i])
        h = layer_norm(DN_ALPHA * h + ffn + ple, ln2_g[i], ln2_b[i])
    return h
```

```python
import numpy as np
from contextlib import ExitStack
import concourse.bass as bass
import concourse.mybir as mybir
from concourse.bass_utils import run_bass_kernel_spmd

F32 = mybir.dt.float32
BF16 = mybir.dt.bfloat16
ALU = mybir.AluOpType
AF = mybir.ActivationFunctionType
AX = mybir.AxisListType

D = 2048
KC = 16
DIN = 12288
PLE = 256
CW = 31
HALO = 32
TG = 256
LN_EPS = 1e-5
DN_ALPHA = 2.0 ** 0.25
SW_ALPHA = 1.702
SW_LIM = 7.0
NWB = 3
P1_ORDER = list(range(4, 16)) + list(range(0, 4)) + list(range(16, 36))
CAP = 256


class _Op:
    __slots__ = ("eng", "fn", "deps", "needs_inc", "dma", "sem", "val", "inc")

    def __init__(self, eng, fn, dma):
        self.eng, self.fn, self.dma = eng, fn, dma
        self.deps = []
        self.needs_inc = False
        self.sem = None
        self.val = 0
        self.inc = 1


class Sched:
    ENGS = ("pe", "act", "dve", "pool", "sp")

    def __init__(self, nc):
        self.nc = nc
        self.ops = {e: [] for e in self.ENGS}
        self.res = {}
        self.last_dma = {}
        self.nps = 0

    def op(self, eng, fn, reads=(), writes=(), dma=None):
        o = _Op(eng, fn, dma)
        seen = set()

        def add(d):
            if d is None or id(d) in seen:
                return
            if d.eng == "pe" and eng == "pe" and d.dma is None and dma is None:
                return
            seen.add(id(d))
            o.deps.append(d)

        for k in reads:
            st = self.res.setdefault(k, [None, []])
            add(st[0])
        for k in writes:
            st = self.res.setdefault(k, [None, []])
            add(st[0])
            for r in st[1]:
                add(r)
        if dma is not None:
            add(self.last_dma.get(dma))
            self.last_dma[dma] = o
        for k in reads:
            self.res[k][1].append(o)
        for k in writes:
            self.res[k][0] = o
            self.res[k][1] = []
        self.ops[eng].append(o)
        return o

    def barrier(self):
        keys = list(self.res.keys())
        for e in self.ENGS:
            self.op(e, None, reads=keys)
        for e in self.ENGS:
            self.op(e, None, writes=keys)

    def finalize(self, stack):
        nc = self.nc
        def expand(o, out, seen):
            for d in o.deps:
                if id(d) in seen:
                    continue
                seen.add(id(d))
                if d.fn is None:
                    expand(d, out, seen)
                else:
                    out.append(d)
        for e in self.ENGS:
            for o in self.ops[e]:
                out = []
                expand(o, out, set())
                o.deps = out
        for e in self.ENGS:
            for o in self.ops[e]:
                for d in o.deps:
                    d.needs_inc = True
        esem = {}
        for e in self.ENGS:
            esem[e] = stack.enter_context(nc.semaphore("sem_" + e))
        dsem = {}
        dcnt = {}
        for e in self.ENGS:
            cnt = 0
            for o in self.ops[e]:
                if o.fn is None:
                    continue
                if o.dma is not None:
                    if o.dma not in dsem:
                        dsem[o.dma] = stack.enter_context(nc.semaphore("dsem%d" % len(dsem)))
                        dcnt[o.dma] = 0
                    dcnt[o.dma] += 16
                    o.sem, o.val, o.inc = dsem[o.dma], dcnt[o.dma], 16
                    o.needs_inc = True
                elif o.needs_inc:
                    cnt += 1
                    o.sem, o.val, o.inc = esem[e], cnt, 1
        self.nsem = len(dsem) + len(esem)

    def emit(self, block):
        meth = {"pe": block.tensor, "act": block.scalar, "dve": block.vector, "pool": block.gpsimd, "sp": block.sync}
        for ename in self.ENGS:
            ops = self.ops[ename]

            def body(e, ops=ops):
                waited = {}
                for o in ops:
                    need = {}
                    for d in o.deps:
                        k = id(d.sem)
                        if k not in need or need[k][1] < d.val:
                            need[k] = (d.sem, d.val)
                    for k, (sem, val) in need.items():
                        if waited.get(k, 0) >= val:
                            continue
                        e.wait_ge(sem, val)
                        waited[k] = val
                    if o.fn is None:
                        continue
                    ins = o.fn(e)
                    if o.needs_inc:
                        ins.then_inc(o.sem, o.inc)

            meth[ename](body)


def build_program(NT=2048, E=32, HALF=1024):
    NG = NT // TG
    TT = TG // 128
    nc = bass.Bass("TRN2", target_bir_lowering=False)
    S = Sched(nc)

    def din(name, shape):
        return nc.dram_tensor(name, list(shape), F32, kind="ExternalInput").ap()

    x = din("x", [NT, D])
    xhalo = din("xhalo", [HALO, D])
    hmask_d = din("hmask", [128, 1])
    p_d = din("p", [NT, PLE])
    w_in = din("w_in", [D, DIN])
    w_sgu = din("w_sgu_out", [D, D])
    w_cv = din("w_conv_out", [D, D])
    w_o = din("w_o", [D, D])
    w_pg = din("w_pg", [D, D])
    w_ple = din("w_ple", [PLE, D])
    w_rt = din("w_router", [D, E])
    w_e1 = din("w_e1", [E, D, 2 * D])
    w_e2 = din("w_e2", [E, D, D])
    lnv = din("lnv", [8, D])
    bfm_d = din("bfm", [128, 176])
    convw_d = din("convw", [128, KC * CW])
    bv_d = din("bv", [1, D])
    bo_d = din("bo", [1, D])
    bpg_d = din("bpg", [1, D])
    brt_d = din("brt", [1, E])
    wsT_d = din("wsT", [128, 16 * 128])
    bs_d = din("bs", [1, 16 * 128])
    b1_d = din("b1", [128, E * 32])
    b2_d = din("b2", [E, D])
    ident_d = din("ident", [128, 128])
    tri_d = din("tri", [128, 128])
    iota_d = din("iota", [128, 512])
    out = nc.dram_tensor("out", [NT, D], F32, kind="ExternalOutput").ap()
    h1scr = nc.dram_tensor("h1scr", [NT, D], F32, kind="Internal").ap()

    def wview(w2d, c0):
        return w2d.rearrange("(kc p) n -> p kc n", p=128)[:, :, c0:c0 + 512]

    with ExitStack() as top:
        def sb(name, shape, dt, stack=top):
            return stack.enter_context(nc.sbuf_tensor("s_" + name, list(shape), dt))

        pst = [top.enter_context(nc.psum_tensor("ps%d" % i, [128, 512], F32)) for i in range(8)]

        def next_ps():
            i = S.nps % 8
            S.nps += 1
            return ("ps", i), pst[i]

        ident = sb("ident", [128, 128], F32)
        ones0 = sb("ones0", [128, 128], BF16)
        wb = [sb("wb%d" % i, [128, KC, 512], BF16) for i in range(NWB)]
        st6 = sb("st6", [128, 4, 6], F32)
        mv = sb("mv", [128, 2], F32)
        rstd = sb("rstd", [128, 1], F32)

        S.op("sp", lambda e: e.dma_start(out=ident[:], in_=ident_d[:, :]), writes=["ident"], dma="c_ident")
        S.op("dve", lambda e: e.memset(ones0[:], 0.0), writes=["ones0"])
        S.op("dve", lambda e: e.memset(ones0[0:1, :], 1.0), writes=["ones0"])

        LNT = {}

        def load_ln_half(i, hf):
            lng, lnb = LNT["g"], LNT["b"]
            S.op("sp", lambda e: e.dma_start(out=lng[:], in_=lnv[2 * i:2 * i + 1, hf * 1024:(hf + 1) * 1024].partition_broadcast(128)),
                 writes=["lng"], dma="lng")
            S.op("sp", lambda e: e.dma_start(out=lnb[:], in_=lnv[2 * i + 1:2 * i + 2, hf * 1024:(hf + 1) * 1024].partition_broadcast(128)),
                 writes=["lnb"], dma="lnb")

        def ln_group(i, tiles):
            lng, lnb = LNT["g"], LNT["b"]
            for (xt, keys, out_ap, out_keys, np_) in tiles:
                ln_tm(xt, keys, out_ap, out_keys, np_=np_, affine=False)
            for hf in range(2):
                load_ln_half(i, hf)
                for (xt, keys, out_ap, out_keys, np_) in tiles:
                    kh = list(keys[2 * hf:2 * hf + 2])
                    xs = xt[:, hf * 1024:(hf + 1) * 1024]
                    os_ = out_ap[:, hf * 1024:(hf + 1) * 1024]
                    S.op("dve", lambda e, xs=xs, np_=np_: e.tensor_tensor(xs, xs, lng[0:np_, :], ALU.mult), reads=kh + ["lng"], writes=kh)
                    okh = kh if list(out_keys) == list(keys) else list(out_keys)
                    S.op("dve", lambda e, xs=xs, os_=os_, np_=np_: e.tensor_tensor(os_, xs, lnb[0:np_, :], ALU.add),
                         reads=kh + ["lnb"], writes=okh)

        def ln_tm(xt, keys, out_ap, out_keys, np_=128, affine=True):
            def f_stats(e):
                ins = None
                for c in range(4):
                    ins = e.bn_stats(st6[0:np_, c, :], xt[:, c * 512:(c + 1) * 512])
                return ins
            S.op("dve", f_stats, reads=keys, writes=["st6"])
            S.op("dve", lambda e: e.bn_aggr(mv[0:np_, :], st6[0:np_, :, :].rearrange("p a b -> p (a b)")),
                 reads=["st6"], writes=["mv"])
            S.op("dve", lambda e: e.tensor_scalar(rstd[0:np_, :], mv[0:np_, 1:2], LN_EPS, None, ALU.add),
                 reads=["mv"], writes=["rstd"])
            S.op("act", lambda e: e.activation(rstd[0:np_, :], rstd[0:np_, :], AF.Sqrt), reads=["rstd"], writes=["rstd"])
            S.op("dve", lambda e: e.reciprocal(rstd[0:np_, :], rstd[0:np_, :]), reads=["rstd"], writes=["rstd"])
            S.op("dve", lambda e: e.tensor_scalar(xt, xt, mv[0:np_, 0:1], rstd[0:np_, 0:1], ALU.subtract, ALU.mult),
                 reads=list(keys) + ["mv", "rstd"], writes=keys)
            if not affine:
                return
            lng, lnb = LNT["g"], LNT["b"]
            S.op("dve", lambda e: e.tensor_tensor(xt, xt, lng[0:np_, :], ALU.mult), reads=list(keys) + ["lng"], writes=keys)
            S.op("dve", lambda e: e.tensor_tensor(out_ap, xt, lnb[0:np_, :], ALU.add),
                 reads=list(keys) + ["lnb"], writes=out_keys)

        def transpose_tile(src, src_keys, dst, dst_key_fn, tcol, nkc=KC, np_=128):
            per = 512 // np_ if np_ < 128 else 4
            q = 0
            kc = 0
            while kc < nkc:
                n = min(per, nkc - kc)
                pk, ps = next_ps()

                def f_t(e, kc=kc, n=n, ps=ps):
                    ins = None
                    for j in range(n):
                        ins = e.transpose(ps[:, j * np_:(j + 1) * np_], src[:, (kc + j) * 128:(kc + j + 1) * 128],
                                          ident[0:np_, 0:np_])
                    return ins
                S.op("pe", f_t, reads=list(src_keys) + ["ident"], writes=[pk])
                S.op("act", lambda e, kc=kc, n=n, ps=ps: e.activation(
                    dst[:, kc:kc + n, tcol:tcol + np_],
                    ps[:, 0:n * np_].rearrange("p (a b) -> p a b", a=n), AF.Identity),
                    reads=[pk], writes=[dst_key_fn(q)])
                kc += n
                q += 1

        with ExitStack() as p1:
            xh0 = sb("xh0", [128, TT, D], F32, p1)
            h0T = sb("h0T", [128, KC, TG], BF16, p1)
            uT = sb("uT", [128, KC, TG], BF16, p1)
            vtco = sb("vtco", [128, TT * D], F32, p1)
            vt = vtco[:, :].rearrange("p (t d) -> p t d", t=TT)
            co = vtco[:, :].rearrange("p (c n) -> p c n", c=KC)
            LNT["g"] = sb("lng", [128, 1024], F32, p1)
            LNT["b"] = sb("lnb", [128, 1024], F32, p1)
            vn = sb("vn", [128, TT, D], BF16, p1)
            cT = sb("cT", [128, KC, HALO + TG], F32, p1)
            cn = sb("cn", [128, KC, TG], BF16, p1)
            gaT = sb("gaT", [128, KC, TG], BF16, p1)
            gbT = sb("gbT", [128, KC, TG], BF16, p1)
            mT = sb("mT", [128, KC, TG], BF16, p1)
            xh = vtco[0:HALO, 0:D]
            xh_keys = [("vc", i) for i in range(4)]
            h0Th = sb("h0Th", [128, KC, HALO], BF16, p1)
            bfm = sb("bfm", [128, 176], F32, p1)
            convw = sb("convw", [128, KC, CW], F32, p1)
            hmask = sb("hmask", [128, 1], F32, p1)
            bvrow = sb("bvrow", [128, D], BF16, p1)
            borow = sb("borow", [128, D], BF16, p1)
            bsrow = sb("bsrow", [128, 16, 128], BF16, p1)
            wsT = sb("wsT", [128, 16, 128], BF16, p1)
            tri = sb("tri", [128, 128], F32, p1)
            ones32 = sb("ones32", [128, 128], F32, p1)
            sgt = [sb("sgt%d" % i, [128, TG], F32, p1) for i in range(2)]
            sqt = [sb("sqt%d" % i, [128, TG], F32, p1) for i in range(2)]
            sgh = sb("sgh", [128, HALO], F32, p1)
            cmean = sb("cmean", [128, TG], F32, p1)
            cmsq = sb("cmsq", [128, TG], F32, p1)
            crstd = sb("crstd", [128, TG], F32, p1)
            cnb = sb("cnb", [128, TG], F32, p1)

            S.op("sp", lambda e: e.dma_start(out=bfm[:], in_=bfm_d[:, :]), writes=["bfm"], dma="c_bfm")
            S.op("sp", lambda e: e.dma_start(out=convw[:], in_=convw_d.rearrange("p (a b) -> p a b", a=KC)),
                 writes=["convw"], dma="c_convw")
            S.op("sp", lambda e: e.dma_start(out=hmask[:], in_=hmask_d[:, :]), writes=["hmask"], dma="c_hmask")
            S.op("pool", lambda e: e.dma_start(out=wsT[:], in_=wsT_d.rearrange("p (a b) -> p a b", a=16)),
                 writes=["wsT"], dma="c_wsT")
            S.op("sp", lambda e: e.dma_start(out=tri[:], in_=tri_d[:, :]), writes=["tri"], dma="c_tri")
            S.op("dve", lambda e: e.memset(ones32[:], 1.0), writes=["ones32"])
            for (t_, d_, nm) in ((bvrow, bv_d, "bvrow"), (borow, bo_d, "borow")):
                S.op("dve", lambda e, t_=t_: e.memset(t_[:], 0.0), writes=[nm])
                S.op("pool", lambda e, t_=t_, d_=d_: e.dma_start(out=t_[0:1, :], in_=d_[:, :]), writes=[nm], dma="c_" + nm)
            S.op("dve", lambda e: e.memset(bsrow[:], 0.0), writes=["bsrow"])
            S.op("pool", lambda e: e.dma_start(out=bsrow[0:1, :, :], in_=bs_d.rearrange("o (a b) -> o a b", a=16)),
                 writes=["bsrow"], dma="c_bsrow")
            for g16 in range(16):
                S.op("dve", lambda e, g16=g16: e.tensor_tensor(wsT[:, g16, :], wsT[:, g16, :], tri[:], ALU.mult),
                     reads=["wsT", "tri"], writes=["wsT"])

            def xkeys(tt):
                return [("xh0", tt, dg) for dg in range(4)]

            def h0T_keys():
                return [("h0T", tt, q) for tt in range(TT) for q in range(4)]

            def prologue(g):
                tok0 = g * TG
                for tt in range(TT):
                    S.op("sp", lambda e, tt=tt: e.dma_start(out=xh0[:, tt, :], in_=x[tok0 + tt * 128: tok0 + (tt + 1) * 128, :]),
                         writes=xkeys(tt), dma=("x", tt))
                tiles0 = [(xh0[:, tt, :], xkeys(tt), xh0[:, tt, :], xkeys(tt), 128) for tt in range(TT)]
                if g == 0:
                    S.op("sp", lambda e: e.dma_start(out=xh, in_=xhalo[:, :]), writes=xh_keys, dma="c_xh")
                    ln_group(0, [(xh, xh_keys, xh, xh_keys, HALO)] + tiles0)
                    transpose_tile(xh, xh_keys, h0Th, lambda q: "h0Th", 0, np_=HALO)
                else:
                    S.op("dve", lambda e: e.tensor_copy(cT[:, :, 0:HALO], cT[:, :, TG:TG + HALO]),
                         reads=[("cT", c) for c in range(KC)], writes=[("cT", c) for c in range(KC)])
                    ln_group(0, tiles0)
                for tt in range(TT):
                    transpose_tile(xh0[:, tt, :], xkeys(tt), h0T, lambda q, tt=tt: ("h0T", tt, q), tt * 128)

            def fform(wbt, wbk, rhs_fn, rhs_keys, N, evac):
                for j in range(4):
                    pk, ps = next_ps()

                    def f_mm(e, j=j, ps=ps):
                        ins = None
                        for kc in range(KC):
                            ins = e.matmul(ps[:, 0:N], lhsT=wbt[:, kc, j * 128:(j + 1) * 128], rhs=rhs_fn(kc),
                                           start=(kc == 0), stop=(kc == KC - 1))
                        return ins
                    S.op("pe", f_mm, reads=[wbk] + list(rhs_keys), writes=[pk])
                    evac(j, ps, pk)

            def tform(wbt, wbk, lhs_fn, lhs_keys_fn, ntt, bias_rhs, bias_key, evac):
                for tt in range(ntt):
                    pk, ps = next_ps()

                    def f_mm(e, tt=tt, ps=ps):
                        ins = None
                        for kc in range(KC):
                            ins = e.matmul(ps[:, :], lhsT=lhs_fn(kc, tt), rhs=wbt[:, kc, :],
                                           start=(kc == 0), stop=(kc == KC - 1 and bias_rhs is None))
                        if bias_rhs is not None:
                            ins = e.matmul(ps[:, :], lhsT=ones0[:, :], rhs=bias_rhs, start=False, stop=True)
                        return ins
                    rk = [wbk, "ones0"] + list(lhs_keys_fn(tt)) + ([bias_key] if bias_key else [])
                    S.op("pe", f_mm, reads=rk, writes=[pk])
                    evac(tt, ps, pk)

            def conv_chunks(cs):
                for c in cs:
                    S.op("dve", lambda e, c=c: e.tensor_scalar(co[:, c, :], cT[:, c, 2:2 + TG], convw[:, c, 0:1], bfm[:, 128 + c:129 + c],
                                                               ALU.mult, ALU.add),
                         reads=[("cT", c), "convw", "bfm"], writes=[("co", c), ("vc", c // 2)])
                for k in range(1, CW):
                    for c in cs:
                        S.op("dve", lambda e, k=k, c=c: e.scalar_tensor_tensor(co[:, c, :], cT[:, c, 2 + k:2 + k + TG], convw[:, c, k:k + 1],
                                                                                co[:, c, :], ALU.mult, ALU.add),
                             reads=[("cT", c), "convw", ("co", c)], writes=[("co", c)])

            def conv_ln():
                pk1, ps1 = next_ps()
                pk2, ps2 = next_ps()
                for c in range(KC):
                    sq = sqt[c % 2]
                    sk = ("sqt", c % 2)
                    S.op("act", lambda e, c=c, sq=sq: e.activation(sq[:], co[:, c, :], AF.Square), reads=[("co", c)], writes=[sk])
                    S.op("pe", lambda e, c=c: e.matmul(ps1[:, 0:TG], lhsT=ones32[:, :], rhs=co[:, c, :], start=(c == 0), stop=(c == KC - 1)),
                         reads=[("co", c), "ones32"], writes=[pk1])
                    S.op("pe", lambda e, c=c, sq=sq: e.matmul(ps2[:, 0:TG], lhsT=ones32[:, :], rhs=sq[:], start=(c == 0), stop=(c == KC - 1)),
                         reads=[sk, "ones32"], writes=[pk2])
                S.op("dve", lambda e: e.tensor_scalar(cmean[:], ps1[:, 0:TG], 1.0 / D, None, ALU.mult), reads=[pk1], writes=["cmean"])
                S.op("dve", lambda e: e.tensor_tensor(cmsq[:], cmean[:], cmean[:], ALU.mult), reads=["cmean"], writes=["cmsq"])
                S.op("dve", lambda e: e.scalar_tensor_tensor(crstd[:], ps2[:, 0:TG], 1.0 / D, cmsq[:], ALU.mult, ALU.subtract),
                     reads=[pk2, "cmsq"], writes=["crstd"])
                S.op("dve", lambda e: e.tensor_scalar(crstd[:], crstd[:], LN_EPS, None, ALU.add), reads=["crstd"], writes=["crstd"])
                S.op("act", lambda e: e.activation(crstd[:], crstd[:], AF.Sqrt), reads=["crstd"], writes=["crstd"])
                S.op("dve", lambda e: e.reciprocal(crstd[:], crstd[:]), reads=["crstd"], writes=["crstd"])
                S.op("dve", lambda e: e.scalar_tensor_tensor(cnb[:], cmean[:], -1.0, crstd[:], ALU.mult, ALU.mult),
                     reads=["cmean", "crstd"], writes=["cnb"])
                for c in range(KC):
                    S.op("dve", lambda e, c=c: e.tensor_tensor(co[:, c, :], co[:, c, :], crstd[:], ALU.mult),
                         reads=[("co", c), "crstd"], writes=[("co", c)])
                    S.op("dve", lambda e, c=c: e.tensor_tensor(co[:, c, :], co[:, c, :], cnb[:], ALU.add),
                         reads=[("co", c), "cnb"], writes=[("co", c)])
                    S.op("act", lambda e, c=c: e.activation(cn[:, c, :], co[:, c, :], AF.Silu, bias=bfm[:, 160 + c:161 + c],
                                                            scale=bfm[:, 144 + c:145 + c]),
                         reads=[("co", c), "bfm"], writes=[("cn", c), ("vc", c // 2)])

            def spatial():
                for tt in range(TT):
                    for q in range(4):
                        pk, ps = next_ps()

                        def f_mm(e, tt=tt, q=q, ps=ps):
                            ins = None
                            for j in range(4):
                                g16 = 4 * q + j
                                e.matmul(ps[:, j * 128:(j + 1) * 128], lhsT=vn[:, tt, g16 * 128:(g16 + 1) * 128], rhs=wsT[:, g16, :],
                                         start=True, stop=False)
                                ins = e.matmul(ps[:, j * 128:(j + 1) * 128], lhsT=ones0[:, :], rhs=bsrow[:, g16, :], start=False, stop=True)
                            return ins
                        S.op("pe", f_mm, reads=[("vn", tt), "wsT", "bsrow", "ones0"], writes=[pk])
                        uk = [("uT", 4 * q + j) for j in range(4)]
                        S.op("dve", lambda e, tt=tt, q=q, ps=ps: e.tensor_tensor(
                            uT[:, 4 * q:4 * q + 4, tt * 128:(tt + 1) * 128], uT[:, 4 * q:4 * q + 4, tt * 128:(tt + 1) * 128],
                            ps[:, :].rearrange("p (a b) -> p a b", a=4), ALU.mult), reads=[pk] + uk, writes=uk)

            def epilogue(g):
                tok0 = g * TG
                ln_group(2, [(xh0[:, tt, :], xkeys(tt), xh0[:, tt, :], xkeys(tt), 128) for tt in range(TT)])
                for tt in range(TT):
                    S.op("sp", lambda e, tt=tt: e.dma_start(out=h1scr[tok0 + tt * 128: tok0 + (tt + 1) * 128, :], in_=xh0[:, tt, :]),
                         reads=xkeys(tt), writes=[("h1scr", g * TT + tt)], dma=("h1st", tt))

            def make_consume(g, bi):
                def consume(wbt, wbk):
                    if bi == P1_ORDER[0]:
                        prologue(g)
                    h0rhs = lambda kc: h0T[:, kc, :]
                    if bi < 4:
                        def ev(j, ps, pk):
                            c = bi * 4 + j
                            S.op("act", lambda e: e.activation(uT[:, c, :], ps[:, 0:TG], AF.Gelu, bias=bfm[:, c:c + 1]),
                                 reads=[pk, "bfm"], writes=[("uT", c)])
                        fform(wbt, wbk, h0rhs, h0T_keys(), TG, ev)
                    elif bi < 8:
                        dg = bi - 4

                        def ev(tt, ps, pk):
                            S.op("act", lambda e: e.activation(vt[:, tt, dg * 512:(dg + 1) * 512], ps[:, :], AF.Gelu),
                                 reads=[pk], writes=[("vc", tt * 4 + dg)])
                        tform(wbt, wbk, lambda kc, tt: h0T[:, kc, tt * 128:(tt + 1) * 128],
                              lambda tt: [("h0T", tt, q) for q in range(4)], TT, bvrow[:, dg * 512:(dg + 1) * 512], "bvrow", ev)
                        if bi == 7:
                            ln_group(1, [(vt[:, tt, :], [("vc", tt * 4 + d_) for d_ in range(4)], vn[:, tt, :], [("vn", tt)], 128)
                                         for tt in range(TT)])
                    elif bi < 12:
                        def ev(j, ps, pk):
                            c = (bi - 8) * 4 + j
                            S.op("act", lambda e: e.activation(cT[:, c, HALO:HALO + TG], ps[:, 0:TG], AF.Identity, bias=bfm[:, 32 + c:33 + c]),
                                 reads=[pk, "bfm"], writes=[("cT", c)])
                            if g == 0:
                                pk2, ps2 = next_ps()

                                def f_mm(e, ps2=ps2, j=j):
                                    ins = None
                                    for kc in range(KC):
                                        ins = e.matmul(ps2[:, 0:HALO], lhsT=wbt[:, kc, j * 128:(j + 1) * 128], rhs=h0Th[:, kc, :],
                                                       start=(kc == 0), stop=(kc == KC - 1))
                                    return ins
                                S.op("pe", f_mm, reads=[wbk, "h0Th"], writes=[pk2])
                                S.op("act", lambda e, ps2=ps2: e.activation(cT[:, c, 0:HALO], ps2[:, 0:HALO], AF.Identity,
                                                                            bias=bfm[:, 32 + c:33 + c]),
                                     reads=[pk2, "bfm"], writes=[("cT", c)])
                        fform(wbt, wbk, h0rhs, h0T_keys(), TG, ev)
                    elif bi < 16:
                        def ev(j, ps, pk):
                            c = (bi - 12) * 4 + j
                            S.op("act", lambda e: e.activation(mT[:, c, :], ps[:, 0:TG], AF.Sigmoid, bias=bfm[:, 48 + c:49 + c]),
                                 reads=[pk, "bfm"], writes=[("mT", c)])
                            if g == 0:
                                pk2, ps2 = next_ps()

                                def f_mm(e, ps2=ps2, j=j):
                                    ins = None
                                    for kc in range(KC):
                                        ins = e.matmul(ps2[:, 0:HALO], lhsT=wbt[:, kc, j * 128:(j + 1) * 128], rhs=h0Th[:, kc, :],
                                                       start=(kc == 0), stop=(kc == KC - 1))
                                    return ins
                                S.op("pe", f_mm, reads=[wbk, "h0Th"], writes=[pk2])
                                S.op("act", lambda e, ps2=ps2: e.activation(sgh[:], ps2[:, 0:HALO], AF.Sigmoid, bias=bfm[:, 48 + c:49 + c]),
                                     reads=[pk2, "bfm"], writes=["sgh"])
                                S.op("dve", lambda e: e.scalar_tensor_tensor(cT[:, c, 0:HALO], cT[:, c, 0:HALO], hmask[:, 0:1], sgh[:],
                                                                             ALU.mult, ALU.mult),
                                     reads=["sgh", "hmask", ("cT", c)], writes=[("cT", c)])
                        fform(wbt, wbk, h0rhs, h0T_keys(), TG, ev)
                        cs = [(bi - 12) * 4 + j for j in range(4)]
                        for c in cs:
                            S.op("dve", lambda e, c=c: e.tensor_tensor(cT[:, c, HALO:HALO + TG], cT[:, c, HALO:HALO + TG], mT[:, c, :], ALU.mult),
                                 reads=[("mT", c), ("cT", c)], writes=[("cT", c)])
                        conv_chunks(cs)
                    elif bi < 24:
                        def ev(j, ps, pk):
                            c = ((bi - 16) % 4) * 4 + j
                            dst, nm, off = (gaT, "gaT", 64) if bi < 20 else (gbT, "gbT", 80)
                            S.op("act", lambda e: e.activation(dst[:, c, :], ps[:, 0:TG], AF.Sigmoid, bias=bfm[:, off + c:off + c + 1]),
                                 reads=[pk, "bfm"], writes=[(nm, c)])
                        fform(wbt, wbk, h0rhs, h0T_keys(), TG, ev)
                    elif bi < 28:
                        if bi == 24:
                            spatial()
                        def ev(j, ps, pk):
                            c = (bi - 24) * 4 + j
                            S.op("dve", lambda e: e.scalar_tensor_tensor(mT[:, c, :], ps[:, 0:TG], bfm[:, 96 + c:97 + c], gaT[:, c, :],
                                                                         ALU.add, ALU.mult),
                                 reads=[pk, "bfm", ("gaT", c)], writes=[("mT", c)])
                        fform(wbt, wbk, lambda kc: uT[:, kc, :], [("uT", c) for c in range(KC)], TG, ev)
                    elif bi < 32:
                        if bi == 28:
                            conv_ln()
                        def ev(j, ps, pk):
                            c = (bi - 28) * 4 + j
                            sg = sgt[c % 2]
                            sk = ("sgt", c % 2)
                            S.op("dve", lambda e: e.scalar_tensor_tensor(sg[:], ps[:, 0:TG], bfm[:, 112 + c:113 + c], gbT[:, c, :],
                                                                         ALU.add, ALU.mult),
                                 reads=[pk, "bfm", ("gbT", c)], writes=[sk])
                            S.op("dve", lambda e: e.tensor_tensor(mT[:, c, :], mT[:, c, :], sg[:], ALU.add),
                                 reads=[sk, ("mT", c)], writes=[("mT", c)])
                        fform(wbt, wbk, lambda kc: cn[:, kc, :], [("cn", c) for c in range(KC)], TG, ev)
                    else:
                        dg = bi - 32

                        def ev(tt, ps, pk):
                            S.op("dve", lambda e: e.scalar_tensor_tensor(xh0[:, tt, dg * 512:(dg + 1) * 512], xh0[:, tt, dg * 512:(dg + 1) * 512],
                                                                         DN_ALPHA, ps[:, :], ALU.mult, ALU.add),
                                 reads=[pk, ("xh0", tt, dg)], writes=[("xh0", tt, dg)])
                        tform(wbt, wbk, lambda kc, tt: mT[:, kc, tt * 128:(tt + 1) * 128],
                              lambda tt: [("mT", c) for c in range(KC)], TT, borow[:, dg * 512:(dg + 1) * 512], "borow", ev)
                        if bi == 35:
                            epilogue(g)
                return consume

            blocks = []
            for g in range(NG):
                for bi in P1_ORDER:
                    if bi < 24:
                        src = wview(w_in, bi * 512)
                    elif bi < 28:
                        src = wview(w_sgu, (bi - 24) * 512)
                    elif bi < 32:
                        src = wview(w_cv, (bi - 28) * 512)
                    else:
                        src = wview(w_o, (bi - 32) * 512)
                    blocks.append((src, make_consume(g, bi)))

            def run_stream(blocks):
                def issue(i):
                    src = blocks[i][0]
                    b = i % NWB
                    S.op("pool", lambda e: e.dma_start(out=wb[b][:], in_=src), writes=[("wb", b)], dma=("wb", b))
                for i in range(min(NWB, len(blocks))):
                    issue(i)
                for i in range(len(blocks)):
                    blocks[i][1](wb[i % NWB], ("wb", i % NWB))
                    if i + NWB < len(blocks):
                        issue(i + NWB)

            run_stream(blocks)
            S.barrier()

        NTT = HALF // 128
        NST = CAP // 128
        uniq = [0]

        def sbn(name, shape, dt, stack):
            uniq[0] += 1
            return stack.enter_context(nc.sbuf_tensor("s_%s_%d" % (name, uniq[0]), list(shape), dt))

        with ExitStack() as p2:
            acc = sb("acc", [128, NTT, D], F32, p2)
            Gt = sb("G", [128, NTT, E], F32, p2)
            Mf = sb("Mf", [128, NTT, E], F32, p2)
            Mbf = sb("Mbf", [128, NTT, E], BF16, p2)
            rank = sb("rank", [128, NTT, E], F32, p2)
            iota = sb("iota", [128, CAP], F32, p2)
            identbf = sb("identbf", [128, 128], BF16, p2)
            allones = sb("allones", [128, 128], BF16, p2)
            striu = sb("striu", [128, 128], BF16, p2)
            trif = sb("trif", [128, 128], F32, p2)
            lg = sb("lg", [128, E], F32, p2)
            ex = sb("ex", [128, E], F32, p2)
            m8 = sb("m8", [128, 8], F32, p2)
            nmx = sb("nmx", [128, 1], F32, p2)
            ssum = sb("ssum", [128, 1], F32, p2)
            b1t = sb("b1t", [128, E, 32], F32, p2)

            S.op("sp", lambda e: e.dma_start(out=iota[:], in_=iota_d[:, 0:CAP]), writes=["iota"], dma="c_iota")
            S.op("sp", lambda e: e.dma_start(out=trif[:], in_=tri_d[:, :]), writes=["trif"], dma="c_trif")
            S.op("sp", lambda e: e.dma_start(out=b1t[:], in_=b1_d.rearrange("p (a b) -> p a b", a=E)), writes=["b1t"], dma="c_b1t")
            S.op("dve", lambda e: e.tensor_copy(identbf[:], ident[:]), reads=["ident"], writes=["identbf"])
            S.op("dve", lambda e: e.memset(allones[:], 1.0), writes=["allones"])
            S.op("dve", lambda e: e.tensor_tensor(striu[:], trif[:], ident[:], ALU.subtract), reads=["trif", "ident"], writes=["striu"])

            def akeys(tt):
                return [("acc", tt, dg) for dg in range(4)]

            PRE = {}
            EXP = {}

            def open_pre():
                st = ExitStack()
                PRE.clear()
                PRE["stack"] = st
                PRE["h1T"] = sbn("h1T", [128, KC, HALF], BF16, st)
                PRE["pt"] = sbn("pt", [128, PLE], F32, st)
                PRE["pT"] = sbn("pT", [128, 2, HALF], BF16, st)
                PRE["wple"] = sbn("wple", [128, 2, D], BF16, st)
                PRE["wr"] = sbn("wr", [128, KC, E], BF16, st)
                PRE["brrow"] = sbn("brrow", [128, E], BF16, st)
                PRE["bpgrow"] = sbn("bpgrow", [128, D], BF16, st)
                PRE["b2bf"] = sbn("b2bf", [128, D], BF16, st)
                PRE["GT"] = sbn("GT", [128, HALF], BF16, st)
                PRE["tmp"] = [sbn("ptmp", [128, 512], F32, st) for _ in range(2)]
                wple, wr, brrow, bpgrow, b2bf, GT = (PRE[k] for k in ("wple", "wr", "brrow", "bpgrow", "b2bf", "GT"))
                S.op("pool", lambda e: e.dma_start(out=wple[:], in_=w_ple.rearrange("(kc p) n -> p kc n", p=128)), writes=["wple"], dma="c_wple")
                S.op("pool", lambda e: e.dma_start(out=wr[:], in_=w_rt.rearrange("(kc p) n -> p kc n", p=128)), writes=["wr"], dma="c_wr")
                S.op("dve", lambda e: e.memset(brrow[:], 0.0), writes=["brrow"])
                S.op("pool", lambda e: e.dma_start(out=brrow[0:1, :], in_=brt_d[:, :]), writes=["brrow"], dma="c_brrow")
                S.op("dve", lambda e: e.memset(bpgrow[:], 0.0), writes=["bpgrow"])
                S.op("pool", lambda e: e.dma_start(out=bpgrow[0:1, :], in_=bpg_d[:, :]), writes=["bpgrow"], dma="c_bpgrow")
                S.op("dve", lambda e: e.memset(b2bf[:], 0.0), writes=["b2bf"])
                S.op("pool", lambda e: e.dma_start(out=b2bf[0:E, :], in_=b2_d[:, :]), writes=["b2bf"], dma="c_b2bf")
                S.op("dve", lambda e: e.memset(GT[:], 0.0), writes=["GT"])

            def close_pre():
                S.barrier()
                PRE["stack"].close()

            def open_exp(hh):
                st = ExitStack()
                EXP.clear()
                EXP["stack"] = st
                h1tm = sbn("h1tm", [128, NTT, D], BF16, st)
                EXP["h1tm"] = h1tm
                for tt in range(NTT):
                    r0 = hh * HALF + tt * 128
                    S.op("pool", lambda e, tt=tt, r0=r0: e.dma_start(out=h1tm[:, tt, :], in_=h1scr[r0:r0 + 128, :]),
                         reads=[("h1scr", hh * NTT + tt)], writes=[("h1tm", tt)], dma=("h1tm", tt % 2))
                EXP["xgT"] = sbn("xgT", [128, KC, CAP], BF16, st)
                EXP["actT"] = sbn("actT", [128, KC, CAP], BF16, st)
                EXP["Pe"] = sbn("Pe", [128, NTT, CAP], BF16, st)
                EXP["Pw"] = sbn("Pw", [128, NTT, CAP], BF16, st)
                EXP["PTw"] = [sbn("PTw", [128, NST * NTT, 128], BF16, st) for _ in range(2)]
                EXP["ysb"] = [sbn("ysb", [128, NST, 512], BF16, st) for _ in range(2)]
                EXP["tA"] = [sbn("tA", [128, CAP], F32, st) for _ in range(2)]
                EXP["tB"] = [sbn("tB", [128, CAP], F32, st) for _ in range(2)]
                EXP["lng2"] = sbn("lng2", [128, 512], F32, st)
                EXP["lnb2"] = sbn("lnb2", [128, 512], F32, st)
                EXP["rot"] = {"A": 0, "B": 0, "Y": 0}

            def close_exp():
                S.barrier()
                EXP["stack"].close()

            def half_prologue(hh):
                t0 = hh * HALF
                open_pre()
                h1T, pt, pT, wr, brrow, b2bf, GT = (PRE[k] for k in ("h1T", "pt", "pT", "wr", "brrow", "b2bf", "GT"))
                for tt in range(NTT):
                    S.op("sp", lambda e, tt=tt: e.dma_start(out=acc[:, tt, :], in_=h1scr[t0 + tt * 128:t0 + (tt + 1) * 128, :]),
                         reads=[("h1scr", hh * NTT + tt)], writes=akeys(tt), dma=("h1ld", tt % 2))
                for tt in range(NTT):
                    transpose_tile(acc[:, tt, :], akeys(tt), h1T, lambda q, tt=tt: ("h1T", tt), tt * 128)
                    S.op("sp", lambda e, tt=tt: e.dma_start(out=pt[:], in_=p_d[t0 + tt * 128:t0 + (tt + 1) * 128, :]),
                         writes=["pt"], dma="pt")
                    transpose_tile(pt, ["pt"], pT, lambda q, tt=tt: ("pT", tt), tt * 128, nkc=2)
                    pk, ps = next_ps()

                    def f_mm(e, tt=tt, ps=ps):
                        for kc in range(KC):
                            e.matmul(ps[:, 0:E], lhsT=h1T[:, kc, tt * 128:(tt + 1) * 128], rhs=wr[:, kc, :], start=(kc == 0), stop=False)
                        return e.matmul(ps[:, 0:E], lhsT=ones0[:, :], rhs=brrow[:, :], start=False, stop=True)
                    S.op("pe", f_mm, reads=[("h1T", tt), "wr", "brrow", "ones0"], writes=[pk])
                    S.op("dve", lambda e, ps=ps: e.tensor_copy(lg[:], ps[:, 0:E]), reads=[pk], writes=["lg"])
                    S.op("dve", lambda e: e.max(m8[:], lg[:]), reads=["lg"], writes=["m8"])
                    S.op("dve", lambda e, tt=tt: e.tensor_scalar(Mf[:, tt, :], lg[:], m8[:, 3:4], None, ALU.is_ge), reads=["lg", "m8"], writes=[("Mf", tt)])
                    S.op("dve", lambda e, tt=tt: e.tensor_copy(Mbf[:, tt, :], Mf[:, tt, :]), reads=[("Mf", tt)], writes=[("Mbf", tt)])
                    S.op("dve", lambda e: e.tensor_scalar(nmx[:], m8[:, 0:1], -1.0, None, ALU.mult), reads=["m8"], writes=["nmx"])
                    S.op("act", lambda e: e.activation(ex[:], lg[:], AF.Exp, bias=nmx[:, 0:1]), reads=["lg", "nmx"], writes=["ex"])
                    S.op("dve", lambda e, tt=tt: e.tensor_tensor(ex[:], ex[:], Mf[:, tt, :], ALU.mult), reads=["ex", ("Mf", tt)], writes=["ex"])
                    S.op("dve", lambda e: e.reduce_sum(ssum[:], ex[:], AX.X), reads=["ex"], writes=["ssum"])
                    S.op("dve", lambda e: e.reciprocal(ssum[:], ssum[:]), reads=["ssum"], writes=["ssum"])
                    S.op("dve", lambda e, tt=tt: e.tensor_scalar(Gt[:, tt, :], ex[:], ssum[:, 0:1], None, ALU.mult),
                         reads=["ex", "ssum"], writes=[("G", tt)])
                    pkr, psr = next_ps()

                    def f_rank(e, tt=tt, psr=psr):
                        ins = None
                        for t2 in range(tt + 1):
                            ins = e.matmul(psr[:, 0:E], lhsT=(allones[:, :] if t2 < tt else striu[:, :]), rhs=Mbf[:, t2, :],
                                           start=(t2 == 0), stop=(t2 == tt))
                        return ins
                    S.op("pe", f_rank, reads=[("Mbf", t2) for t2 in range(tt + 1)] + ["allones", "striu"], writes=[pkr])
                    S.op("dve", lambda e, tt=tt, psr=psr: e.tensor_copy(rank[:, tt, :], psr[:, 0:E]), reads=[pkr], writes=[("rank", tt)])
                    pk2, ps2 = next_ps()
                    S.op("pe", lambda e, tt=tt, ps2=ps2: e.transpose(ps2[0:E, 0:128], Gt[:, tt, :], ident[:, :]),
                         reads=[("G", tt), "ident"], writes=[pk2])
                    S.op("act", lambda e, tt=tt, ps2=ps2: e.activation(GT[0:E, tt * 128:(tt + 1) * 128], ps2[0:E, 0:128], AF.Identity),
                         reads=[pk2], writes=[("GT", tt)])
                    S.op("dve", lambda e, tt=tt: e.tensor_scalar(acc[:, tt, :], acc[:, tt, :], DN_ALPHA, None, ALU.mult),
                         reads=akeys(tt), writes=akeys(tt))
                    for dg in range(4):
                        pk3, ps3 = next_ps()
                        S.op("pe", lambda e, tt=tt, dg=dg, ps3=ps3: e.matmul(ps3[:, :], lhsT=GT[:, tt * 128:(tt + 1) * 128],
                                                                             rhs=b2bf[:, dg * 512:(dg + 1) * 512], start=True, stop=True),
                             reads=[("GT", tt), "GT", "b2bf"], writes=[pk3])
                        S.op("dve", lambda e, tt=tt, dg=dg, ps3=ps3: e.tensor_tensor(acc[:, tt, dg * 512:(dg + 1) * 512],
                                                                                    acc[:, tt, dg * 512:(dg + 1) * 512], ps3[:, :], ALU.add),
                             reads=[pk3, ("acc", tt, dg)], writes=[("acc", tt, dg)])

            def make_ple(hh, dg):
                def consume(wbt, wbk):
                    if dg == 0:
                        half_prologue(hh)
                    h1T, pT, wple, bpgrow = (PRE[k] for k in ("h1T", "pT", "wple", "bpgrow"))
                    tmps = PRE["tmp"]

                    def ev(tt, ps, pk):
                        sg = tmps[tt % 2]
                        sk = ("ptmp", tt % 2)
                        S.op("act", lambda e: e.activation(sg[:], ps[:, :], AF.Sigmoid), reads=[pk], writes=[sk])
                        pk2, ps2 = next_ps()

                        def f_mm(e):
                            e.matmul(ps2[:, :], lhsT=pT[:, 0, tt * 128:(tt + 1) * 128], rhs=wple[:, 0, dg * 512:(dg + 1) * 512], start=True, stop=False)
                            return e.matmul(ps2[:, :], lhsT=pT[:, 1, tt * 128:(tt + 1) * 128], rhs=wple[:, 1, dg * 512:(dg + 1) * 512],
                                            start=False, stop=True)
                        S.op("pe", f_mm, reads=[("pT", tt), "wple"], writes=[pk2])
                        S.op("dve", lambda e: e.tensor_tensor(sg[:], sg[:], ps2[:, :], ALU.mult), reads=[sk, pk2], writes=[sk])
                        S.op("dve", lambda e: e.tensor_tensor(acc[:, tt, dg * 512:(dg + 1) * 512], acc[:, tt, dg * 512:(dg + 1) * 512], sg[:], ALU.add),
                             reads=[sk, ("acc", tt, dg)], writes=[("acc", tt, dg)])
                    tform(wbt, wbk, lambda kc, tt: h1T[:, kc, tt * 128:(tt + 1) * 128], lambda tt: [("h1T", tt)], NTT,
                          bpgrow[:, dg * 512:(dg + 1) * 512], "bpgrow", ev)
                    if dg == 3:
                        close_pre()
                        open_exp(hh)
                return consume

            def etmp(kind):
                i = EXP["rot"][kind] % 2
                EXP["rot"][kind] += 1
                return {"A": EXP["tA"], "B": EXP["tB"], "Y": EXP["ysb"]}[kind][i], ("e" + kind, i)

            def expert_prologue(ex_):
                xgT, Pe, Pw, h1tm = EXP["xgT"], EXP["Pe"], EXP["Pw"], EXP["h1tm"]
                pb = ex_ % 2
                PTw = EXP["PTw"][pb]
                for tt in range(NTT):
                    S.op("dve", lambda e, tt=tt: e.tensor_scalar(Pe[:, tt, :], iota[:], rank[:, tt, ex_:ex_ + 1], Mf[:, tt, ex_:ex_ + 1],
                                                                 ALU.is_equal, ALU.mult),
                         reads=["iota", ("rank", tt), ("Mf", tt)], writes=[("Pe", tt)])
                    S.op("dve", lambda e, tt=tt: e.tensor_scalar(Pw[:, tt, :], iota[:], rank[:, tt, ex_:ex_ + 1], Gt[:, tt, ex_:ex_ + 1],
                                                                 ALU.is_equal, ALU.mult),
                         reads=["iota", ("rank", tt), ("G", tt)], writes=[("Pw", tt)])
                npair = 512 // CAP
                for q in range(KC // npair):
                    pk, ps = next_ps()

                    def f_mm(e, q=q, ps=ps):
                        ins = None
                        for j in range(npair):
                            dc = q * npair + j
                            for tt in range(NTT):
                                ins = e.matmul(ps[:, j * CAP:(j + 1) * CAP], lhsT=h1tm[:, tt, dc * 128:(dc + 1) * 128], rhs=Pe[:, tt, :],
                                               start=(tt == 0), stop=(tt == NTT - 1))
                        return ins
                    S.op("pe", f_mm, reads=[("Pe", tt) for tt in range(NTT)] + [("h1tm", tt) for tt in range(NTT)], writes=[pk])
                    S.op("act", lambda e, q=q, ps=ps: e.activation(xgT[:, q * npair:(q + 1) * npair, :],
                                                                   ps[:, 0:npair * CAP].rearrange("p (a b) -> p a b", a=npair), AF.Identity),
                         reads=[pk], writes=[("xgT", q)])
                nblk = NST * NTT
                for q in range((nblk + 3) // 4):
                    pk, ps = next_ps()
                    n = min(4, nblk - q * 4)

                    def f_mm(e, q=q, ps=ps, n=n):
                        ins = None
                        for j in range(n):
                            blk = q * 4 + j
                            st_, tt = blk // NTT, blk % NTT
                            ins = e.matmul(ps[:, j * 128:(j + 1) * 128], lhsT=Pw[:, tt, st_ * 128:(st_ + 1) * 128], rhs=identbf[:, :],
                                           start=True, stop=True)
                        return ins
                    S.op("pe", f_mm, reads=[("Pw", tt) for tt in range(NTT)] + ["identbf"], writes=[pk])
                    S.op("act", lambda e, q=q, ps=ps, n=n: e.activation(PTw[:, q * 4:q * 4 + n, :],
                                                                        ps[:, 0:n * 128].rearrange("p (a b) -> p a b", a=n), AF.Identity),
                         reads=[pk], writes=[("PTw", pb)])

            def make_w1(hh, ex_, bi):
                def consume(wbt, wbk):
                    if bi == 0:
                        expert_prologue(ex_)
                    xgT, actT = EXP["xgT"], EXP["actT"]
                    for j in range(4):
                        fc = (bi % 4) * 4 + j
                        pk, ps = next_ps()

                        def f_mm(e, j=j, ps=ps):
                            ins = None
                            for kc in range(KC):
                                ins = e.matmul(ps[:, 0:CAP], lhsT=wbt[:, kc, j * 128:(j + 1) * 128], rhs=xgT[:, kc, :],
                                               start=(kc == 0), stop=(kc == KC - 1))
                            return ins
                        S.op("pe", f_mm, reads=[wbk] + [("xgT", q) for q in range(KC * CAP // 512)], writes=[pk])
                        dst = actT[:, fc, :]
                        ak = ("actT", fc)
                        if bi < 4:
                            gc, gk = etmp("A")
                            sg, sk = etmp("B")
                            S.op("dve", lambda e, ps=ps, gc=gc, fc=fc: e.tensor_scalar(gc[:], ps[:, 0:CAP], b1t[:, ex_, fc:fc + 1], SW_LIM,
                                                                                       ALU.add, ALU.min),
                                 reads=[pk, "b1t"], writes=[gk])
                            S.op("act", lambda e, gc=gc, sg=sg: e.activation(sg[:], gc[:], AF.Sigmoid, scale=SW_ALPHA),
                                 reads=[gk], writes=[sk])
                            S.op("dve", lambda e, gc=gc, sg=sg, dst=dst: e.tensor_tensor(dst, gc[:], sg[:], ALU.mult),
                                 reads=[gk, sk], writes=[ak])
                        else:
                            ub, uk = etmp("A")
                            S.op("act", lambda e, ps=ps, ub=ub, fc=fc: e.activation(ub[:], ps[:, 0:CAP], AF.Identity,
                                                                                    bias=b1t[:, ex_, 16 + fc:17 + fc]),
                                 reads=[pk, "b1t"], writes=[uk])
                            S.op("dve", lambda e, ub=ub: e.tensor_scalar(ub[:], ub[:], SW_LIM, -SW_LIM, ALU.min, ALU.max),
                                 reads=[uk], writes=[uk])
                            S.op("dve", lambda e, ub=ub, dst=dst: e.scalar_tensor_tensor(dst, ub[:], 1.0, dst, ALU.add, ALU.mult),
                                 reads=[uk, ak], writes=[ak])
                return consume

            def make_w2(hh, ex_, dg):
                def consume(wbt, wbk):
                    actT = EXP["actT"]
                    pb = ex_ % 2
                    PTw = EXP["PTw"][pb]
                    ysb, yk = etmp("Y")
                    for st_ in range(NST):
                        pk, ps = next_ps()

                        def f_mm(e, st_=st_, ps=ps):
                            ins = None
                            for fc in range(KC):
                                ins = e.matmul(ps[:, :], lhsT=actT[:, fc, st_ * 128:(st_ + 1) * 128], rhs=wbt[:, fc, :],
                                               start=(fc == 0), stop=(fc == KC - 1))
                            return ins
                        S.op("pe", f_mm, reads=[wbk] + [("actT", fc) for fc in range(KC)], writes=[pk])
                        S.op("act", lambda e, st_=st_, ps=ps: e.activation(ysb[:, st_, :], ps[:, :], AF.Identity), reads=[pk], writes=[yk])
                    for tt in range(NTT):
                        pk, ps = next_ps()

                        def f_sc(e, tt=tt, ps=ps):
                            ins = None
                            for st_ in range(NST):
                                ins = e.matmul(ps[:, :], lhsT=PTw[:, st_ * NTT + tt, :], rhs=ysb[:, st_, :], start=(st_ == 0), stop=(st_ == NST - 1))
                            return ins
                        S.op("pe", f_sc, reads=[yk, ("PTw", pb)], writes=[pk])
                        S.op("dve", lambda e, tt=tt, ps=ps: e.tensor_tensor(acc[:, tt, dg * 512:(dg + 1) * 512], acc[:, tt, dg * 512:(dg + 1) * 512],
                                                                            ps[:, :], ALU.add),
                             reads=[pk, ("acc", tt, dg)], writes=[("acc", tt, dg)])
                    if ex_ == E - 1 and dg == 3:
                        half_epilogue(hh)
                return consume

            def half_epilogue(hh):
                t0 = hh * HALF
                lng2, lnb2 = EXP["lng2"], EXP["lnb2"]
                for tt in range(NTT):
                    ln_tm(acc[:, tt, :], akeys(tt), acc[:, tt, :], akeys(tt), affine=False)
                for dg in range(4):
                    S.op("sp", lambda e, dg=dg: e.dma_start(out=lng2[:], in_=lnv[6:7, dg * 512:(dg + 1) * 512].partition_broadcast(128)),
                         writes=["lng2"], dma="lng2")
                    S.op("sp", lambda e, dg=dg: e.dma_start(out=lnb2[:], in_=lnv[7:8, dg * 512:(dg + 1) * 512].partition_broadcast(128)),
                         writes=["lnb2"], dma="lnb2")
                    for tt in range(NTT):
                        a_ = acc[:, tt, dg * 512:(dg + 1) * 512]
                        S.op("dve", lambda e, a_=a_: e.tensor_tensor(a_, a_, lng2[:], ALU.mult), reads=[("acc", tt, dg), "lng2"], writes=[("acc", tt, dg)])
                        S.op("dve", lambda e, a_=a_: e.tensor_tensor(a_, a_, lnb2[:], ALU.add), reads=[("acc", tt, dg), "lnb2"], writes=[("acc", tt, dg)])
                for tt in range(NTT):
                    S.op("sp", lambda e, tt=tt: e.dma_start(out=out[t0 + tt * 128:t0 + (tt + 1) * 128, :], in_=acc[:, tt, :]),
                         reads=akeys(tt), writes=[("out", hh * NTT + tt)], dma=("ost", tt % 2))
                close_exp()

            blocks2 = []
            for hh in range(NT // HALF):
                for dg in range(4):
                    blocks2.append((wview(w_pg, dg * 512), make_ple(hh, dg)))
                for ex_ in range(E):
                    for bi in range(8):
                        blocks2.append((wview(w_e1[ex_], bi * 512), make_w1(hh, ex_, bi)))
                    for dg in range(4):
                        blocks2.append((wview(w_e2[ex_], dg * 512), make_w2(hh, ex_, dg)))
            run_stream(blocks2)
            S.op("sp", None, reads=[("out", i) for i in range(NT // 128)])

            S.finalize(top)
            with nc.Block() as block:
                S.emit(block)
    return nc


def _core_inputs(c, NT, E, inp, n_per_batch):
    b = c // n_per_batch
    s0 = (c % n_per_batch) * NT
    f = lambda a: np.ascontiguousarray(a, dtype=np.float32)
    x = inp["x"]
    m = {}
    m["x"] = f(x[b, s0:s0 + NT])
    if s0 == 0:
        m["xhalo"] = np.zeros((HALO, D), np.float32)
        m["hmask"] = np.zeros((128, 1), np.float32)
    else:
        m["xhalo"] = f(x[b, s0 - HALO:s0])
        m["hmask"] = np.ones((128, 1), np.float32)
    m["p"] = f(inp["p"][0, b, s0:s0 + NT])
    m["w_in"] = f(inp["w_in"][0])
    m["w_sgu_out"] = f(inp["w_sgu_out"][0])
    m["w_conv_out"] = f(inp["w_conv_out"][0])
    m["w_o"] = f(inp["w_o"][0])
    m["w_pg"] = f(inp["w_pg"][0])
    m["w_ple"] = f(inp["w_ple"][0])
    m["w_router"] = f(inp["w_router"][0])
    m["w_e1"] = f(inp["w_e1"][0])
    m["w_e2"] = f(inp["w_e2"][0])
    m["lnv"] = f(np.stack([inp["ln0_g"], inp["ln0_b"], inp["sgu_ln_g"][0], inp["sgu_ln_b"][0],
                           inp["ln1_g"][0], inp["ln1_b"][0], inp["ln2_g"][0], inp["ln2_b"][0]], 0))
    fm = lambda v: np.asarray(v).reshape(-1, 128).T
    m["bfm"] = f(np.concatenate([fm(inp["b_in"][0]), fm(inp["b_sgu_out"][0]), fm(inp["b_conv_out"][0]), fm(inp["conv_b"][0]),
                                 fm(inp["conv_ln_g"][0]), fm(inp["conv_ln_b"][0])], axis=1))
    m["convw"] = f(np.asarray(inp["conv_w"][0]).T.reshape(KC, 128, CW).transpose(1, 0, 2).reshape(128, KC * CW))
    m["bv"] = f(np.asarray(inp["b_in"][0])[None, D:2 * D])
    m["bo"] = f(np.asarray(inp["b_o"][0])[None])
    m["bpg"] = f(np.asarray(inp["b_pg"][0])[None])
    m["brt"] = f(np.asarray(inp["b_router"][0])[None])
    m["wsT"] = f(np.asarray(inp["w_s"][0]).transpose(2, 0, 1).reshape(128, 16 * 128))
    m["bs"] = f(np.asarray(inp["b_s"][0]).reshape(1, 16 * 128))
    m["b1"] = f(np.asarray(inp["b_e1"][0]).reshape(E, 32, 128).transpose(2, 0, 1).reshape(128, E * 32))
    m["b2"] = f(inp["b_e2"][0])
    m["ident"] = np.eye(128, dtype=np.float32)
    m["tri"] = np.triu(np.ones((128, 128), np.float32))
    m["iota"] = np.ascontiguousarray(np.broadcast_to(np.arange(512, dtype=np.float32), (128, 512)))
    return m


_NC_CACHE = {}


def kernel(**inputs):
    inp = {k: np.asarray(v) for k, v in inputs.items()}
    B, SEQ, _ = inp["x"].shape
    E = inp["w_router"].shape[-1]
    n_cores = 8
    NT = B * SEQ // n_cores
    n_per_batch = SEQ // NT
    key = (NT, E)
    if key not in _NC_CACHE:
        _NC_CACHE[key] = build_program(NT=NT, E=E, HALF=min(1024, NT))
    nc = _NC_CACHE[key]
    in_maps = [_core_inputs(c, NT, E, inp, n_per_batch) for c in range(n_cores)]
    res = run_bass_kernel_spmd(nc, in_maps, core_ids=list(range(n_cores)))
    outs = [np.asarray(r["out"]) for r in res.results]
    return np.stack(outs, 0).reshape(B, SEQ, D).astype(np.float32)
```

```python
import numpy as np
from contextlib import ExitStack
import concourse.bass as bass
import concourse.mybir as mybir
from concourse.bass_utils import run_bass_kernel_spmd

F32 = mybir.dt.float32
BF16 = mybir.dt.bfloat16
ALU = mybir.AluOpType
AF = mybir.ActivationFunctionType
AX = mybir.AxisListType

D = 2048
KC = 16
DIN = 12288
PLE = 256
CW = 31
HALO = 32
TG = 256
LN_EPS = 1e-5
DN_ALPHA = 2.0 ** 0.25
SW_ALPHA = 1.702
SW_LIM = 7.0
NWB = 3
P1_ORDER = list(range(4, 16)) + list(range(0, 4)) + list(range(16, 36))
CAP = 256


class _Op:
    __slots__ = ("eng", "fn", "deps", "needs_inc", "dma", "sem", "val", "inc")

    def __init__(self, eng, fn, dma):
        self.eng, self.fn, self.dma = eng, fn, dma
        self.deps = []
        self.needs_inc = False
        self.sem = None
        self.val = 0
        self.inc = 1


class Sched:
    ENGS = ("pe", "act", "dve", "pool", "sp")

    def __init__(self, nc):
        self.nc = nc
        self.ops = {e: [] for e in self.ENGS}
        self.res = {}
        self.last_dma = {}
        self.nps = 0

    def op(self, eng, fn, reads=(), writes=(), dma=None):
        o = _Op(eng, fn, dma)
        seen = set()

        def add(d):
            if d is None or id(d) in seen:
                return
            if d.eng == "pe" and eng == "pe" and d.dma is None and dma is None:
                return
            seen.add(id(d))
            o.deps.append(d)

        for k in reads:
            st = self.res.setdefault(k, [None, []])
            add(st[0])
        for k in writes:
            st = self.res.setdefault(k, [None, []])
            add(st[0])
            for r in st[1]:
                add(r)
        if dma is not None:
            add(self.last_dma.get(dma))
            self.last_dma[dma] = o
        for k in reads:
            self.res[k][1].append(o)
        for k in writes:
            self.res[k][0] = o
            self.res[k][1] = []
        self.ops[eng].append(o)
        return o

    def barrier(self):
        keys = list(self.res.keys())
        for e in self.ENGS:
            self.op(e, None, reads=keys)
        for e in self.ENGS:
            self.op(e, None, writes=keys)

    def finalize(self, stack):
        nc = self.nc
        def expand(o, out, seen):
            for d in o.deps:
                if id(d) in seen:
                    continue
                seen.add(id(d))
                if d.fn is None:
                    expand(d, out, seen)
                else:
                    out.append(d)
        for e in self.ENGS:
            for o in self.ops[e]:
                out = []
                expand(o, out, set())
                o.deps = out
        for e in self.ENGS:
            for o in self.ops[e]:
                for d in o.deps:
                    d.needs_inc = True
        esem = {}
        for e in self.ENGS:
            esem[e] = stack.enter_context(nc.semaphore("sem_" + e))
        dsem = {}
        dcnt = {}
        for e in self.ENGS:
            cnt = 0
            for o in self.ops[e]:
                if o.fn is None:
                    continue
                if o.dma is not None:
                    if o.dma not in dsem:
                        dsem[o.dma] = stack.enter_context(nc.semaphore("dsem%d" % len(dsem)))
                        dcnt[o.dma] = 0
                    dcnt[o.dma] += 16
                    o.sem, o.val, o.inc = dsem[o.dma], dcnt[o.dma], 16
                    o.needs_inc = True
                elif o.needs_inc:
                    cnt += 1
                    o.sem, o.val, o.inc = esem[e], cnt, 1
        self.nsem = len(dsem) + len(esem)

    def emit(self, block):
        meth = {"pe": block.tensor, "act": block.scalar, "dve": block.vector, "pool": block.gpsimd, "sp": block.sync}
        for ename in self.ENGS:
            ops = self.ops[ename]

            def body(e, ops=ops):
                waited = {}
                for o in ops:
                    need = {}
                    for d in o.deps:
                        k = id(d.sem)
                        if k not in need or need[k][1] < d.val:
                            need[k] = (d.sem, d.val)
                    for k, (sem, val) in need.items():
                        if waited.get(k, 0) >= val:
                            continue
                        e.wait_ge(sem, val)
                        waited[k] = val
                    if o.fn is None:
                        continue
                    ins = o.fn(e)
                    if o.needs_inc:
                        ins.then_inc(o.sem, o.inc)

            meth[ename](body)


def build_program(NT=2048, E=32, HALF=1024):
    NG = NT // TG
    TT = TG // 128
    nc = bass.Bass("TRN2", target_bir_lowering=False)
    S = Sched(nc)

    def din(name, shape):
        return nc.dram_tensor(name, list(shape), F32, kind="ExternalInput").ap()

    x = din("x", [NT, D])
    xhalo = din("xhalo", [HALO, D])
    hmask_d = din("hmask", [128, 1])
    p_d = din("p", [NT, PLE])
    w_in = din("w_in", [D, DIN])
    w_sgu = din("w_sgu_out", [D, D])
    w_cv = din("w_conv_out", [D, D])
    w_o = din("w_o", [D, D])
    w_pg = din("w_pg", [D, D])
    w_ple = din("w_ple", [PLE, D])
    w_rt = din("w_router", [D, E])
    w_e1 = din("w_e1", [E, D, 2 * D])
    w_e2 = din("w_e2", [E, D, D])
    lnv = din("lnv", [8, D])
    bfm_d = din("bfm", [128, 176])
    convw_d = din("convw", [128, KC * CW])
    bv_d = din("bv", [1, D])
    bo_d = din("bo", [1, D])
    bpg_d = din("bpg", [1, D])
    brt_d = din("brt", [1, E])
    wsT_d = din("wsT", [128, 16 * 128])
    bs_d = din("bs", [1, 16 * 128])
    b1_d = din("b1", [128, E * 32])
    b2_d = din("b2", [E, D])
    ident_d = din("ident", [128, 128])
    tri_d = din("tri", [128, 128])
    iota_d = din("iota", [128, 512])
    out = nc.dram_tensor("out", [NT, D], F32, kind="ExternalOutput").ap()
    h1scr = nc.dram_tensor("h1scr", [NT, D], F32, kind="Internal").ap()
    wscr_t = nc.dram_tensor("wscr", [36, 128, KC * 512], BF16, kind="Internal").ap()
    wscr = [wscr_t[i].rearrange("p (a b) -> p a b", a=KC) for i in range(36)]

    def wview(w2d, c0):
        return w2d.rearrange("(kc p) n -> p kc n", p=128)[:, :, c0:c0 + 512]

    with ExitStack() as top:
        def sb(name, shape, dt, stack=top):
            return stack.enter_context(nc.sbuf_tensor("s_" + name, list(shape), dt))

        pst = [top.enter_context(nc.psum_tensor("ps%d" % i, [128, 512], F32)) for i in range(8)]

        def next_ps():
            i = S.nps % 8
            S.nps += 1
            return ("ps", i), pst[i]

        ident = sb("ident", [128, 128], F32)
        ones0 = sb("ones0", [128, 128], BF16)
        wb = [sb("wb%d" % i, [128, KC, 512], BF16) for i in range(NWB)]
        st6 = sb("st6", [128, 4, 6], F32)
        mv = sb("mv", [128, 2], F32)
        rstd = sb("rstd", [128, 1], F32)

        S.op("sp", lambda e: e.dma_start(out=ident[:], in_=ident_d[:, :]), writes=["ident"], dma="c_ident")
        S.op("dve", lambda e: e.memset(ones0[:], 0.0), writes=["ones0"])
        S.op("dve", lambda e: e.memset(ones0[0:1, :], 1.0), writes=["ones0"])

        LNT = {}

        def load_ln_half(i, hf):
            lng, lnb = LNT["g"], LNT["b"]
            S.op("sp", lambda e: e.dma_start(out=lng[:], in_=lnv[2 * i:2 * i + 1, hf * 1024:(hf + 1) * 1024].partition_broadcast(128)),
                 writes=["lng"], dma="lng")
            S.op("sp", lambda e: e.dma_start(out=lnb[:], in_=lnv[2 * i + 1:2 * i + 2, hf * 1024:(hf + 1) * 1024].partition_broadcast(128)),
                 writes=["lnb"], dma="lnb")

        def ln_group(i, tiles):
            lng, lnb = LNT["g"], LNT["b"]
            for (xt, keys, out_ap, out_keys, np_) in tiles:
                ln_tm(xt, keys, out_ap, out_keys, np_=np_, affine=False)
            for hf in range(2):
                load_ln_half(i, hf)
                for (xt, keys, out_ap, out_keys, np_) in tiles:
                    kh = list(keys[2 * hf:2 * hf + 2])
                    xs = xt[:, hf * 1024:(hf + 1) * 1024]
                    os_ = out_ap[:, hf * 1024:(hf + 1) * 1024]
                    S.op("dve", lambda e, xs=xs, np_=np_: e.tensor_tensor(xs, xs, lng[0:np_, :], ALU.mult), reads=kh + ["lng"], writes=kh)
                    okh = kh if list(out_keys) == list(keys) else list(out_keys)
                    S.op("dve", lambda e, xs=xs, os_=os_, np_=np_: e.tensor_tensor(os_, xs, lnb[0:np_, :], ALU.add),
                         reads=kh + ["lnb"], writes=okh)

        def ln_tm(xt, keys, out_ap, out_keys, np_=128, affine=True):
            def f_stats(e):
                ins = None
                for c in range(4):
                    ins = e.bn_stats(st6[0:np_, c, :], xt[:, c * 512:(c + 1) * 512])
                return ins
            S.op("dve", f_stats, reads=keys, writes=["st6"])
            S.op("dve", lambda e: e.bn_aggr(mv[0:np_, :], st6[0:np_, :, :].rearrange("p a b -> p (a b)")),
                 reads=["st6"], writes=["mv"])
            S.op("dve", lambda e: e.tensor_scalar(rstd[0:np_, :], mv[0:np_, 1:2], LN_EPS, None, ALU.add),
                 reads=["mv"], writes=["rstd"])
            S.op("act", lambda e: e.activation(rstd[0:np_, :], rstd[0:np_, :], AF.Sqrt), reads=["rstd"], writes=["rstd"])
            S.op("dve", lambda e: e.reciprocal(rstd[0:np_, :], rstd[0:np_, :]), reads=["rstd"], writes=["rstd"])
            S.op("dve", lambda e: e.tensor_scalar(xt, xt, mv[0:np_, 0:1], rstd[0:np_, 0:1], ALU.subtract, ALU.mult),
                 reads=list(keys) + ["mv", "rstd"], writes=keys)
            if not affine:
                return
            lng, lnb = LNT["g"], LNT["b"]
            S.op("dve", lambda e: e.tensor_tensor(xt, xt, lng[0:np_, :], ALU.mult), reads=list(keys) + ["lng"], writes=keys)
            S.op("dve", lambda e: e.tensor_tensor(out_ap, xt, lnb[0:np_, :], ALU.add),
                 reads=list(keys) + ["lnb"], writes=out_keys)

        def transpose_tile(src, src_keys, dst, dst_key_fn, tcol, nkc=KC, np_=128):
            per = 512 // np_ if np_ < 128 else 4
            q = 0
            kc = 0
            while kc < nkc:
                n = min(per, nkc - kc)
                pk, ps = next_ps()

                def f_t(e, kc=kc, n=n, ps=ps):
                    ins = None
                    for j in range(n):
                        ins = e.transpose(ps[:, j * np_:(j + 1) * np_], src[:, (kc + j) * 128:(kc + j + 1) * 128],
                                          ident[0:np_, 0:np_])
                    return ins
                S.op("pe", f_t, reads=list(src_keys) + ["ident"], writes=[pk])
                S.op("act", lambda e, kc=kc, n=n, ps=ps: e.activation(
                    dst[:, kc:kc + n, tcol:tcol + np_],
                    ps[:, 0:n * np_].rearrange("p (a b) -> p a b", a=n), AF.Identity),
                    reads=[pk], writes=[dst_key_fn(q)])
                kc += n
                q += 1

        with ExitStack() as p1:
            xh0 = sb("xh0", [128, TT, D], F32, p1)
            h0T = sb("h0T", [128, KC, TG], BF16, p1)
            uT = sb("uT", [128, KC, TG], BF16, p1)
            vtco = sb("vtco", [128, TT * D], F32, p1)
            vt = vtco[:, :].rearrange("p (t d) -> p t d", t=TT)
            co = vtco[:, :].rearrange("p (c n) -> p c n", c=KC)
            LNT["g"] = sb("lng", [128, 1024], F32, p1)
            LNT["b"] = sb("lnb", [128, 1024], F32, p1)
            vn = sb("vn", [128, TT, D], BF16, p1)
            cT = sb("cT", [128, KC, HALO + TG], F32, p1)
            cn = sb("cn", [128, KC, TG], BF16, p1)
            gaT = sb("gaT", [128, KC, TG], BF16, p1)
            gbT = sb("gbT", [128, KC, TG], BF16, p1)
            mT = sb("mT", [128, KC, TG], BF16, p1)
            xh = vtco[0:HALO, 0:D]
            xh_keys = [("vc", i) for i in range(4)]
            h0Th = sb("h0Th", [128, KC, HALO], BF16, p1)
            bfm = sb("bfm", [128, 176], F32, p1)
            convw = sb("convw", [128, KC, CW], F32, p1)
            hmask = sb("hmask", [128, 1], F32, p1)
            bvrow = sb("bvrow", [128, D], BF16, p1)
            borow = sb("borow", [128, D], BF16, p1)
            bsrow = sb("bsrow", [128, 16, 128], BF16, p1)
            wsT = sb("wsT", [128, 16, 128], BF16, p1)
            tri = sb("tri", [128, 128], F32, p1)
            ones32 = sb("ones32", [128, 128], F32, p1)
            sgt = [sb("sgt%d" % i, [128, TG], F32, p1) for i in range(2)]
            sqt = [sb("sqt%d" % i, [128, TG], F32, p1) for i in range(2)]
            sgh = sb("sgh", [128, HALO], F32, p1)
            cmean = sb("cmean", [128, TG], F32, p1)
            cmsq = sb("cmsq", [128, TG], F32, p1)
            crstd = sb("crstd", [128, TG], F32, p1)
            cnb = sb("cnb", [128, TG], F32, p1)

            S.op("sp", lambda e: e.dma_start(out=bfm[:], in_=bfm_d[:, :]), writes=["bfm"], dma="c_bfm")
            S.op("sp", lambda e: e.dma_start(out=convw[:], in_=convw_d.rearrange("p (a b) -> p a b", a=KC)),
                 writes=["convw"], dma="c_convw")
            S.op("sp", lambda e: e.dma_start(out=hmask[:], in_=hmask_d[:, :]), writes=["hmask"], dma="c_hmask")
            S.op("pool", lambda e: e.dma_start(out=wsT[:], in_=wsT_d.rearrange("p (a b) -> p a b", a=16)),
                 writes=["wsT"], dma="c_wsT")
            S.op("sp", lambda e: e.dma_start(out=tri[:], in_=tri_d[:, :]), writes=["tri"], dma="c_tri")
            S.op("dve", lambda e: e.memset(ones32[:], 1.0), writes=["ones32"])
            for (t_, d_, nm) in ((bvrow, bv_d, "bvrow"), (borow, bo_d, "borow")):
                S.op("dve", lambda e, t_=t_: e.memset(t_[:], 0.0), writes=[nm])
                S.op("pool", lambda e, t_=t_, d_=d_: e.dma_start(out=t_[0:1, :], in_=d_[:, :]), writes=[nm], dma="c_" + nm)
            S.op("dve", lambda e: e.memset(bsrow[:], 0.0), writes=["bsrow"])
            S.op("pool", lambda e: e.dma_start(out=bsrow[0:1, :, :], in_=bs_d.rearrange("o (a b) -> o a b", a=16)),
                 writes=["bsrow"], dma="c_bsrow")
            for g16 in range(16):
                S.op("dve", lambda e, g16=g16: e.tensor_tensor(wsT[:, g16, :], wsT[:, g16, :], tri[:], ALU.mult),
                     reads=["wsT", "tri"], writes=["wsT"])

            def xkeys(tt):
                return [("xh0", tt, dg) for dg in range(4)]

            def h0T_keys():
                return [("h0T", tt, q) for tt in range(TT) for q in range(4)]

            def prologue(g):
                tok0 = g * TG
                for tt in range(TT):
                    S.op("sp", lambda e, tt=tt: e.dma_start(out=xh0[:, tt, :], in_=x[tok0 + tt * 128: tok0 + (tt + 1) * 128, :]),
                         writes=xkeys(tt), dma=("x", tt))
                tiles0 = [(xh0[:, tt, :], xkeys(tt), xh0[:, tt, :], xkeys(tt), 128) for tt in range(TT)]
                if g == 0:
                    S.op("sp", lambda e: e.dma_start(out=xh, in_=xhalo[:, :]), writes=xh_keys, dma="c_xh")
                    ln_group(0, [(xh, xh_keys, xh, xh_keys, HALO)] + tiles0)
                    transpose_tile(xh, xh_keys, h0Th, lambda q: "h0Th", 0, np_=HALO)
                else:
                    S.op("dve", lambda e: e.tensor_copy(cT[:, :, 0:HALO], cT[:, :, TG:TG + HALO]),
                         reads=[("cT", c) for c in range(KC)], writes=[("cT", c) for c in range(KC)])
                    ln_group(0, tiles0)
                for tt in range(TT):
                    transpose_tile(xh0[:, tt, :], xkeys(tt), h0T, lambda q, tt=tt: ("h0T", tt, q), tt * 128)

            def fform(wbt, wbk, rhs_fn, rhs_keys, N, evac):
                for j in range(4):
                    pk, ps = next_ps()

                    def f_mm(e, j=j, ps=ps):
                        ins = None
                        for kc in range(KC):
                            ins = e.matmul(ps[:, 0:N], lhsT=wbt[:, kc, j * 128:(j + 1) * 128], rhs=rhs_fn(kc),
                                           start=(kc == 0), stop=(kc == KC - 1))
                        return ins
                    S.op("pe", f_mm, reads=[wbk] + list(rhs_keys), writes=[pk])
                    evac(j, ps, pk)

            def tform(wbt, wbk, lhs_fn, lhs_keys_fn, ntt, bias_rhs, bias_key, evac):
                for tt in range(ntt):
                    pk, ps = next_ps()

                    def f_mm(e, tt=tt, ps=ps):
                        ins = None
                        for kc in range(KC):
                            ins = e.matmul(ps[:, :], lhsT=lhs_fn(kc, tt), rhs=wbt[:, kc, :],
                                           start=(kc == 0), stop=(kc == KC - 1 and bias_rhs is None))
                        if bias_rhs is not None:
                            ins = e.matmul(ps[:, :], lhsT=ones0[:, :], rhs=bias_rhs, start=False, stop=True)
                        return ins
                    rk = [wbk, "ones0"] + list(lhs_keys_fn(tt)) + ([bias_key] if bias_key else [])
                    S.op("pe", f_mm, reads=rk, writes=[pk])
                    evac(tt, ps, pk)

            def conv_chunks(cs):
                for c in cs:
                    S.op("dve", lambda e, c=c: e.tensor_scalar(co[:, c, :], cT[:, c, 2:2 + TG], convw[:, c, 0:1], bfm[:, 128 + c:129 + c],
                                                               ALU.mult, ALU.add),
                         reads=[("cT", c), "convw", "bfm"], writes=[("co", c), ("vc", c // 2)])
                for k in range(1, CW):
                    for c in cs:
                        S.op("dve", lambda e, k=k, c=c: e.scalar_tensor_tensor(co[:, c, :], cT[:, c, 2 + k:2 + k + TG], convw[:, c, k:k + 1],
                                                                                co[:, c, :], ALU.mult, ALU.add),
                             reads=[("cT", c), "convw", ("co", c)], writes=[("co", c)])

            def conv_ln():
                pk1, ps1 = next_ps()
                pk2, ps2 = next_ps()
                for c in range(KC):
                    sq = sqt[c % 2]
                    sk = ("sqt", c % 2)
                    S.op("act", lambda e, c=c, sq=sq: e.activation(sq[:], co[:, c, :], AF.Square), reads=[("co", c)], writes=[sk])
                    S.op("pe", lambda e, c=c: e.matmul(ps1[:, 0:TG], lhsT=ones32[:, :], rhs=co[:, c, :], start=(c == 0), stop=(c == KC - 1)),
                         reads=[("co", c), "ones32"], writes=[pk1])
                    S.op("pe", lambda e, c=c, sq=sq: e.matmul(ps2[:, 0:TG], lhsT=ones32[:, :], rhs=sq[:], start=(c == 0), stop=(c == KC - 1)),
                         reads=[sk, "ones32"], writes=[pk2])
                S.op("dve", lambda e: e.tensor_scalar(cmean[:], ps1[:, 0:TG], 1.0 / D, None, ALU.mult), reads=[pk1], writes=["cmean"])
                S.op("dve", lambda e: e.tensor_tensor(cmsq[:], cmean[:], cmean[:], ALU.mult), reads=["cmean"], writes=["cmsq"])
                S.op("dve", lambda e: e.scalar_tensor_tensor(crstd[:], ps2[:, 0:TG], 1.0 / D, cmsq[:], ALU.mult, ALU.subtract),
                     reads=[pk2, "cmsq"], writes=["crstd"])
                S.op("dve", lambda e: e.tensor_scalar(crstd[:], crstd[:], LN_EPS, None, ALU.add), reads=["crstd"], writes=["crstd"])
                S.op("act", lambda e: e.activation(crstd[:], crstd[:], AF.Sqrt), reads=["crstd"], writes=["crstd"])
                S.op("dve", lambda e: e.reciprocal(crstd[:], crstd[:]), reads=["crstd"], writes=["crstd"])
                S.op("dve", lambda e: e.scalar_tensor_tensor(cnb[:], cmean[:], -1.0, crstd[:], ALU.mult, ALU.mult),
                     reads=["cmean", "crstd"], writes=["cnb"])
                for c in range(KC):
                    S.op("dve", lambda e, c=c: e.tensor_tensor(co[:, c, :], co[:, c, :], crstd[:], ALU.mult),
                         reads=[("co", c), "crstd"], writes=[("co", c)])
                    S.op("dve", lambda e, c=c: e.tensor_tensor(co[:, c, :], co[:, c, :], cnb[:], ALU.add),
                         reads=[("co", c), "cnb"], writes=[("co", c)])
                    S.op("act", lambda e, c=c: e.activation(cn[:, c, :], co[:, c, :], AF.Silu, bias=bfm[:, 160 + c:161 + c],
                                                            scale=bfm[:, 144 + c:145 + c]),
                         reads=[("co", c), "bfm"], writes=[("cn", c), ("vc", c // 2)])

            def spatial():
                for tt in range(TT):
                    for q in range(4):
                        pk, ps = next_ps()

                        def f_mm(e, tt=tt, q=q, ps=ps):
                            ins = None
                            for j in range(4):
                                g16 = 4 * q + j
                                e.matmul(ps[:, j * 128:(j + 1) * 128], lhsT=vn[:, tt, g16 * 128:(g16 + 1) * 128], rhs=wsT[:, g16, :],
                                         start=True, stop=False)
                                ins = e.matmul(ps[:, j * 128:(j + 1) * 128], lhsT=ones0[:, :], rhs=bsrow[:, g16, :], start=False, stop=True)
                            return ins
                        S.op("pe", f_mm, reads=[("vn", tt), "wsT", "bsrow", "ones0"], writes=[pk])
                        uk = [("uT", 4 * q + j) for j in range(4)]
                        S.op("dve", lambda e, tt=tt, q=q, ps=ps: e.tensor_tensor(
                            uT[:, 4 * q:4 * q + 4, tt * 128:(tt + 1) * 128], uT[:, 4 * q:4 * q + 4, tt * 128:(tt + 1) * 128],
                            ps[:, :].rearrange("p (a b) -> p a b", a=4), ALU.mult), reads=[pk] + uk, writes=uk)

            def epilogue(g):
                tok0 = g * TG
                ln_group(2, [(xh0[:, tt, :], xkeys(tt), xh0[:, tt, :], xkeys(tt), 128) for tt in range(TT)])
                for tt in range(TT):
                    S.op("sp", lambda e, tt=tt: e.dma_start(out=h1scr[tok0 + tt * 128: tok0 + (tt + 1) * 128, :], in_=xh0[:, tt, :]),
                         reads=xkeys(tt), writes=[("h1scr", g * TT + tt)], dma=("h1st", tt))

            def make_consume(g, bi):
                def consume(wbt, wbk):
                    if bi == P1_ORDER[0]:
                        prologue(g)
                    h0rhs = lambda kc: h0T[:, kc, :]
                    if bi < 4:
                        def ev(j, ps, pk):
                            c = bi * 4 + j
                            S.op("act", lambda e: e.activation(uT[:, c, :], ps[:, 0:TG], AF.Gelu, bias=bfm[:, c:c + 1]),
                                 reads=[pk, "bfm"], writes=[("uT", c)])
                        fform(wbt, wbk, h0rhs, h0T_keys(), TG, ev)
                    elif bi < 8:
                        dg = bi - 4

                        def ev(tt, ps, pk):
                            S.op("act", lambda e: e.activation(vt[:, tt, dg * 512:(dg + 1) * 512], ps[:, :], AF.Gelu),
                                 reads=[pk], writes=[("vc", tt * 4 + dg)])
                        tform(wbt, wbk, lambda kc, tt: h0T[:, kc, tt * 128:(tt + 1) * 128],
                              lambda tt: [("h0T", tt, q) for q in range(4)], TT, bvrow[:, dg * 512:(dg + 1) * 512], "bvrow", ev)
                        if bi == 7:
                            ln_group(1, [(vt[:, tt, :], [("vc", tt * 4 + d_) for d_ in range(4)], vn[:, tt, :], [("vn", tt)], 128)
                                         for tt in range(TT)])
                    elif bi < 12:
                        def ev(j, ps, pk):
                            c = (bi - 8) * 4 + j
                            S.op("act", lambda e: e.activation(cT[:, c, HALO:HALO + TG], ps[:, 0:TG], AF.Identity, bias=bfm[:, 32 + c:33 + c]),
                                 reads=[pk, "bfm"], writes=[("cT", c)])
                            if g == 0:
                                pk2, ps2 = next_ps()

                                def f_mm(e, ps2=ps2, j=j):
                                    ins = None
                                    for kc in range(KC):
                                        ins = e.matmul(ps2[:, 0:HALO], lhsT=wbt[:, kc, j * 128:(j + 1) * 128], rhs=h0Th[:, kc, :],
                                                       start=(kc == 0), stop=(kc == KC - 1))
                                    return ins
                                S.op("pe", f_mm, reads=[wbk, "h0Th"], writes=[pk2])
                                S.op("act", lambda e, ps2=ps2: e.activation(cT[:, c, 0:HALO], ps2[:, 0:HALO], AF.Identity,
                                                                            bias=bfm[:, 32 + c:33 + c]),
                                     reads=[pk2, "bfm"], writes=[("cT", c)])
                        fform(wbt, wbk, h0rhs, h0T_keys(), TG, ev)
                    elif bi < 16:
                        def ev(j, ps, pk):
                            c = (bi - 12) * 4 + j
                            S.op("act", lambda e: e.activation(mT[:, c, :], ps[:, 0:TG], AF.Sigmoid, bias=bfm[:, 48 + c:49 + c]),
                                 reads=[pk, "bfm"], writes=[("mT", c)])
                            if g == 0:
                                pk2, ps2 = next_ps()

                                def f_mm(e, ps2=ps2, j=j):
                                    ins = None
                                    for kc in range(KC):
                                        ins = e.matmul(ps2[:, 0:HALO], lhsT=wbt[:, kc, j * 128:(j + 1) * 128], rhs=h0Th[:, kc, :],
                                                       start=(kc == 0), stop=(kc == KC - 1))
                                    return ins
                                S.op("pe", f_mm, reads=[wbk, "h0Th"], writes=[pk2])
                                S.op("act", lambda e, ps2=ps2: e.activation(sgh[:], ps2[:, 0:HALO], AF.Sigmoid, bias=bfm[:, 48 + c:49 + c]),
                                     reads=[pk2, "bfm"], writes=["sgh"])
                                S.op("dve", lambda e: e.scalar_tensor_tensor(cT[:, c, 0:HALO], cT[:, c, 0:HALO], hmask[:, 0:1], sgh[:],
                                                                             ALU.mult, ALU.mult),
                                     reads=["sgh", "hmask", ("cT", c)], writes=[("cT", c)])
                        fform(wbt, wbk, h0rhs, h0T_keys(), TG, ev)
                        cs = [(bi - 12) * 4 + j for j in range(4)]
                        for c in cs:
                            S.op("dve", lambda e, c=c: e.tensor_tensor(cT[:, c, HALO:HALO + TG], cT[:, c, HALO:HALO + TG], mT[:, c, :], ALU.mult),
                                 reads=[("mT", c), ("cT", c)], writes=[("cT", c)])
                        conv_chunks(cs)
                    elif bi < 24:
                        def ev(j, ps, pk):
                            c = ((bi - 16) % 4) * 4 + j
                            dst, nm, off = (gaT, "gaT", 64) if bi < 20 else (gbT, "gbT", 80)
                            S.op("act", lambda e: e.activation(dst[:, c, :], ps[:, 0:TG], AF.Sigmoid, bias=bfm[:, off + c:off + c + 1]),
                                 reads=[pk, "bfm"], writes=[(nm, c)])
                        fform(wbt, wbk, h0rhs, h0T_keys(), TG, ev)
                    elif bi < 28:
                        if bi == 24:
                            spatial()
                        def ev(j, ps, pk):
                            c = (bi - 24) * 4 + j
                            S.op("dve", lambda e: e.scalar_tensor_tensor(mT[:, c, :], ps[:, 0:TG], bfm[:, 96 + c:97 + c], gaT[:, c, :],
                                                                         ALU.add, ALU.mult),
                                 reads=[pk, "bfm", ("gaT", c)], writes=[("mT", c)])
                        fform(wbt, wbk, lambda kc: uT[:, kc, :], [("uT", c) for c in range(KC)], TG, ev)
                    elif bi < 32:
                        if bi == 28:
                            conv_ln()
                        def ev(j, ps, pk):
                            c = (bi - 28) * 4 + j
                            sg = sgt[c % 2]
                            sk = ("sgt", c % 2)
                            S.op("dve", lambda e: e.scalar_tensor_tensor(sg[:], ps[:, 0:TG], bfm[:, 112 + c:113 + c], gbT[:, c, :],
                                                                         ALU.add, ALU.mult),
                                 reads=[pk, "bfm", ("gbT", c)], writes=[sk])
                            S.op("dve", lambda e: e.tensor_tensor(mT[:, c, :], mT[:, c, :], sg[:], ALU.add),
                                 reads=[sk, ("mT", c)], writes=[("mT", c)])
                        fform(wbt, wbk, lambda kc: cn[:, kc, :], [("cn", c) for c in range(KC)], TG, ev)
                    else:
                        dg = bi - 32

                        def ev(tt, ps, pk):
                            S.op("dve", lambda e: e.scalar_tensor_tensor(xh0[:, tt, dg * 512:(dg + 1) * 512], xh0[:, tt, dg * 512:(dg + 1) * 512],
                                                                         DN_ALPHA, ps[:, :], ALU.mult, ALU.add),
                                 reads=[pk, ("xh0", tt, dg)], writes=[("xh0", tt, dg)])
                        tform(wbt, wbk, lambda kc, tt: mT[:, kc, tt * 128:(tt + 1) * 128],
                              lambda tt: [("mT", c) for c in range(KC)], TT, borow[:, dg * 512:(dg + 1) * 512], "borow", ev)
                        if bi == 35:
                            epilogue(g)
                return consume

            blocks = []
            for g in range(NG):
                for bi in P1_ORDER:
                    if bi < 24:
                        src = wview(w_in, bi * 512)
                    elif bi < 28:
                        src = wview(w_sgu, (bi - 24) * 512)
                    elif bi < 32:
                        src = wview(w_cv, (bi - 28) * 512)
                    else:
                        src = wview(w_o, (bi - 32) * 512)
                    if NG > 1 and g == 0:
                        blocks.append((src, make_consume(g, bi), (wscr[bi], ("wscr", bi))))
                    elif NG > 1:
                        blocks.append(((wscr[bi], ("wscr", bi)), make_consume(g, bi)))
                    else:
                        blocks.append((src, make_consume(g, bi)))

            def run_stream(blocks):
                def issue(i):
                    src = blocks[i][0]
                    b = i % NWB
                    rk = []
                    if isinstance(src, tuple):
                        src, k_ = src
                        rk = [k_]
                    S.op("pool", lambda e: e.dma_start(out=wb[b][:], in_=src), reads=rk, writes=[("wb", b)], dma=("wb", b))
                    if len(blocks[i]) > 2:
                        dst, dk = blocks[i][2]
                        S.op("sp", lambda e: e.dma_start(out=dst, in_=wb[b][:]), reads=[("wb", b)], writes=[dk], dma=("wst", b))
                for i in range(min(NWB, len(blocks))):
                    issue(i)
                for i in range(len(blocks)):
                    blocks[i][1](wb[i % NWB], ("wb", i % NWB))
                    if i + NWB < len(blocks):
                        issue(i + NWB)

            run_stream(blocks)
            S.barrier()

        NTT = HALF // 128
        NST = CAP // 128
        uniq = [0]

        def sbn(name, shape, dt, stack):
            uniq[0] += 1
            return stack.enter_context(nc.sbuf_tensor("s_%s_%d" % (name, uniq[0]), list(shape), dt))

        with ExitStack() as p2:
            acc = sb("acc", [128, NTT, D], F32, p2)
            Gt = sb("G", [128, NTT, E], F32, p2)
            Mf = sb("Mf", [128, NTT, E], F32, p2)
            Mbf = sb("Mbf", [128, NTT, E], BF16, p2)
            rank = sb("rank", [128, NTT, E], F32, p2)
            iota = sb("iota", [128, CAP], F32, p2)
            identbf = sb("identbf", [128, 128], BF16, p2)
            allones = sb("allones", [128, 128], BF16, p2)
            striu = sb("striu", [128, 128], BF16, p2)
            trif = sb("trif", [128, 128], F32, p2)
            lg = sb("lg", [128, E], F32, p2)
            ex = sb("ex", [128, E], F32, p2)
            m8 = sb("m8", [128, 8], F32, p2)
            nmx = sb("nmx", [128, 1], F32, p2)
            ssum = sb("ssum", [128, 1], F32, p2)
            b1t = sb("b1t", [128, E, 32], F32, p2)

            S.op("sp", lambda e: e.dma_start(out=iota[:], in_=iota_d[:, 0:CAP]), writes=["iota"], dma="c_iota")
            S.op("sp", lambda e: e.dma_start(out=trif[:], in_=tri_d[:, :]), writes=["trif"], dma="c_trif")
            S.op("sp", lambda e: e.dma_start(out=b1t[:], in_=b1_d.rearrange("p (a b) -> p a b", a=E)), writes=["b1t"], dma="c_b1t")
            S.op("dve", lambda e: e.tensor_copy(identbf[:], ident[:]), reads=["ident"], writes=["identbf"])
            S.op("dve", lambda e: e.memset(allones[:], 1.0), writes=["allones"])
            S.op("dve", lambda e: e.tensor_tensor(striu[:], trif[:], ident[:], ALU.subtract), reads=["trif", "ident"], writes=["striu"])

            def akeys(tt):
                return [("acc", tt, dg) for dg in range(4)]

            PRE = {}
            EXP = {}

            def open_pre():
                st = ExitStack()
                PRE.clear()
                PRE["stack"] = st
                PRE["h1T"] = sbn("h1T", [128, KC, HALF], BF16, st)
                PRE["pt"] = sbn("pt", [128, PLE], F32, st)
                PRE["pT"] = sbn("pT", [128, 2, HALF], BF16, st)
                PRE["wple"] = sbn("wple", [128, 2, D], BF16, st)
                PRE["wr"] = sbn("wr", [128, KC, E], BF16, st)
                PRE["brrow"] = sbn("brrow", [128, E], BF16, st)
                PRE["bpgrow"] = sbn("bpgrow", [128, D], BF16, st)
                PRE["b2bf"] = sbn("b2bf", [128, D], BF16, st)
                PRE["GT"] = sbn("GT", [128, HALF], BF16, st)
                PRE["tmp"] = [sbn("ptmp", [128, 512], F32, st) for _ in range(2)]
                wple, wr, brrow, bpgrow, b2bf, GT = (PRE[k] for k in ("wple", "wr", "brrow", "bpgrow", "b2bf", "GT"))
                S.op("pool", lambda e: e.dma_start(out=wple[:], in_=w_ple.rearrange("(kc p) n -> p kc n", p=128)), writes=["wple"], dma="c_wple")
                S.op("pool", lambda e: e.dma_start(out=wr[:], in_=w_rt.rearrange("(kc p) n -> p kc n", p=128)), writes=["wr"], dma="c_wr")
                S.op("dve", lambda e: e.memset(brrow[:], 0.0), writes=["brrow"])
                S.op("pool", lambda e: e.dma_start(out=brrow[0:1, :], in_=brt_d[:, :]), writes=["brrow"], dma="c_brrow")
                S.op("dve", lambda e: e.memset(bpgrow[:], 0.0), writes=["bpgrow"])
                S.op("pool", lambda e: e.dma_start(out=bpgrow[0:1, :], in_=bpg_d[:, :]), writes=["bpgrow"], dma="c_bpgrow")
                S.op("dve", lambda e: e.memset(b2bf[:], 0.0), writes=["b2bf"])
                S.op("pool", lambda e: e.dma_start(out=b2bf[0:E, :], in_=b2_d[:, :]), writes=["b2bf"], dma="c_b2bf")
                S.op("dve", lambda e: e.memset(GT[:], 0.0), writes=["GT"])

            def close_pre():
                S.barrier()
                PRE["stack"].close()

            def open_exp(hh):
                st = ExitStack()
                EXP.clear()
                EXP["stack"] = st
                h1tm = sbn("h1tm", [128, NTT, D], BF16, st)
                EXP["h1tm"] = h1tm
                for tt in range(NTT):
                    r0 = hh * HALF + tt * 128
                    S.op("pool", lambda e, tt=tt, r0=r0: e.dma_start(out=h1tm[:, tt, :], in_=h1scr[r0:r0 + 128, :]),
                         reads=[("h1scr", hh * NTT + tt)], writes=[("h1tm", tt)], dma=("h1tm", tt % 2))
                EXP["xgT"] = sbn("xgT", [128, KC, CAP], BF16, st)
                EXP["actT"] = sbn("actT", [128, KC, CAP], BF16, st)
                EXP["Pe"] = sbn("Pe", [128, NTT, CAP], BF16, st)
                EXP["Pw"] = sbn("Pw", [128, NTT, CAP], BF16, st)
                EXP["PTw"] = [sbn("PTw", [128, NST * NTT, 128], BF16, st) for _ in range(2)]
                EXP["ysb"] = [sbn("ysb", [128, NST, 512], BF16, st) for _ in range(2)]
                EXP["tA"] = [sbn("tA", [128, CAP], F32, st) for _ in range(2)]
                EXP["tB"] = [sbn("tB", [128, CAP], F32, st) for _ in range(2)]
                EXP["lng2"] = sbn("lng2", [128, 512], F32, st)
                EXP["lnb2"] = sbn("lnb2", [128, 512], F32, st)
                EXP["rot"] = {"A": 0, "B": 0, "Y": 0}

            def close_exp():
                S.barrier()
                EXP["stack"].close()

            def half_prologue(hh):
                t0 = hh * HALF
                open_pre()
                h1T, pt, pT, wr, brrow, b2bf, GT = (PRE[k] for k in ("h1T", "pt", "pT", "wr", "brrow", "b2bf", "GT"))
                for tt in range(NTT):
                    S.op("sp", lambda e, tt=tt: e.dma_start(out=acc[:, tt, :], in_=h1scr[t0 + tt * 128:t0 + (tt + 1) * 128, :]),
                         reads=[("h1scr", hh * NTT + tt)], writes=akeys(tt), dma=("h1ld", tt % 2))
                for tt in range(NTT):
                    transpose_tile(acc[:, tt, :], akeys(tt), h1T, lambda q, tt=tt: ("h1T", tt), tt * 128)
                    S.op("sp", lambda e, tt=tt: e.dma_start(out=pt[:], in_=p_d[t0 + tt * 128:t0 + (tt + 1) * 128, :]),
                         writes=["pt"], dma="pt")
                    transpose_tile(pt, ["pt"], pT, lambda q, tt=tt: ("pT", tt), tt * 128, nkc=2)
                    pk, ps = next_ps()

                    def f_mm(e, tt=tt, ps=ps):
                        for kc in range(KC):
                            e.matmul(ps[:, 0:E], lhsT=h1T[:, kc, tt * 128:(tt + 1) * 128], rhs=wr[:, kc, :], start=(kc == 0), stop=False)
                        return e.matmul(ps[:, 0:E], lhsT=ones0[:, :], rhs=brrow[:, :], start=False, stop=True)
                    S.op("pe", f_mm, reads=[("h1T", tt), "wr", "brrow", "ones0"], writes=[pk])
                    S.op("dve", lambda e, ps=ps: e.tensor_copy(lg[:], ps[:, 0:E]), reads=[pk], writes=["lg"])
                    S.op("dve", lambda e: e.max(m8[:], lg[:]), reads=["lg"], writes=["m8"])
                    S.op("dve", lambda e, tt=tt: e.tensor_scalar(Mf[:, tt, :], lg[:], m8[:, 3:4], None, ALU.is_ge), reads=["lg", "m8"], writes=[("Mf", tt)])
                    S.op("dve", lambda e, tt=tt: e.tensor_copy(Mbf[:, tt, :], Mf[:, tt, :]), reads=[("Mf", tt)], writes=[("Mbf", tt)])
                    S.op("dve", lambda e: e.tensor_scalar(nmx[:], m8[:, 0:1], -1.0, None, ALU.mult), reads=["m8"], writes=["nmx"])
                    S.op("act", lambda e: e.activation(ex[:], lg[:], AF.Exp, bias=nmx[:, 0:1]), reads=["lg", "nmx"], writes=["ex"])
                    S.op("dve", lambda e, tt=tt: e.tensor_tensor(ex[:], ex[:], Mf[:, tt, :], ALU.mult), reads=["ex", ("Mf", tt)], writes=["ex"])
                    S.op("dve", lambda e: e.reduce_sum(ssum[:], ex[:], AX.X), reads=["ex"], writes=["ssum"])
                    S.op("dve", lambda e: e.reciprocal(ssum[:], ssum[:]), reads=["ssum"], writes=["ssum"])
                    S.op("dve", lambda e, tt=tt: e.tensor_scalar(Gt[:, tt, :], ex[:], ssum[:, 0:1], None, ALU.mult),
                         reads=["ex", "ssum"], writes=[("G", tt)])
                    pkr, psr = next_ps()

                    def f_rank(e, tt=tt, psr=psr):
                        ins = None
                        for t2 in range(tt + 1):
                            ins = e.matmul(psr[:, 0:E], lhsT=(allones[:, :] if t2 < tt else striu[:, :]), rhs=Mbf[:, t2, :],
                                           start=(t2 == 0), stop=(t2 == tt))
                        return ins
                    S.op("pe", f_rank, reads=[("Mbf", t2) for t2 in range(tt + 1)] + ["allones", "striu"], writes=[pkr])
                    S.op("dve", lambda e, tt=tt, psr=psr: e.tensor_copy(rank[:, tt, :], psr[:, 0:E]), reads=[pkr], writes=[("rank", tt)])
                    pk2, ps2 = next_ps()
                    S.op("pe", lambda e, tt=tt, ps2=ps2: e.transpose(ps2[0:E, 0:128], Gt[:, tt, :], ident[:, :]),
                         reads=[("G", tt), "ident"], writes=[pk2])
                    S.op("act", lambda e, tt=tt, ps2=ps2: e.activation(GT[0:E, tt * 128:(tt + 1) * 128], ps2[0:E, 0:128], AF.Identity),
                         reads=[pk2], writes=[("GT", tt)])
                    S.op("dve", lambda e, tt=tt: e.tensor_scalar(acc[:, tt, :], acc[:, tt, :], DN_ALPHA, None, ALU.mult),
                         reads=akeys(tt), writes=akeys(tt))
                    for dg in range(4):
                        pk3, ps3 = next_ps()
                        S.op("pe", lambda e, tt=tt, dg=dg, ps3=ps3: e.matmul(ps3[:, :], lhsT=GT[:, tt * 128:(tt + 1) * 128],
                                                                             rhs=b2bf[:, dg * 512:(dg + 1) * 512], start=True, stop=True),
                             reads=[("GT", tt), "GT", "b2bf"], writes=[pk3])
                        S.op("dve", lambda e, tt=tt, dg=dg, ps3=ps3: e.tensor_tensor(acc[:, tt, dg * 512:(dg + 1) * 512],
                                                                                    acc[:, tt, dg * 512:(dg + 1) * 512], ps3[:, :], ALU.add),
                             reads=[pk3, ("acc", tt, dg)], writes=[("acc", tt, dg)])

            def make_ple(hh, dg):
                def consume(wbt, wbk):
                    if dg == 0:
                        half_prologue(hh)
                    h1T, pT, wple, bpgrow = (PRE[k] for k in ("h1T", "pT", "wple", "bpgrow"))
                    tmps = PRE["tmp"]

                    def ev(tt, ps, pk):
                        sg = tmps[tt % 2]
                        sk = ("ptmp", tt % 2)
                        S.op("act", lambda e: e.activation(sg[:], ps[:, :], AF.Sigmoid), reads=[pk], writes=[sk])
                        pk2, ps2 = next_ps()

                        def f_mm(e):
                            e.matmul(ps2[:, :], lhsT=pT[:, 0, tt * 128:(tt + 1) * 128], rhs=wple[:, 0, dg * 512:(dg + 1) * 512], start=True, stop=False)
                            return e.matmul(ps2[:, :], lhsT=pT[:, 1, tt * 128:(tt + 1) * 128], rhs=wple[:, 1, dg * 512:(dg + 1) * 512],
                                            start=False, stop=True)
                        S.op("pe", f_mm, reads=[("pT", tt), "wple"], writes=[pk2])
                        S.op("dve", lambda e: e.tensor_tensor(sg[:], sg[:], ps2[:, :], ALU.mult), reads=[sk, pk2], writes=[sk])
                        S.op("dve", lambda e: e.tensor_tensor(acc[:, tt, dg * 512:(dg + 1) * 512], acc[:, tt, dg * 512:(dg + 1) * 512], sg[:], ALU.add),
                             reads=[sk, ("acc", tt, dg)], writes=[("acc", tt, dg)])
                    tform(wbt, wbk, lambda kc, tt: h1T[:, kc, tt * 128:(tt + 1) * 128], lambda tt: [("h1T", tt)], NTT,
                          bpgrow[:, dg * 512:(dg + 1) * 512], "bpgrow", ev)
                    if dg == 3:
                        close_pre()
                        open_exp(hh)
                return consume

            def etmp(kind):
                i = EXP["rot"][kind] % 2
                EXP["rot"][kind] += 1
                return {"A": EXP["tA"], "B": EXP["tB"], "Y": EXP["ysb"]}[kind][i], ("e" + kind, i)

            def expert_prologue(ex_):
                xgT, Pe, Pw, h1tm = EXP["xgT"], EXP["Pe"], EXP["Pw"], EXP["h1tm"]
                pb = ex_ % 2
                PTw = EXP["PTw"][pb]
                for tt in range(NTT):
                    S.op("dve", lambda e, tt=tt: e.tensor_scalar(Pe[:, tt, :], iota[:], rank[:, tt, ex_:ex_ + 1], Mf[:, tt, ex_:ex_ + 1],
                                                                 ALU.is_equal, ALU.mult),
                         reads=["iota", ("rank", tt), ("Mf", tt)], writes=[("Pe", tt)])
                    S.op("dve", lambda e, tt=tt: e.tensor_scalar(Pw[:, tt, :], iota[:], rank[:, tt, ex_:ex_ + 1], Gt[:, tt, ex_:ex_ + 1],
                                                                 ALU.is_equal, ALU.mult),
                         reads=["iota", ("rank", tt), ("G", tt)], writes=[("Pw", tt)])
                npair = 512 // CAP
                for q in range(KC // npair):
                    pk, ps = next_ps()

                    def f_mm(e, q=q, ps=ps):
                        ins = None
                        for j in range(npair):
                            dc = q * npair + j
                            for tt in range(NTT):
                                ins = e.matmul(ps[:, j * CAP:(j + 1) * CAP], lhsT=h1tm[:, tt, dc * 128:(dc + 1) * 128], rhs=Pe[:, tt, :],
                                               start=(tt == 0), stop=(tt == NTT - 1))
                        return ins
                    S.op("pe", f_mm, reads=[("Pe", tt) for tt in range(NTT)] + [("h1tm", tt) for tt in range(NTT)], writes=[pk])
                    S.op("act", lambda e, q=q, ps=ps: e.activation(xgT[:, q * npair:(q + 1) * npair, :],
                                                                   ps[:, 0:npair * CAP].rearrange("p (a b) -> p a b", a=npair), AF.Identity),
                         reads=[pk], writes=[("xgT", q)])
                nblk = NST * NTT
                for q in range((nblk + 3) // 4):
                    pk, ps = next_ps()
                    n = min(4, nblk - q * 4)

                    def f_mm(e, q=q, ps=ps, n=n):
                        ins = None
                        for j in range(n):
                            blk = q * 4 + j
                            st_, tt = blk // NTT, blk % NTT
                            ins = e.matmul(ps[:, j * 128:(j + 1) * 128], lhsT=Pw[:, tt, st_ * 128:(st_ + 1) * 128], rhs=identbf[:, :],
                                           start=True, stop=True)
                        return ins
                    S.op("pe", f_mm, reads=[("Pw", tt) for tt in range(NTT)] + ["identbf"], writes=[pk])
                    S.op("act", lambda e, q=q, ps=ps, n=n: e.activation(PTw[:, q * 4:q * 4 + n, :],
                                                                        ps[:, 0:n * 128].rearrange("p (a b) -> p a b", a=n), AF.Identity),
                         reads=[pk], writes=[("PTw", pb)])

            def make_w1(hh, ex_, bi):
                def consume(wbt, wbk):
                    if bi == 0:
                        expert_prologue(ex_)
                    xgT, actT = EXP["xgT"], EXP["actT"]
                    for j in range(4):
                        fc = (bi % 4) * 4 + j
                        pk, ps = next_ps()

                        def f_mm(e, j=j, ps=ps):
                            ins = None
                            for kc in range(KC):
                                ins = e.matmul(ps[:, 0:CAP], lhsT=wbt[:, kc, j * 128:(j + 1) * 128], rhs=xgT[:, kc, :],
                                               start=(kc == 0), stop=(kc == KC - 1))
                            return ins
                        S.op("pe", f_mm, reads=[wbk] + [("xgT", q) for q in range(KC * CAP // 512)], writes=[pk])
                        dst = actT[:, fc, :]
                        ak = ("actT", fc)
                        if bi < 4:
                            gc, gk = etmp("A")
                            sg, sk = etmp("B")
                            S.op("dve", lambda e, ps=ps, gc=gc, fc=fc: e.tensor_scalar(gc[:], ps[:, 0:CAP], b1t[:, ex_, fc:fc + 1], SW_LIM,
                                                                                       ALU.add, ALU.min),
                                 reads=[pk, "b1t"], writes=[gk])
                            S.op("act", lambda e, gc=gc, sg=sg: e.activation(sg[:], gc[:], AF.Sigmoid, scale=SW_ALPHA),
                                 reads=[gk], writes=[sk])
                            S.op("dve", lambda e, gc=gc, sg=sg, dst=dst: e.tensor_tensor(dst, gc[:], sg[:], ALU.mult),
                                 reads=[gk, sk], writes=[ak])
                        else:
                            ub, uk = etmp("A")
                            S.op("act", lambda e, ps=ps, ub=ub, fc=fc: e.activation(ub[:], ps[:, 0:CAP], AF.Identity,
                                                                                    bias=b1t[:, ex_, 16 + fc:17 + fc]),
                                 reads=[pk, "b1t"], writes=[uk])
                            S.op("dve", lambda e, ub=ub: e.tensor_scalar(ub[:], ub[:], SW_LIM, -SW_LIM, ALU.min, ALU.max),
                                 reads=[uk], writes=[uk])
                            S.op("dve", lambda e, ub=ub, dst=dst: e.scalar_tensor_tensor(dst, ub[:], 1.0, dst, ALU.add, ALU.mult),
                                 reads=[uk, ak], writes=[ak])
                return consume

            def make_w2(hh, ex_, dg):
                def consume(wbt, wbk):
                    actT = EXP["actT"]
                    pb = ex_ % 2
                    PTw = EXP["PTw"][pb]
                    ysb, yk = etmp("Y")
                    for st_ in range(NST):
                        pk, ps = next_ps()

                        def f_mm(e, st_=st_, ps=ps):
                            ins = None
                            for fc in range(KC):
                                ins = e.matmul(ps[:, :], lhsT=actT[:, fc, st_ * 128:(st_ + 1) * 128], rhs=wbt[:, fc, :],
                                               start=(fc == 0), stop=(fc == KC - 1))
                            return ins
                        S.op("pe", f_mm, reads=[wbk] + [("actT", fc) for fc in range(KC)], writes=[pk])
                        S.op("act", lambda e, st_=st_, ps=ps: e.activation(ysb[:, st_, :], ps[:, :], AF.Identity), reads=[pk], writes=[yk])
                    for tt in range(NTT):
                        pk, ps = next_ps()

                        def f_sc(e, tt=tt, ps=ps):
                            ins = None
                            for st_ in range(NST):
                                ins = e.matmul(ps[:, :], lhsT=PTw[:, st_ * NTT + tt, :], rhs=ysb[:, st_, :], start=(st_ == 0), stop=(st_ == NST - 1))
                            return ins
                        S.op("pe", f_sc, reads=[yk, ("PTw", pb)], writes=[pk])
                        S.op("dve", lambda e, tt=tt, ps=ps: e.tensor_tensor(acc[:, tt, dg * 512:(dg + 1) * 512], acc[:, tt, dg * 512:(dg + 1) * 512],
                                                                            ps[:, :], ALU.add),
                             reads=[pk, ("acc", tt, dg)], writes=[("acc", tt, dg)])
                    if ex_ == E - 1 and dg == 3:
                        half_epilogue(hh)
                return consume

            def half_epilogue(hh):
                t0 = hh * HALF
                lng2, lnb2 = EXP["lng2"], EXP["lnb2"]
                for tt in range(NTT):
                    ln_tm(acc[:, tt, :], akeys(tt), acc[:, tt, :], akeys(tt), affine=False)
                for dg in range(4):
                    S.op("sp", lambda e, dg=dg: e.dma_start(out=lng2[:], in_=lnv[6:7, dg * 512:(dg + 1) * 512].partition_broadcast(128)),
                         writes=["lng2"], dma="lng2")
                    S.op("sp", lambda e, dg=dg: e.dma_start(out=lnb2[:], in_=lnv[7:8, dg * 512:(dg + 1) * 512].partition_broadcast(128)),
                         writes=["lnb2"], dma="lnb2")
                    for tt in range(NTT):
                        a_ = acc[:, tt, dg * 512:(dg + 1) * 512]
                        S.op("dve", lambda e, a_=a_: e.tensor_tensor(a_, a_, lng2[:], ALU.mult), reads=[("acc", tt, dg), "lng2"], writes=[("acc", tt, dg)])
                        S.op("dve", lambda e, a_=a_: e.tensor_tensor(a_, a_, lnb2[:], ALU.add), reads=[("acc", tt, dg), "lnb2"], writes=[("acc", tt, dg)])
                for tt in range(NTT):
                    S.op("sp", lambda e, tt=tt: e.dma_start(out=out[t0 + tt * 128:t0 + (tt + 1) * 128, :], in_=acc[:, tt, :]),
                         reads=akeys(tt), writes=[("out", hh * NTT + tt)], dma=("ost", tt % 2))
                close_exp()

            blocks2 = []
            for hh in range(NT // HALF):
                for dg in range(4):
                    blocks2.append((wview(w_pg, dg * 512), make_ple(hh, dg)))
                for ex_ in range(E):
                    for bi in range(8):
                        blocks2.append((wview(w_e1[ex_], bi * 512), make_w1(hh, ex_, bi)))
                    for dg in range(4):
                        blocks2.append((wview(w_e2[ex_], dg * 512), make_w2(hh, ex_, dg)))
            run_stream(blocks2)
            S.op("sp", None, reads=[("out", i) for i in range(NT // 128)])

            S.finalize(top)
            with nc.Block() as block:
                S.emit(block)
    return nc


def _core_inputs(c, NT, E, inp, n_per_batch):
    b = c // n_per_batch
    s0 = (c % n_per_batch) * NT
    f = lambda a: np.ascontiguousarray(a, dtype=np.float32)
    x = inp["x"]
    m = {}
    m["x"] = f(x[b, s0:s0 + NT])
    if s0 == 0:
        m["xhalo"] = np.zeros((HALO, D), np.float32)
        m["hmask"] = np.zeros((128, 1), np.float32)
    else:
        m["xhalo"] = f(x[b, s0 - HALO:s0])
        m["hmask"] = np.ones((128, 1), np.float32)
    m["p"] = f(inp["p"][0, b, s0:s0 + NT])
    m["w_in"] = f(inp["w_in"][0])
    m["w_sgu_out"] = f(inp["w_sgu_out"][0])
    m["w_conv_out"] = f(inp["w_conv_out"][0])
    m["w_o"] = f(inp["w_o"][0])
    m["w_pg"] = f(inp["w_pg"][0])
    m["w_ple"] = f(inp["w_ple"][0])
    m["w_router"] = f(inp["w_router"][0])
    m["w_e1"] = f(inp["w_e1"][0])
    m["w_e2"] = f(inp["w_e2"][0])
    m["lnv"] = f(np.stack([inp["ln0_g"], inp["ln0_b"], inp["sgu_ln_g"][0], inp["sgu_ln_b"][0],
                           inp["ln1_g"][0], inp["ln1_b"][0], inp["ln2_g"][0], inp["ln2_b"][0]], 0))
    fm = lambda v: np.asarray(v).reshape(-1, 128).T
    m["bfm"] = f(np.concatenate([fm(inp["b_in"][0]), fm(inp["b_sgu_out"][0]), fm(inp["b_conv_out"][0]), fm(inp["conv_b"][0]),
                                 fm(inp["conv_ln_g"][0]), fm(inp["conv_ln_b"][0])], axis=1))
    m["convw"] = f(np.asarray(inp["conv_w"][0]).T.reshape(KC, 128, CW).transpose(1, 0, 2).reshape(128, KC * CW))
    m["bv"] = f(np.asarray(inp["b_in"][0])[None, D:2 * D])
    m["bo"] = f(np.asarray(inp["b_o"][0])[None])
    m["bpg"] = f(np.asarray(inp["b_pg"][0])[None])
    m["brt"] = f(np.asarray(inp["b_router"][0])[None])
    m["wsT"] = f(np.asarray(inp["w_s"][0]).transpose(2, 0, 1).reshape(128, 16 * 128))
    m["bs"] = f(np.asarray(inp["b_s"][0]).reshape(1, 16 * 128))
    m["b1"] = f(np.asarray(inp["b_e1"][0]).reshape(E, 32, 128).transpose(2, 0, 1).reshape(128, E * 32))
    m["b2"] = f(inp["b_e2"][0])
    m["ident"] = np.eye(128, dtype=np.float32)
    m["tri"] = np.triu(np.ones((128, 128), np.float32))
    m["iota"] = np.ascontiguousarray(np.broadcast_to(np.arange(512, dtype=np.float32), (128, 512)))
    return m


_NC_CACHE = {}


def kernel(**inputs):
    inp = {k: np.asarray(v) for k, v in inputs.items()}
    B, SEQ, _ = inp["x"].shape
    E = inp["w_router"].shape[-1]
    n_cores = 8
    NT = B * SEQ // n_cores
    n_per_batch = SEQ // NT
    key = (NT, E)
    if key not in _NC_CACHE:
        _NC_CACHE[key] = build_program(NT=NT, E=E, HALF=min(1024, NT))
    nc = _NC_CACHE[key]
    in_maps = [_core_inputs(c, NT, E, inp, n_per_batch) for c in range(n_cores)]
    res = run_bass_kernel_spmd(nc, in_maps, core_ids=list(range(n_cores)))
    outs = [np.asarray(r["out"]) for r in res.results]
    return np.stack(outs, 0).reshape(B, SEQ, D).astype(np.float32)
```

```python
import numpy as np
from contextlib import ExitStack
import concourse.bass as bass
import concourse.mybir as mybir
from concourse.bass_utils import run_bass_kernel_spmd

F32 = mybir.dt.float32
BF16 = mybir.dt.bfloat16
ALU = mybir.AluOpType
AF = mybir.ActivationFunctionType
AX = mybir.AxisListType

D = 2048
KC = 16
DIN = 12288
PLE = 256
CW = 31
HALO = 32
TG = 256
LN_EPS = 1e-5
DN_ALPHA = 2.0 ** 0.25
SW_ALPHA = 1.702
SW_LIM = 7.0
NWB = 3
P1_ORDER = list(range(4, 16)) + list(range(0, 4)) + list(range(16, 36))
CAP = 256


class _Op:
    __slots__ = ("eng", "fn", "deps", "needs_inc", "dma", "sem", "val", "inc")

    def __init__(self, eng, fn, dma):
        self.eng, self.fn, self.dma = eng, fn, dma
        self.deps = []
        self.needs_inc = False
        self.sem = None
        self.val = 0
        self.inc = 1


class Sched:
    ENGS = ("pe", "act", "dve", "pool", "sp")

    def __init__(self, nc):
        self.nc = nc
        self.ops = {e: [] for e in self.ENGS}
        self.res = {}
        self.last_dma = {}
        self.nps = 0

    def op(self, eng, fn, reads=(), writes=(), dma=None):
        o = _Op(eng, fn, dma)
        seen = set()

        def add(d):
            if d is None or id(d) in seen:
                return
            if d.eng == "pe" and eng == "pe" and d.dma is None and dma is None:
                return
            seen.add(id(d))
            o.deps.append(d)

        for k in reads:
            st = self.res.setdefault(k, [None, []])
            add(st[0])
        for k in writes:
            st = self.res.setdefault(k, [None, []])
            add(st[0])
            for r in st[1]:
                add(r)
        if dma is not None:
            add(self.last_dma.get(dma))
            self.last_dma[dma] = o
        for k in reads:
            self.res[k][1].append(o)
        for k in writes:
            self.res[k][0] = o
            self.res[k][1] = []
        self.ops[eng].append(o)
        return o

    def barrier(self):
        keys = list(self.res.keys())
        for e in self.ENGS:
            self.op(e, None, reads=keys)
        for e in self.ENGS:
            self.op(e, None, writes=keys)

    def finalize(self, stack):
        nc = self.nc
        def expand(o, out, seen):
            for d in o.deps:
                if id(d) in seen:
                    continue
                seen.add(id(d))
                if d.fn is None:
                    expand(d, out, seen)
                else:
                    out.append(d)
        for e in self.ENGS:
            for o in self.ops[e]:
                out = []
                expand(o, out, set())
                o.deps = out
        for e in self.ENGS:
            for o in self.ops[e]:
                for d in o.deps:
                    d.needs_inc = True
        esem = {}
        for e in self.ENGS:
            esem[e] = stack.enter_context(nc.semaphore("sem_" + e))
        dsem = {}
        dcnt = {}
        for e in self.ENGS:
            cnt = 0
            for o in self.ops[e]:
                if o.fn is None:
                    continue
                if o.dma is not None:
                    if o.dma not in dsem:
                        dsem[o.dma] = stack.enter_context(nc.semaphore("dsem%d" % len(dsem)))
                        dcnt[o.dma] = 0
                    dcnt[o.dma] += 16
                    o.sem, o.val, o.inc = dsem[o.dma], dcnt[o.dma], 16
                    o.needs_inc = True
                elif o.needs_inc:
                    cnt += 1
                    o.sem, o.val, o.inc = esem[e], cnt, 1
        self.nsem = len(dsem) + len(esem)

    def emit(self, block):
        meth = {"pe": block.tensor, "act": block.scalar, "dve": block.vector, "pool": block.gpsimd, "sp": block.sync}
        for ename in self.ENGS:
            ops = self.ops[ename]

            def body(e, ops=ops):
                waited = {}
                for o in ops:
                    need = {}
                    for d in o.deps:
                        k = id(d.sem)
                        if k not in need or need[k][1] < d.val:
                            need[k] = (d.sem, d.val)
                    for k, (sem, val) in need.items():
                        if waited.get(k, 0) >= val:
                            continue
                        e.wait_ge(sem, val)
                        waited[k] = val
                    if o.fn is None:
                        continue
                    ins = o.fn(e)
                    if o.needs_inc:
                        ins.then_inc(o.sem, o.inc)

            meth[ename](body)


def build_program(NT=2048, E=32, HALF=1024):
    NG = NT // TG
    TT = TG // 128
    nc = bass.Bass("TRN2", target_bir_lowering=False)
    S = Sched(nc)

    def din(name, shape):
        return nc.dram_tensor(name, list(shape), F32, kind="ExternalInput").ap()

    x = din("x", [NT, D])
    xhalo = din("xhalo", [HALO, D])
    hmask_d = din("hmask", [128, 1])
    p_d = din("p", [NT, PLE])
    w_in = din("w_in", [D, DIN])
    w_sgu = din("w_sgu_out", [D, D])
    w_cv = din("w_conv_out", [D, D])
    w_o = din("w_o", [D, D])
    w_pg = din("w_pg", [D, D])
    w_ple = din("w_ple", [PLE, D])
    w_rt = din("w_router", [D, E])
    w_e1 = din("w_e1", [E, D, 2 * D])
    w_e2 = din("w_e2", [E, D, D])
    lnv = din("lnv", [8, D])
    bfm_d = din("bfm", [128, 176])
    convw_d = din("convw", [128, KC * CW])
    bv_d = din("bv", [1, D])
    bo_d = din("bo", [1, D])
    bpg_d = din("bpg", [1, D])
    brt_d = din("brt", [1, E])
    wsT_d = din("wsT", [128, 16 * 128])
    bs_d = din("bs", [1, 16 * 128])
    b1_d = din("b1", [128, E * 32])
    b2_d = din("b2", [E, D])
    ident_d = din("ident", [128, 128])
    tri_d = din("tri", [128, 128])
    iota_d = din("iota", [128, 512])
    out = nc.dram_tensor("out", [NT, D], F32, kind="ExternalOutput").ap()
    h1scr = nc.dram_tensor("h1scr", [NT, D], F32, kind="Internal").ap()
    wscr_t = nc.dram_tensor("wscr", [36, 128, KC * 512], BF16, kind="Internal").ap()
    wscr = [wscr_t[i].rearrange("p (a b) -> p a b", a=KC) for i in range(36)]

    def wview(w2d, c0):
        return w2d.rearrange("(kc p) n -> p kc n", p=128)[:, :, c0:c0 + 512]

    with ExitStack() as top:
        def sb(name, shape, dt, stack=top):
            return stack.enter_context(nc.sbuf_tensor("s_" + name, list(shape), dt))

        pst = [top.enter_context(nc.psum_tensor("ps%d" % i, [128, 512], F32)) for i in range(8)]

        def next_ps():
            i = S.nps % 8
            S.nps += 1
            return ("ps", i), pst[i]

        ident = sb("ident", [128, 128], F32)
        ones0 = sb("ones0", [128, 128], BF16)
        wb = [sb("wb%d" % i, [128, KC, 512], BF16) for i in range(NWB)]
        st6 = sb("st6", [128, 4, 6], F32)
        mv = sb("mv", [128, 2], F32)
        rstd = sb("rstd", [128, 1], F32)

        S.op("sp", lambda e: e.dma_start(out=ident[:], in_=ident_d[:, :]), writes=["ident"], dma="c_ident")
        S.op("dve", lambda e: e.memset(ones0[:], 0.0), writes=["ones0"])
        S.op("dve", lambda e: e.memset(ones0[0:1, :], 1.0), writes=["ones0"])

        LNT = {}

        def load_ln_half(i, hf):
            lng, lnb = LNT["g"], LNT["b"]
            S.op("sp", lambda e: e.dma_start(out=lng[:], in_=lnv[2 * i:2 * i + 1, hf * 1024:(hf + 1) * 1024].partition_broadcast(128)),
                 writes=["lng"], dma="lng")
            S.op("sp", lambda e: e.dma_start(out=lnb[:], in_=lnv[2 * i + 1:2 * i + 2, hf * 1024:(hf + 1) * 1024].partition_broadcast(128)),
                 writes=["lnb"], dma="lnb")

        def ln_group(i, tiles):
            lng, lnb = LNT["g"], LNT["b"]
            for (xt, keys, out_ap, out_keys, np_) in tiles:
                ln_tm(xt, keys, out_ap, out_keys, np_=np_, affine=False)
            for hf in range(2):
                load_ln_half(i, hf)
                for (xt, keys, out_ap, out_keys, np_) in tiles:
                    kh = list(keys[2 * hf:2 * hf + 2])
                    xs = xt[:, hf * 1024:(hf + 1) * 1024]
                    os_ = out_ap[:, hf * 1024:(hf + 1) * 1024]
                    S.op("dve", lambda e, xs=xs, np_=np_: e.tensor_tensor(xs, xs, lng[0:np_, :], ALU.mult), reads=kh + ["lng"], writes=kh)
                    okh = kh if list(out_keys) == list(keys) else list(out_keys)
                    S.op("dve", lambda e, xs=xs, os_=os_, np_=np_: e.tensor_tensor(os_, xs, lnb[0:np_, :], ALU.add),
                         reads=kh + ["lnb"], writes=okh)

        def ln_tm(xt, keys, out_ap, out_keys, np_=128, affine=True):
            def f_stats(e):
                ins = None
                for c in range(4):
                    ins = e.bn_stats(st6[0:np_, c, :], xt[:, c * 512:(c + 1) * 512])
                return ins
            S.op("dve", f_stats, reads=keys, writes=["st6"])
            S.op("dve", lambda e: e.bn_aggr(mv[0:np_, :], st6[0:np_, :, :].rearrange("p a b -> p (a b)")),
                 reads=["st6"], writes=["mv"])
            S.op("dve", lambda e: e.tensor_scalar(rstd[0:np_, :], mv[0:np_, 1:2], LN_EPS, None, ALU.add),
                 reads=["mv"], writes=["rstd"])
            S.op("act", lambda e: e.activation(rstd[0:np_, :], rstd[0:np_, :], AF.Sqrt), reads=["rstd"], writes=["rstd"])
            S.op("dve", lambda e: e.reciprocal(rstd[0:np_, :], rstd[0:np_, :]), reads=["rstd"], writes=["rstd"])
            S.op("dve", lambda e: e.tensor_scalar(xt, xt, mv[0:np_, 0:1], rstd[0:np_, 0:1], ALU.subtract, ALU.mult),
                 reads=list(keys) + ["mv", "rstd"], writes=keys)
            if not affine:
                return
            lng, lnb = LNT["g"], LNT["b"]
            S.op("dve", lambda e: e.tensor_tensor(xt, xt, lng[0:np_, :], ALU.mult), reads=list(keys) + ["lng"], writes=keys)
            S.op("dve", lambda e: e.tensor_tensor(out_ap, xt, lnb[0:np_, :], ALU.add),
                 reads=list(keys) + ["lnb"], writes=out_keys)

        def transpose_tile(src, src_keys, dst, dst_key_fn, tcol, nkc=KC, np_=128):
            per = 512 // np_ if np_ < 128 else 4
            q = 0
            kc = 0
            while kc < nkc:
                n = min(per, nkc - kc)
                pk, ps = next_ps()

                def f_t(e, kc=kc, n=n, ps=ps):
                    ins = None
                    for j in range(n):
                        ins = e.transpose(ps[:, j * np_:(j + 1) * np_], src[:, (kc + j) * 128:(kc + j + 1) * 128],
                                          ident[0:np_, 0:np_])
                    return ins
                S.op("pe", f_t, reads=list(src_keys) + ["ident"], writes=[pk])
                S.op("act", lambda e, kc=kc, n=n, ps=ps: e.activation(
                    dst[:, kc:kc + n, tcol:tcol + np_],
                    ps[:, 0:n * np_].rearrange("p (a b) -> p a b", a=n), AF.Identity),
                    reads=[pk], writes=[dst_key_fn(q)])
                kc += n
                q += 1

        with ExitStack() as p1:
            xh0 = sb("xh0", [128, TT, D], F32, p1)
            h0T = sb("h0T", [128, KC, TG], BF16, p1)
            uT = sb("uT", [128, KC, TG], BF16, p1)
            vtco = sb("vtco", [128, TT * D], F32, p1)
            vt = vtco[:, :].rearrange("p (t d) -> p t d", t=TT)
            co = vtco[:, :].rearrange("p (c n) -> p c n", c=KC)
            LNT["g"] = sb("lng", [128, 1024], F32, p1)
            LNT["b"] = sb("lnb", [128, 1024], F32, p1)
            vn = sb("vn", [128, TT, D], BF16, p1)
            cT = sb("cT", [128, KC, HALO + TG], F32, p1)
            cn = sb("cn", [128, KC, TG], BF16, p1)
            gaT = sb("gaT", [128, KC, TG], BF16, p1)
            gbT = sb("gbT", [128, KC, TG], BF16, p1)
            mT = sb("mT", [128, KC, TG], BF16, p1)
            xh = vtco[0:HALO, 0:D]
            xh_keys = [("vc", i) for i in range(4)]
            h0Th = sb("h0Th", [128, KC, HALO], BF16, p1)
            bfm = sb("bfm", [128, 176], F32, p1)
            convw = sb("convw", [128, KC, CW], F32, p1)
            hmask = sb("hmask", [128, 1], F32, p1)
            bvrow = sb("bvrow", [128, D], BF16, p1)
            borow = sb("borow", [128, D], BF16, p1)
            bsrow = sb("bsrow", [128, 16, 128], BF16, p1)
            wsT = sb("wsT", [128, 16, 128], BF16, p1)
            tri = sb("tri", [128, 128], F32, p1)
            ones32 = sb("ones32", [128, 128], F32, p1)
            sgt = [sb("sgt%d" % i, [128, TG], F32, p1) for i in range(2)]
            sqt = [sb("sqt%d" % i, [128, TG], F32, p1) for i in range(2)]
            sgh = sb("sgh", [128, HALO], F32, p1)
            cmean = sb("cmean", [128, TG], F32, p1)
            cmsq = sb("cmsq", [128, TG], F32, p1)
            crstd = sb("crstd", [128, TG], F32, p1)
            cnb = sb("cnb", [128, TG], F32, p1)

            S.op("sp", lambda e: e.dma_start(out=bfm[:], in_=bfm_d[:, :]), writes=["bfm"], dma="c_bfm")
            S.op("sp", lambda e: e.dma_start(out=convw[:], in_=convw_d.rearrange("p (a b) -> p a b", a=KC)),
                 writes=["convw"], dma="c_convw")
            S.op("sp", lambda e: e.dma_start(out=hmask[:], in_=hmask_d[:, :]), writes=["hmask"], dma="c_hmask")
            S.op("pool", lambda e: e.dma_start(out=wsT[:], in_=wsT_d.rearrange("p (a b) -> p a b", a=16)),
                 writes=["wsT"], dma="c_wsT")
            S.op("sp", lambda e: e.dma_start(out=tri[:], in_=tri_d[:, :]), writes=["tri"], dma="c_tri")
            S.op("dve", lambda e: e.memset(ones32[:], 1.0), writes=["ones32"])
            for (t_, d_, nm) in ((bvrow, bv_d, "bvrow"), (borow, bo_d, "borow")):
                S.op("dve", lambda e, t_=t_: e.memset(t_[:], 0.0), writes=[nm])
                S.op("pool", lambda e, t_=t_, d_=d_: e.dma_start(out=t_[0:1, :], in_=d_[:, :]), writes=[nm], dma="c_" + nm)
            S.op("dve", lambda e: e.memset(bsrow[:], 0.0), writes=["bsrow"])
            S.op("pool", lambda e: e.dma_start(out=bsrow[0:1, :, :], in_=bs_d.rearrange("o (a b) -> o a b", a=16)),
                 writes=["bsrow"], dma="c_bsrow")
            for g16 in range(16):
                S.op("dve", lambda e, g16=g16: e.tensor_tensor(wsT[:, g16, :], wsT[:, g16, :], tri[:], ALU.mult),
                     reads=["wsT", "tri"], writes=["wsT"])

            def xkeys(tt):
                return [("xh0", tt, dg) for dg in range(4)]

            def h0T_keys():
                return [("h0T", tt, q) for tt in range(TT) for q in range(4)]

            def prologue(g):
                tok0 = g * TG
                for tt in range(TT):
                    S.op("sp", lambda e, tt=tt: e.dma_start(out=xh0[:, tt, :], in_=x[tok0 + tt * 128: tok0 + (tt + 1) * 128, :]),
                         writes=xkeys(tt), dma=("x", tt))
                tiles0 = [(xh0[:, tt, :], xkeys(tt), xh0[:, tt, :], xkeys(tt), 128) for tt in range(TT)]
                if g == 0:
                    S.op("sp", lambda e: e.dma_start(out=xh, in_=xhalo[:, :]), writes=xh_keys, dma="c_xh")
                    ln_group(0, [(xh, xh_keys, xh, xh_keys, HALO)] + tiles0)
                    transpose_tile(xh, xh_keys, h0Th, lambda q: "h0Th", 0, np_=HALO)
                else:
                    S.op("dve", lambda e: e.tensor_copy(cT[:, :, 0:HALO], cT[:, :, TG:TG + HALO]),
                         reads=[("cT", c) for c in range(KC)], writes=[("cT", c) for c in range(KC)])
                    ln_group(0, tiles0)
                for tt in range(TT):
                    transpose_tile(xh0[:, tt, :], xkeys(tt), h0T, lambda q, tt=tt: ("h0T", tt, q), tt * 128)

            def fform(wbt, wbk, rhs_fn, rhs_keys, N, evac):
                for j in range(4):
                    pk, ps = next_ps()

                    def f_mm(e, j=j, ps=ps):
                        ins = None
                        for kc in range(KC):
                            ins = e.matmul(ps[:, 0:N], lhsT=wbt[:, kc, j * 128:(j + 1) * 128], rhs=rhs_fn(kc),
                                           start=(kc == 0), stop=(kc == KC - 1))
                        return ins
                    S.op("pe", f_mm, reads=[wbk] + list(rhs_keys), writes=[pk])
                    evac(j, ps, pk)

            def tform(wbt, wbk, lhs_fn, lhs_keys_fn, ntt, bias_rhs, bias_key, evac):
                for tt in range(ntt):
                    pk, ps = next_ps()

                    def f_mm(e, tt=tt, ps=ps):
                        ins = None
                        for kc in range(KC):
                            ins = e.matmul(ps[:, :], lhsT=lhs_fn(kc, tt), rhs=wbt[:, kc, :],
                                           start=(kc == 0), stop=(kc == KC - 1 and bias_rhs is None))
                        if bias_rhs is not None:
                            ins = e.matmul(ps[:, :], lhsT=ones0[:, :], rhs=bias_rhs, start=False, stop=True)
                        return ins
                    rk = [wbk, "ones0"] + list(lhs_keys_fn(tt)) + ([bias_key] if bias_key else [])
                    S.op("pe", f_mm, reads=rk, writes=[pk])
                    evac(tt, ps, pk)

            def conv_chunks(cs):
                for c in cs:
                    S.op("dve", lambda e, c=c: e.tensor_scalar(co[:, c, :], cT[:, c, 2:2 + TG], convw[:, c, 0:1], bfm[:, 128 + c:129 + c],
                                                               ALU.mult, ALU.add),
                         reads=[("cT", c), "convw", "bfm"], writes=[("co", c), ("vc", c // 2)])
                for k in range(1, CW):
                    for c in cs:
                        S.op("dve", lambda e, k=k, c=c: e.scalar_tensor_tensor(co[:, c, :], cT[:, c, 2 + k:2 + k + TG], convw[:, c, k:k + 1],
                                                                                co[:, c, :], ALU.mult, ALU.add),
                             reads=[("cT", c), "convw", ("co", c)], writes=[("co", c)])

            def conv_ln():
                pk1, ps1 = next_ps()
                pk2, ps2 = next_ps()
                for c in range(KC):
                    sq = sqt[c % 2]
                    sk = ("sqt", c % 2)
                    S.op("act", lambda e, c=c, sq=sq: e.activation(sq[:], co[:, c, :], AF.Square), reads=[("co", c)], writes=[sk])
                    S.op("pe", lambda e, c=c: e.matmul(ps1[:, 0:TG], lhsT=ones32[:, :], rhs=co[:, c, :], start=(c == 0), stop=(c == KC - 1)),
                         reads=[("co", c), "ones32"], writes=[pk1])
                    S.op("pe", lambda e, c=c, sq=sq: e.matmul(ps2[:, 0:TG], lhsT=ones32[:, :], rhs=sq[:], start=(c == 0), stop=(c == KC - 1)),
                         reads=[sk, "ones32"], writes=[pk2])
                S.op("dve", lambda e: e.tensor_scalar(cmean[:], ps1[:, 0:TG], 1.0 / D, None, ALU.mult), reads=[pk1], writes=["cmean"])
                S.op("dve", lambda e: e.tensor_tensor(cmsq[:], cmean[:], cmean[:], ALU.mult), reads=["cmean"], writes=["cmsq"])
                S.op("dve", lambda e: e.scalar_tensor_tensor(crstd[:], ps2[:, 0:TG], 1.0 / D, cmsq[:], ALU.mult, ALU.subtract),
                     reads=[pk2, "cmsq"], writes=["crstd"])
                S.op("dve", lambda e: e.tensor_scalar(crstd[:], crstd[:], LN_EPS, None, ALU.add), reads=["crstd"], writes=["crstd"])
                S.op("act", lambda e: e.activation(crstd[:], crstd[:], AF.Sqrt), reads=["crstd"], writes=["crstd"])
                S.op("dve", lambda e: e.reciprocal(crstd[:], crstd[:]), reads=["crstd"], writes=["crstd"])
                S.op("dve", lambda e: e.scalar_tensor_tensor(cnb[:], cmean[:], -1.0, crstd[:], ALU.mult, ALU.mult),
                     reads=["cmean", "crstd"], writes=["cnb"])
                for c in range(KC):
                    S.op("dve", lambda e, c=c: e.tensor_tensor(co[:, c, :], co[:, c, :], crstd[:], ALU.mult),
                         reads=[("co", c), "crstd"], writes=[("co", c)])
                    S.op("dve", lambda e, c=c: e.tensor_tensor(co[:, c, :], co[:, c, :], cnb[:], ALU.add),
                         reads=[("co", c), "cnb"], writes=[("co", c)])
                    S.op("act", lambda e, c=c: e.activation(cn[:, c, :], co[:, c, :], AF.Silu, bias=bfm[:, 160 + c:161 + c],
                                                            scale=bfm[:, 144 + c:145 + c]),
                         reads=[("co", c), "bfm"], writes=[("cn", c), ("vc", c // 2)])

            def spatial():
                for tt in range(TT):
                    for q in range(4):
                        pk, ps = next_ps()

                        def f_mm(e, tt=tt, q=q, ps=ps):
                            ins = None
                            for j in range(4):
                                g16 = 4 * q + j
                                e.matmul(ps[:, j * 128:(j + 1) * 128], lhsT=vn[:, tt, g16 * 128:(g16 + 1) * 128], rhs=wsT[:, g16, :],
                                         start=True, stop=False)
                                ins = e.matmul(ps[:, j * 128:(j + 1) * 128], lhsT=ones0[:, :], rhs=bsrow[:, g16, :], start=False, stop=True)
                            return ins
                        S.op("pe", f_mm, reads=[("vn", tt), "wsT", "bsrow", "ones0"], writes=[pk])
                        uk = [("uT", 4 * q + j) for j in range(4)]
                        S.op("dve", lambda e, tt=tt, q=q, ps=ps: e.tensor_tensor(
                            uT[:, 4 * q:4 * q + 4, tt * 128:(tt + 1) * 128], uT[:, 4 * q:4 * q + 4, tt * 128:(tt + 1) * 128],
                            ps[:, :].rearrange("p (a b) -> p a b", a=4), ALU.mult), reads=[pk] + uk, writes=uk)

            def epilogue(g):
                tok0 = g * TG
                ln_group(2, [(xh0[:, tt, :], xkeys(tt), xh0[:, tt, :], xkeys(tt), 128) for tt in range(TT)])
                for tt in range(TT):
                    S.op("sp", lambda e, tt=tt: e.dma_start(out=h1scr[tok0 + tt * 128: tok0 + (tt + 1) * 128, :], in_=xh0[:, tt, :]),
                         reads=xkeys(tt), writes=[("h1scr", g * TT + tt)], dma=("h1st", tt))

            def make_consume(g, bi):
                def consume(wbt, wbk):
                    if bi == P1_ORDER[0]:
                        prologue(g)
                    h0rhs = lambda kc: h0T[:, kc, :]
                    if bi < 4:
                        def ev(j, ps, pk):
                            c = bi * 4 + j
                            S.op("act", lambda e: e.activation(uT[:, c, :], ps[:, 0:TG], AF.Gelu, bias=bfm[:, c:c + 1]),
                                 reads=[pk, "bfm"], writes=[("uT", c)])
                        fform(wbt, wbk, h0rhs, h0T_keys(), TG, ev)
                    elif bi < 8:
                        dg = bi - 4

                        def ev(tt, ps, pk):
                            S.op("act", lambda e: e.activation(vt[:, tt, dg * 512:(dg + 1) * 512], ps[:, :], AF.Gelu),
                                 reads=[pk], writes=[("vc", tt * 4 + dg)])
                        tform(wbt, wbk, lambda kc, tt: h0T[:, kc, tt * 128:(tt + 1) * 128],
                              lambda tt: [("h0T", tt, q) for q in range(4)], TT, bvrow[:, dg * 512:(dg + 1) * 512], "bvrow", ev)
                        if bi == 7:
                            ln_group(1, [(vt[:, tt, :], [("vc", tt * 4 + d_) for d_ in range(4)], vn[:, tt, :], [("vn", tt)], 128)
                                         for tt in range(TT)])
                    elif bi < 12:
                        def ev(j, ps, pk):
                            c = (bi - 8) * 4 + j
                            S.op("act", lambda e: e.activation(cT[:, c, HALO:HALO + TG], ps[:, 0:TG], AF.Identity, bias=bfm[:, 32 + c:33 + c]),
                                 reads=[pk, "bfm"], writes=[("cT", c)])
                            if g == 0:
                                pk2, ps2 = next_ps()

                                def f_mm(e, ps2=ps2, j=j):
                                    ins = None
                                    for kc in range(KC):
                                        ins = e.matmul(ps2[:, 0:HALO], lhsT=wbt[:, kc, j * 128:(j + 1) * 128], rhs=h0Th[:, kc, :],
                                                       start=(kc == 0), stop=(kc == KC - 1))
                                    return ins
                                S.op("pe", f_mm, reads=[wbk, "h0Th"], writes=[pk2])
                                S.op("act", lambda e, ps2=ps2: e.activation(cT[:, c, 0:HALO], ps2[:, 0:HALO], AF.Identity,
                                                                            bias=bfm[:, 32 + c:33 + c]),
                                     reads=[pk2, "bfm"], writes=[("cT", c)])
                        fform(wbt, wbk, h0rhs, h0T_keys(), TG, ev)
                    elif bi < 16:
                        def ev(j, ps, pk):
                            c = (bi - 12) * 4 + j
                            S.op("act", lambda e: e.activation(mT[:, c, :], ps[:, 0:TG], AF.Sigmoid, bias=bfm[:, 48 + c:49 + c]),
                                 reads=[pk, "bfm"], writes=[("mT", c)])
                            if g == 0:
                                pk2, ps2 = next_ps()

                                def f_mm(e, ps2=ps2, j=j):
                                    ins = None
                                    for kc in range(KC):
                                        ins = e.matmul(ps2[:, 0:HALO], lhsT=wbt[:, kc, j * 128:(j + 1) * 128], rhs=h0Th[:, kc, :],
                                                       start=(kc == 0), stop=(kc == KC - 1))
                                    return ins
                                S.op("pe", f_mm, reads=[wbk, "h0Th"], writes=[pk2])
                                S.op("act", lambda e, ps2=ps2: e.activation(sgh[:], ps2[:, 0:HALO], AF.Sigmoid, bias=bfm[:, 48 + c:49 + c]),
                                     reads=[pk2, "bfm"], writes=["sgh"])
                                S.op("dve", lambda e: e.scalar_tensor_tensor(cT[:, c, 0:HALO], cT[:, c, 0:HALO], hmask[:, 0:1], sgh[:],
                                                                             ALU.mult, ALU.mult),
                                     reads=["sgh", "hmask", ("cT", c)], writes=[("cT", c)])
                        fform(wbt, wbk, h0rhs, h0T_keys(), TG, ev)
                        cs = [(bi - 12) * 4 + j for j in range(4)]
                        for c in cs:
                            S.op("dve", lambda e, c=c: e.tensor_tensor(cT[:, c, HALO:HALO + TG], cT[:, c, HALO:HALO + TG], mT[:, c, :], ALU.mult),
                                 reads=[("mT", c), ("cT", c)], writes=[("cT", c)])
                        conv_chunks(cs)
                    elif bi < 24:
                        def ev(j, ps, pk):
                            c = ((bi - 16) % 4) * 4 + j
                            dst, nm, off = (gaT, "gaT", 64) if bi < 20 else (gbT, "gbT", 80)
                            S.op("act", lambda e: e.activation(dst[:, c, :], ps[:, 0:TG], AF.Sigmoid, bias=bfm[:, off + c:off + c + 1]),
                                 reads=[pk, "bfm"], writes=[(nm, c)])
                        fform(wbt, wbk, h0rhs, h0T_keys(), TG, ev)
                    elif bi < 28:
                        if bi == 24:
                            spatial()
                        def ev(j, ps, pk):
                            c = (bi - 24) * 4 + j
                            S.op("dve", lambda e: e.scalar_tensor_tensor(mT[:, c, :], ps[:, 0:TG], bfm[:, 96 + c:97 + c], gaT[:, c, :],
                                                                         ALU.add, ALU.mult),
                                 reads=[pk, "bfm", ("gaT", c)], writes=[("mT", c)])
                        fform(wbt, wbk, lambda kc: uT[:, kc, :], [("uT", c) for c in range(KC)], TG, ev)
                    elif bi < 32:
                        if bi == 28:
                            conv_ln()
                        def ev(j, ps, pk):
                            c = (bi - 28) * 4 + j
                            sg = sgt[c % 2]
                            sk = ("sgt", c % 2)
                            S.op("dve", lambda e: e.scalar_tensor_tensor(sg[:], ps[:, 0:TG], bfm[:, 112 + c:113 + c], gbT[:, c, :],
                                                                         ALU.add, ALU.mult),
                                 reads=[pk, "bfm", ("gbT", c)], writes=[sk])
                            S.op("dve", lambda e: e.tensor_tensor(mT[:, c, :], mT[:, c, :], sg[:], ALU.add),
                                 reads=[sk, ("mT", c)], writes=[("mT", c)])
                        fform(wbt, wbk, lambda kc: cn[:, kc, :], [("cn", c) for c in range(KC)], TG, ev)
                    else:
                        dg = bi - 32

                        def ev(tt, ps, pk):
                            S.op("dve", lambda e: e.scalar_tensor_tensor(xh0[:, tt, dg * 512:(dg + 1) * 512], xh0[:, tt, dg * 512:(dg + 1) * 512],
                                                                         DN_ALPHA, ps[:, :], ALU.mult, ALU.add),
                                 reads=[pk, ("xh0", tt, dg)], writes=[("xh0", tt, dg)])
                        tform(wbt, wbk, lambda kc, tt: mT[:, kc, tt * 128:(tt + 1) * 128],
                              lambda tt: [("mT", c) for c in range(KC)], TT, borow[:, dg * 512:(dg + 1) * 512], "borow", ev)
                        if bi == 35:
                            epilogue(g)
                return consume

            blocks = []
            for g in range(NG):
                for bi in P1_ORDER:
                    if bi < 24:
                        src = wview(w_in, bi * 512)
                    elif bi < 28:
                        src = wview(w_sgu, (bi - 24) * 512)
                    elif bi < 32:
                        src = wview(w_cv, (bi - 28) * 512)
                    else:
                        src = wview(w_o, (bi - 32) * 512)
                    if NG > 1 and g == 0:
                        blocks.append((src, make_consume(g, bi), (wscr[bi], ("wscr", bi))))
                    elif NG > 1:
                        blocks.append(((wscr[bi], ("wscr", bi)), make_consume(g, bi)))
                    else:
                        blocks.append((src, make_consume(g, bi)))

            def run_stream(blocks):
                def issue(i):
                    src = blocks[i][0]
                    b = i % NWB
                    rk = []
                    if isinstance(src, tuple):
                        src, k_ = src
                        rk = [k_]
                    S.op("pool", lambda e: e.dma_start(out=wb[b][:], in_=src), reads=rk, writes=[("wb", b)], dma=("wb", b))
                    if len(blocks[i]) > 2:
                        dst, dk = blocks[i][2]
                        S.op("sp", lambda e: e.dma_start(out=dst, in_=wb[b][:]), reads=[("wb", b)], writes=[dk], dma=("wst", b))
                for i in range(min(NWB, len(blocks))):
                    issue(i)
                for i in range(len(blocks)):
                    blocks[i][1](wb[i % NWB], ("wb", i % NWB))
                    if i + NWB < len(blocks):
                        issue(i + NWB)

            run_stream(blocks)
            S.barrier()

        NTT = HALF // 128
        NH = NT // HALF
        NTA = NH * NTT
        NST = CAP // 128
        CAPA = NH * CAP
        assert CAPA <= 512
        actscr = nc.dram_tensor("actscr", [E, 128, KC * CAPA], BF16, kind="Internal").ap()
        accscr = nc.dram_tensor("accscr", [NT, D], F32, kind="Internal").ap()
        uniq = [0]

        def sbn(name, shape, dt, stack):
            uniq[0] += 1
            return stack.enter_context(nc.sbuf_tensor("s_%s_%d" % (name, uniq[0]), list(shape), dt))

        with ExitStack() as p2:
            Gt = sb("G", [128, NTA, E], F32, p2)
            Mf = sb("Mf", [128, NTA, E], F32, p2)
            Mbf = sb("Mbf", [128, NTA, E], BF16, p2)
            rank = sb("rank", [128, NTA, E], F32, p2)
            iota = sb("iota", [128, CAP], F32, p2)
            identbf = sb("identbf", [128, 128], BF16, p2)
            allones = sb("allones", [128, 128], BF16, p2)
            striu = sb("striu", [128, 128], BF16, p2)
            trif = sb("trif", [128, 128], F32, p2)
            lg = sb("lg", [128, E], F32, p2)
            ex = sb("ex", [128, E], F32, p2)
            m8 = sb("m8", [128, 8], F32, p2)
            nmx = sb("nmx", [128, 1], F32, p2)
            ssum = sb("ssum", [128, 1], F32, p2)
            b1t = sb("b1t", [128, E, 32], F32, p2)

            S.op("sp", lambda e: e.dma_start(out=iota[:], in_=iota_d[:, 0:CAP]), writes=["iota"], dma="c_iota")
            S.op("sp", lambda e: e.dma_start(out=trif[:], in_=tri_d[:, :]), writes=["trif"], dma="c_trif")
            S.op("sp", lambda e: e.dma_start(out=b1t[:], in_=b1_d.rearrange("p (a b) -> p a b", a=E)), writes=["b1t"], dma="c_b1t")
            S.op("dve", lambda e: e.tensor_copy(identbf[:], ident[:]), reads=["ident"], writes=["identbf"])
            S.op("dve", lambda e: e.memset(allones[:], 1.0), writes=["allones"])
            S.op("dve", lambda e: e.tensor_tensor(striu[:], trif[:], ident[:], ALU.subtract), reads=["trif", "ident"], writes=["striu"])

            def akeys(tt):
                return [("acc", tt, dg) for dg in range(4)]

            SA = {}
            SB = {}
            SC = {}

            def open_A():
                st = ExitStack()
                SA.clear()
                SA["stack"] = st
                SA["acc"] = sbn("accA", [128, NTT, D], F32, st)
                SA["h1T"] = sbn("h1T", [128, KC, HALF], BF16, st)
                SA["pt"] = sbn("pt", [128, PLE], F32, st)
                SA["pT"] = sbn("pT", [128, 2, HALF], BF16, st)
                SA["wple"] = sbn("wple", [128, 2, D], BF16, st)
                SA["wr"] = sbn("wr", [128, KC, E], BF16, st)
                SA["brrow"] = sbn("brrow", [128, E], BF16, st)
                SA["bpgrow"] = sbn("bpgrow", [128, D], BF16, st)
                SA["b2bf"] = sbn("b2bf", [128, D], BF16, st)
                SA["GT"] = sbn("GT", [128, HALF], BF16, st)
                SA["tmp"] = [sbn("ptmp", [128, 512], F32, st) for _ in range(2)]
                wple, wr, brrow, bpgrow, b2bf, GT = (SA[k] for k in ("wple", "wr", "brrow", "bpgrow", "b2bf", "GT"))
                S.op("pool", lambda e: e.dma_start(out=wple[:], in_=w_ple.rearrange("(kc p) n -> p kc n", p=128)), writes=["wple"], dma="c_wple")
                S.op("pool", lambda e: e.dma_start(out=wr[:], in_=w_rt.rearrange("(kc p) n -> p kc n", p=128)), writes=["wr"], dma="c_wr")
                S.op("dve", lambda e: e.memset(brrow[:], 0.0), writes=["brrow"])
                S.op("pool", lambda e: e.dma_start(out=brrow[0:1, :], in_=brt_d[:, :]), writes=["brrow"], dma="c_brrow")
                S.op("dve", lambda e: e.memset(bpgrow[:], 0.0), writes=["bpgrow"])
                S.op("pool", lambda e: e.dma_start(out=bpgrow[0:1, :], in_=bpg_d[:, :]), writes=["bpgrow"], dma="c_bpgrow")
                S.op("dve", lambda e: e.memset(b2bf[:], 0.0), writes=["b2bf"])
                S.op("pool", lambda e: e.dma_start(out=b2bf[0:E, :], in_=b2_d[:, :]), writes=["b2bf"], dma="c_b2bf")
                S.op("dve", lambda e: e.memset(GT[:], 0.0), writes=["GT"])

            def close_A():
                S.barrier()
                SA["stack"].close()

            def stageA_prologue(hh):
                t0 = hh * HALF
                open_A()
                acc, h1T, pt, pT, wr, brrow, b2bf, GT = (SA[k] for k in ("acc", "h1T", "pt", "pT", "wr", "brrow", "b2bf", "GT"))
                for tt in range(NTT):
                    S.op("sp", lambda e, tt=tt: e.dma_start(out=acc[:, tt, :], in_=h1scr[t0 + tt * 128:t0 + (tt + 1) * 128, :]),
                         reads=[("h1scr", hh * NTT + tt)], writes=akeys(tt), dma=("h1ld", tt % 2))
                for tt in range(NTT):
                    gt = hh * NTT + tt
                    transpose_tile(acc[:, tt, :], akeys(tt), h1T, lambda q, tt=tt: ("h1T", tt), tt * 128)
                    S.op("sp", lambda e, tt=tt: e.dma_start(out=pt[:], in_=p_d[t0 + tt * 128:t0 + (tt + 1) * 128, :]),
                         writes=["pt"], dma="pt")
                    transpose_tile(pt, ["pt"], pT, lambda q, tt=tt: ("pT", tt), tt * 128, nkc=2)
                    pk, ps = next_ps()

                    def f_mm(e, tt=tt, ps=ps):
                        for kc in range(KC):
                            e.matmul(ps[:, 0:E], lhsT=h1T[:, kc, tt * 128:(tt + 1) * 128], rhs=wr[:, kc, :], start=(kc == 0), stop=False)
                        return e.matmul(ps[:, 0:E], lhsT=ones0[:, :], rhs=brrow[:, :], start=False, stop=True)
                    S.op("pe", f_mm, reads=[("h1T", tt), "wr", "brrow", "ones0"], writes=[pk])
                    S.op("dve", lambda e, ps=ps: e.tensor_copy(lg[:], ps[:, 0:E]), reads=[pk], writes=["lg"])
                    S.op("dve", lambda e: e.max(m8[:], lg[:]), reads=["lg"], writes=["m8"])
                    S.op("dve", lambda e, gt=gt: e.tensor_scalar(Mf[:, gt, :], lg[:], m8[:, 3:4], None, ALU.is_ge), reads=["lg", "m8"], writes=[("Mf", gt)])
                    S.op("dve", lambda e, gt=gt: e.tensor_copy(Mbf[:, gt, :], Mf[:, gt, :]), reads=[("Mf", gt)], writes=[("Mbf", gt)])
                    S.op("dve", lambda e: e.tensor_scalar(nmx[:], m8[:, 0:1], -1.0, None, ALU.mult), reads=["m8"], writes=["nmx"])
                    S.op("act", lambda e: e.activation(ex[:], lg[:], AF.Exp, bias=nmx[:, 0:1]), reads=["lg", "nmx"], writes=["ex"])
                    S.op("dve", lambda e, gt=gt: e.tensor_tensor(ex[:], ex[:], Mf[:, gt, :], ALU.mult), reads=["ex", ("Mf", gt)], writes=["ex"])
                    S.op("dve", lambda e: e.reduce_sum(ssum[:], ex[:], AX.X), reads=["ex"], writes=["ssum"])
                    S.op("dve", lambda e: e.reciprocal(ssum[:], ssum[:]), reads=["ssum"], writes=["ssum"])
                    S.op("dve", lambda e, gt=gt: e.tensor_scalar(Gt[:, gt, :], ex[:], ssum[:, 0:1], None, ALU.mult),
                         reads=["ex", "ssum"], writes=[("G", gt)])
                    pkr, psr = next_ps()

                    def f_rank(e, tt=tt, psr=psr):
                        ins = None
                        for t2 in range(tt + 1):
                            ins = e.matmul(psr[:, 0:E], lhsT=(allones[:, :] if t2 < tt else striu[:, :]), rhs=Mbf[:, hh * NTT + t2, :],
                                           start=(t2 == 0), stop=(t2 == tt))
                        return ins
                    S.op("pe", f_rank, reads=[("Mbf", hh * NTT + t2) for t2 in range(tt + 1)] + ["allones", "striu"], writes=[pkr])
                    S.op("dve", lambda e, gt=gt, psr=psr: e.tensor_copy(rank[:, gt, :], psr[:, 0:E]), reads=[pkr], writes=[("rank", gt)])
                    pk2, ps2 = next_ps()
                    S.op("pe", lambda e, gt=gt, ps2=ps2: e.transpose(ps2[0:E, 0:128], Gt[:, gt, :], ident[:, :]),
                         reads=[("G", gt), "ident"], writes=[pk2])
                    S.op("act", lambda e, tt=tt, ps2=ps2: e.activation(GT[0:E, tt * 128:(tt + 1) * 128], ps2[0:E, 0:128], AF.Identity),
                         reads=[pk2], writes=[("GT", tt)])
                    S.op("dve", lambda e, tt=tt: e.tensor_scalar(acc[:, tt, :], acc[:, tt, :], DN_ALPHA, None, ALU.mult),
                         reads=akeys(tt), writes=akeys(tt))
                    for dg in range(4):
                        pk3, ps3 = next_ps()
                        S.op("pe", lambda e, tt=tt, dg=dg, ps3=ps3: e.matmul(ps3[:, :], lhsT=GT[:, tt * 128:(tt + 1) * 128],
                                                                             rhs=b2bf[:, dg * 512:(dg + 1) * 512], start=True, stop=True),
                             reads=[("GT", tt), "GT", "b2bf"], writes=[pk3])
                        S.op("dve", lambda e, tt=tt, dg=dg, ps3=ps3: e.tensor_tensor(acc[:, tt, dg * 512:(dg + 1) * 512],
                                                                                    acc[:, tt, dg * 512:(dg + 1) * 512], ps3[:, :], ALU.add),
                             reads=[pk3, ("acc", tt, dg)], writes=[("acc", tt, dg)])

            def make_ple(hh, dg):
                def consume(wbt, wbk):
                    if dg == 0:
                        stageA_prologue(hh)
                    acc, h1T, pT, wple, bpgrow = (SA[k] for k in ("acc", "h1T", "pT", "wple", "bpgrow"))
                    tmps = SA["tmp"]

                    def ev(tt, ps, pk):
                        sg = tmps[tt % 2]
                        sk = ("ptmp", tt % 2)
                        S.op("act", lambda e: e.activation(sg[:], ps[:, :], AF.Sigmoid), reads=[pk], writes=[sk])
                        pk2, ps2 = next_ps()

                        def f_mm(e):
                            e.matmul(ps2[:, :], lhsT=pT[:, 0, tt * 128:(tt + 1) * 128], rhs=wple[:, 0, dg * 512:(dg + 1) * 512], start=True, stop=False)
                            return e.matmul(ps2[:, :], lhsT=pT[:, 1, tt * 128:(tt + 1) * 128], rhs=wple[:, 1, dg * 512:(dg + 1) * 512],
                                            start=False, stop=True)
                        S.op("pe", f_mm, reads=[("pT", tt), "wple"], writes=[pk2])
                        S.op("dve", lambda e: e.tensor_tensor(sg[:], sg[:], ps2[:, :], ALU.mult), reads=[sk, pk2], writes=[sk])
                        S.op("dve", lambda e: e.tensor_tensor(acc[:, tt, dg * 512:(dg + 1) * 512], acc[:, tt, dg * 512:(dg + 1) * 512], sg[:], ALU.add),
                             reads=[sk, ("acc", tt, dg)], writes=[("acc", tt, dg)])
                    tform(wbt, wbk, lambda kc, tt: h1T[:, kc, tt * 128:(tt + 1) * 128], lambda tt: [("h1T", tt)], NTT,
                          bpgrow[:, dg * 512:(dg + 1) * 512], "bpgrow", ev)
                    if dg == 3:
                        t0 = hh * HALF
                        for tt in range(NTT):
                            S.op("sp", lambda e, tt=tt: e.dma_start(out=accscr[t0 + tt * 128:t0 + (tt + 1) * 128, :], in_=acc[:, tt, :]),
                                 reads=akeys(tt), writes=[("accscr", hh * NTT + tt)], dma=("accst", tt % 2))
                        close_A()
                        if hh == NH - 1:
                            open_B()
                return consume

            def open_B():
                st = ExitStack()
                SB.clear()
                SB["stack"] = st
                h1tm = sbn("h1tm", [128, NTA, D], BF16, st)
                SB["h1tm"] = h1tm
                SB["xgT"] = sbn("xgT", [128, KC, CAPA], BF16, st)
                SB["actT"] = [sbn("actT", [128, KC, CAPA], BF16, st) for _ in range(2)]
                SB["Pe"] = [sbn("Pe", [128, NTT, CAP], BF16, st) for _ in range(NH)]
                SB["tA"] = [sbn("tA", [128, CAPA], F32, st) for _ in range(2)]
                SB["tB"] = [sbn("tB", [128, CAPA], F32, st) for _ in range(2)]
                SB["rot"] = {"A": 0, "B": 0}
                for gt in range(NTA):
                    S.op("pool", lambda e, gt=gt: e.dma_start(out=h1tm[:, gt, :], in_=h1scr[gt * 128:(gt + 1) * 128, :]),
                         reads=[("h1scr", gt)], writes=[("h1tm", gt)], dma=("h1tm", gt % 2))

            def close_B():
                S.barrier()
                SB["stack"].close()

            def btmp(kind):
                i = SB["rot"][kind] % 2
                SB["rot"][kind] += 1
                return {"A": SB["tA"], "B": SB["tB"]}[kind][i], ("b" + kind, i)

            def expertB_prologue(ex_):
                xgT, h1tm = SB["xgT"], SB["h1tm"]
                npair = 512 // CAP
                for hh in range(NH):
                    Pe = SB["Pe"][hh]
                    for tt in range(NTT):
                        gt = hh * NTT + tt
                        S.op("dve", lambda e, tt=tt, gt=gt, Pe=Pe: e.tensor_scalar(Pe[:, tt, :], iota[:], rank[:, gt, ex_:ex_ + 1], Mf[:, gt, ex_:ex_ + 1],
                                                                                 ALU.is_equal, ALU.mult),
                             reads=["iota", ("rank", gt), ("Mf", gt)], writes=[("Pe", hh, tt)])
                    for q in range(KC // npair):
                        pk, ps = next_ps()

                        def f_mm(e, q=q, ps=ps, hh=hh, Pe=Pe):
                            ins = None
                            for j in range(npair):
                                dc = q * npair + j
                                for tt in range(NTT):
                                    ins = e.matmul(ps[:, j * CAP:(j + 1) * CAP], lhsT=h1tm[:, hh * NTT + tt, dc * 128:(dc + 1) * 128], rhs=Pe[:, tt, :],
                                                   start=(tt == 0), stop=(tt == NTT - 1))
                            return ins
                        S.op("pe", f_mm, reads=[("Pe", hh, tt) for tt in range(NTT)] + [("h1tm", hh * NTT + tt) for tt in range(NTT)], writes=[pk])
                        S.op("act", lambda e, q=q, ps=ps, hh=hh: e.activation(xgT[:, q * npair:(q + 1) * npair, hh * CAP:(hh + 1) * CAP],
                                                                              ps[:, 0:npair * CAP].rearrange("p (a b) -> p a b", a=npair), AF.Identity),
                             reads=[pk], writes=[("xgT", q, hh)])

            def make_w1(ex_, bi):
                def consume(wbt, wbk):
                    if bi == 0:
                        expertB_prologue(ex_)
                    xgT = SB["xgT"]
                    ab = ex_ % 2
                    actT = SB["actT"][ab]
                    for j in range(4):
                        fc = (bi % 4) * 4 + j
                        pk, ps = next_ps()

                        def f_mm(e, j=j, ps=ps):
                            ins = None
                            for kc in range(KC):
                                ins = e.matmul(ps[:, 0:CAPA], lhsT=wbt[:, kc, j * 128:(j + 1) * 128], rhs=xgT[:, kc, :],
                                               start=(kc == 0), stop=(kc == KC - 1))
                            return ins
                        S.op("pe", f_mm, reads=[wbk] + [("xgT", q, hh) for q in range(KC * CAP // 512) for hh in range(NH)], writes=[pk])
                        dst = actT[:, fc, :]
                        ak = ("actT", ab, fc)
                        if bi < 4:
                            gc, gk = btmp("A")
                            sg, sk = btmp("B")
                            S.op("dve", lambda e, ps=ps, gc=gc, fc=fc: e.tensor_scalar(gc[:], ps[:, 0:CAPA], b1t[:, ex_, fc:fc + 1], SW_LIM,
                                                                                       ALU.add, ALU.min),
                                 reads=[pk, "b1t"], writes=[gk])
                            S.op("act", lambda e, gc=gc, sg=sg: e.activation(sg[:], gc[:], AF.Sigmoid, scale=SW_ALPHA),
                                 reads=[gk], writes=[sk])
                            S.op("dve", lambda e, gc=gc, sg=sg, dst=dst: e.tensor_tensor(dst, gc[:], sg[:], ALU.mult),
                                 reads=[gk, sk], writes=[ak])
                        else:
                            ub, uk = btmp("A")
                            S.op("act", lambda e, ps=ps, ub=ub, fc=fc: e.activation(ub[:], ps[:, 0:CAPA], AF.Identity,
                                                                                    bias=b1t[:, ex_, 16 + fc:17 + fc]),
                                 reads=[pk, "b1t"], writes=[uk])
                            S.op("dve", lambda e, ub=ub: e.tensor_scalar(ub[:], ub[:], SW_LIM, -SW_LIM, ALU.min, ALU.max),
                                 reads=[uk], writes=[uk])
                            S.op("dve", lambda e, ub=ub, dst=dst: e.scalar_tensor_tensor(dst, ub[:], 1.0, dst, ALU.add, ALU.mult),
                                 reads=[uk, ak], writes=[ak])
                    if bi == 7:
                        S.op("sp", lambda e: e.dma_start(out=actscr[ex_].rearrange("p (a b) -> p a b", a=KC), in_=actT[:]),
                             reads=[("actT", ab, fc) for fc in range(KC)], writes=[("actscr", ex_)], dma=("actst", ab))
                        if ex_ == E - 1:
                            close_B()
                            open_C(0)
                return consume

            def load_act(hh, ex_):
                actH = SC["actH"][ex_ % 2]
                S.op("sp", lambda e: e.dma_start(out=actH[:], in_=actscr[ex_].rearrange("p (a b) -> p a b", a=KC)[:, :, hh * CAP:(hh + 1) * CAP]),
                     reads=[("actscr", ex_)], writes=[("actH", ex_ % 2)], dma=("actld", ex_ % 2))

            def open_C(hh):
                st = ExitStack()
                SC.clear()
                SC["stack"] = st
                acc = sbn("accC", [128, NTT, D], F32, st)
                SC["acc"] = acc
                SC["actH"] = [sbn("actH", [128, KC, CAP], BF16, st) for _ in range(2)]
                SC["Pw"] = sbn("Pw", [128, NTT, CAP], BF16, st)
                SC["PTw"] = [sbn("PTw", [128, NST * NTT, 128], BF16, st) for _ in range(2)]
                SC["ysb"] = [sbn("ysb", [128, NST, 512], BF16, st) for _ in range(2)]
                SC["lng2"] = sbn("lng2", [128, 512], F32, st)
                SC["lnb2"] = sbn("lnb2", [128, 512], F32, st)
                SC["rot"] = {"Y": 0}
                t0 = hh * HALF
                for tt in range(NTT):
                    S.op("sp", lambda e, tt=tt: e.dma_start(out=acc[:, tt, :], in_=accscr[t0 + tt * 128:t0 + (tt + 1) * 128, :]),
                         reads=[("accscr", hh * NTT + tt)], writes=akeys(tt), dma=("accld", tt % 2))
                load_act(hh, 0)

            def close_C():
                S.barrier()
                SC["stack"].close()

            def expertC_prologue(hh, ex_):
                Pw = SC["Pw"]
                pb = ex_ % 2
                PTw = SC["PTw"][pb]
                if ex_ + 1 < E:
                    load_act(hh, ex_ + 1)
                for tt in range(NTT):
                    gt = hh * NTT + tt
                    S.op("dve", lambda e, tt=tt, gt=gt: e.tensor_scalar(Pw[:, tt, :], iota[:], rank[:, gt, ex_:ex_ + 1], Gt[:, gt, ex_:ex_ + 1],
                                                                       ALU.is_equal, ALU.mult),
                         reads=["iota", ("rank", gt), ("G", gt)], writes=[("Pw", tt)])
                nblk = NST * NTT
                for q in range((nblk + 3) // 4):
                    pk, ps = next_ps()
                    n = min(4, nblk - q * 4)

                    def f_mm(e, q=q, ps=ps, n=n):
                        ins = None
                        for j in range(n):
                            blk = q * 4 + j
                            st_, tt = blk // NTT, blk % NTT
                            ins = e.matmul(ps[:, j * 128:(j + 1) * 128], lhsT=Pw[:, tt, st_ * 128:(st_ + 1) * 128], rhs=identbf[:, :],
                                           start=True, stop=True)
                        return ins
                    S.op("pe", f_mm, reads=[("Pw", tt) for tt in range(NTT)] + ["identbf"], writes=[pk])
                    S.op("act", lambda e, q=q, ps=ps, n=n: e.activation(PTw[:, q * 4:q * 4 + n, :],
                                                                        ps[:, 0:n * 128].rearrange("p (a b) -> p a b", a=n), AF.Identity),
                         reads=[pk], writes=[("PTw", pb)])

            def make_w2(hh, ex_, dg):
                def consume(wbt, wbk):
                    if dg == 0:
                        expertC_prologue(hh, ex_)
                    acc = SC["acc"]
                    actT = SC["actH"][ex_ % 2]
                    pb = ex_ % 2
                    PTw = SC["PTw"][pb]
                    yi = SC["rot"]["Y"] % 2
                    SC["rot"]["Y"] += 1
                    ysb, yk = SC["ysb"][yi], ("ysb", yi)
                    for st_ in range(NST):
                        pk, ps = next_ps()

                        def f_mm(e, st_=st_, ps=ps):
                            ins = None
                            for fc in range(KC):
                                ins = e.matmul(ps[:, :], lhsT=actT[:, fc, st_ * 128:(st_ + 1) * 128], rhs=wbt[:, fc, :],
                                               start=(fc == 0), stop=(fc == KC - 1))
                            return ins
                        S.op("pe", f_mm, reads=[wbk, ("actH", ex_ % 2)], writes=[pk])
                        S.op("act", lambda e, st_=st_, ps=ps: e.activation(ysb[:, st_, :], ps[:, :], AF.Identity), reads=[pk], writes=[yk])
                    for tt in range(NTT):
                        pk, ps = next_ps()

                        def f_sc(e, tt=tt, ps=ps):
                            ins = None
                            for st_ in range(NST):
                                ins = e.matmul(ps[:, :], lhsT=PTw[:, st_ * NTT + tt, :], rhs=ysb[:, st_, :], start=(st_ == 0), stop=(st_ == NST - 1))
                            return ins
                        S.op("pe", f_sc, reads=[yk, ("PTw", pb)], writes=[pk])
                        S.op("dve", lambda e, tt=tt, ps=ps: e.tensor_tensor(acc[:, tt, dg * 512:(dg + 1) * 512], acc[:, tt, dg * 512:(dg + 1) * 512],
                                                                            ps[:, :], ALU.add),
                             reads=[pk, ("acc", tt, dg)], writes=[("acc", tt, dg)])
                    if ex_ == E - 1 and dg == 3:
                        stageC_epilogue(hh)
                return consume

            def stageC_epilogue(hh):
                t0 = hh * HALF
                acc, lng2, lnb2 = SC["acc"], SC["lng2"], SC["lnb2"]
                for tt in range(NTT):
                    ln_tm(acc[:, tt, :], akeys(tt), acc[:, tt, :], akeys(tt), affine=False)
                for dg in range(4):
                    S.op("sp", lambda e, dg=dg: e.dma_start(out=lng2[:], in_=lnv[6:7, dg * 512:(dg + 1) * 512].partition_broadcast(128)),
                         writes=["lng2"], dma="lng2")
                    S.op("sp", lambda e, dg=dg: e.dma_start(out=lnb2[:], in_=lnv[7:8, dg * 512:(dg + 1) * 512].partition_broadcast(128)),
                         writes=["lnb2"], dma="lnb2")
                    for tt in range(NTT):
                        a_ = acc[:, tt, dg * 512:(dg + 1) * 512]
                        S.op("dve", lambda e, a_=a_: e.tensor_tensor(a_, a_, lng2[:], ALU.mult), reads=[("acc", tt, dg), "lng2"], writes=[("acc", tt, dg)])
                        S.op("dve", lambda e, a_=a_: e.tensor_tensor(a_, a_, lnb2[:], ALU.add), reads=[("acc", tt, dg), "lnb2"], writes=[("acc", tt, dg)])
                for tt in range(NTT):
                    S.op("sp", lambda e, tt=tt: e.dma_start(out=out[t0 + tt * 128:t0 + (tt + 1) * 128, :], in_=acc[:, tt, :]),
                         reads=akeys(tt), writes=[("out", hh * NTT + tt)], dma=("ost", tt % 2))
                close_C()
                if hh + 1 < NH:
                    open_C(hh + 1)

            blocks2 = []
            for hh in range(NH):
                for dg in range(4):
                    blocks2.append((wview(w_pg, dg * 512), make_ple(hh, dg)))
            for ex_ in range(E):
                for bi in range(8):
                    blocks2.append((wview(w_e1[ex_], bi * 512), make_w1(ex_, bi)))
            for hh in range(NH):
                for ex_ in range(E):
                    for dg in range(4):
                        blocks2.append((wview(w_e2[ex_], dg * 512), make_w2(hh, ex_, dg)))
            run_stream(blocks2)
            S.op("sp", None, reads=[("out", i) for i in range(NT // 128)])

            S.finalize(top)
            with nc.Block() as block:
                S.emit(block)
    return nc


def _core_inputs(c, NT, E, inp, n_per_batch):
    b = c // n_per_batch
    s0 = (c % n_per_batch) * NT
    f = lambda a: np.ascontiguousarray(a, dtype=np.float32)
    x = inp["x"]
    m = {}
    m["x"] = f(x[b, s0:s0 + NT])
    if s0 == 0:
        m["xhalo"] = np.zeros((HALO, D), np.float32)
        m["hmask"] = np.zeros((128, 1), np.float32)
    else:
        m["xhalo"] = f(x[b, s0 - HALO:s0])
        m["hmask"] = np.ones((128, 1), np.float32)
    m["p"] = f(inp["p"][0, b, s0:s0 + NT])
    m["w_in"] = f(inp["w_in"][0])
    m["w_sgu_out"] = f(inp["w_sgu_out"][0])
    m["w_conv_out"] = f(inp["w_conv_out"][0])
    m["w_o"] = f(inp["w_o"][0])
    m["w_pg"] = f(inp["w_pg"][0])
    m["w_ple"] = f(inp["w_ple"][0])
    m["w_router"] = f(inp["w_router"][0])
    m["w_e1"] = f(inp["w_e1"][0])
    m["w_e2"] = f(inp["w_e2"][0])
    m["lnv"] = f(np.stack([inp["ln0_g"], inp["ln0_b"], inp["sgu_ln_g"][0], inp["sgu_ln_b"][0],
                           inp["ln1_g"][0], inp["ln1_b"][0], inp["ln2_g"][0], inp["ln2_b"][0]], 0))
    fm = lambda v: np.asarray(v).reshape(-1, 128).T
    m["bfm"] = f(np.concatenate([fm(inp["b_in"][0]), fm(inp["b_sgu_out"][0]), fm(inp["b_conv_out"][0]), fm(inp["conv_b"][0]),
                                 fm(inp["conv_ln_g"][0]), fm(inp["conv_ln_b"][0])], axis=1))
    m["convw"] = f(np.asarray(inp["conv_w"][0]).T.reshape(KC, 128, CW).transpose(1, 0, 2).reshape(128, KC * CW))
    m["bv"] = f(np.asarray(inp["b_in"][0])[None, D:2 * D])
    m["bo"] = f(np.asarray(inp["b_o"][0])[None])
    m["bpg"] = f(np.asarray(inp["b_pg"][0])[None])
    m["brt"] = f(np.asarray(inp["b_router"][0])[None])
    m["wsT"] = f(np.asarray(inp["w_s"][0]).transpose(2, 0, 1).reshape(128, 16 * 128))
    m["bs"] = f(np.asarray(inp["b_s"][0]).reshape(1, 16 * 128))
    m["b1"] = f(np.asarray(inp["b_e1"][0]).reshape(E, 32, 128).transpose(2, 0, 1).reshape(128, E * 32))
    m["b2"] = f(inp["b_e2"][0])
    m["ident"] = np.eye(128, dtype=np.float32)
    m["tri"] = np.triu(np.ones((128, 128), np.float32))
    m["iota"] = np.ascontiguousarray(np.broadcast_to(np.arange(512, dtype=np.float32), (128, 512)))
    return m


_NC_CACHE = {}


def kernel(**inputs):
    inp = {k: np.asarray(v) for k, v in inputs.items()}
    B, SEQ, _ = inp["x"].shape
    E = inp["w_router"].shape[-1]
    n_cores = 8
    NT = B * SEQ // n_cores
    n_per_batch = SEQ // NT
    key = (NT, E)
    if key not in _NC_CACHE:
        _NC_CACHE[key] = build_program(NT=NT, E=E, HALF=min(1024, NT))
    nc = _NC_CACHE[key]
    in_maps = [_core_inputs(c, NT, E, inp, n_per_batch) for c in range(n_cores)]
    res = run_bass_kernel_spmd(nc, in_maps, core_ids=list(range(n_cores)))
    outs = [np.asarray(r["out"]) for r in res.results]
    return np.stack(outs, 0).reshape(B, SEQ, D).astype(np.float32)
```

```python
import numpy as np
from contextlib import ExitStack
import concourse.bass as bass
import concourse.mybir as mybir
from concourse.bass_utils import run_bass_kernel_spmd

F32 = mybir.dt.float32
BF16 = mybir.dt.bfloat16
ALU = mybir.AluOpType
AF = mybir.ActivationFunctionType
AX = mybir.AxisListType

D = 2048
KC = 16
DIN = 12288
PLE = 256
CW = 31
HALO = 32
TG = 256
LN_EPS = 1e-5
DN_ALPHA = 2.0 ** 0.25
SW_ALPHA = 1.702
SW_LIM = 7.0
NWB = 3
P1_ORDER = list(range(4, 16)) + list(range(0, 4)) + list(range(16, 36))
CAP = 256


class _Op:
    __slots__ = ("eng", "fn", "deps", "needs_inc", "dma", "sem", "val", "inc")

    def __init__(self, eng, fn, dma):
        self.eng, self.fn, self.dma = eng, fn, dma
        self.deps = []
        self.needs_inc = False
        self.sem = None
        self.val = 0
        self.inc = 1


class Sched:
    ENGS = ("pe", "act", "dve", "pool", "sp")

    def __init__(self, nc):
        self.nc = nc
        self.ops = {e: [] for e in self.ENGS}
        self.res = {}
        self.last_dma = {}
        self.nps = 0

    def op(self, eng, fn, reads=(), writes=(), dma=None):
        o = _Op(eng, fn, dma)
        seen = set()

        def add(d):
            if d is None or id(d) in seen:
                return
            if d.eng == "pe" and eng == "pe" and d.dma is None and dma is None:
                return
            seen.add(id(d))
            o.deps.append(d)

        for k in reads:
            st = self.res.setdefault(k, [None, []])
            add(st[0])
        for k in writes:
            st = self.res.setdefault(k, [None, []])
            add(st[0])
            for r in st[1]:
                add(r)
        if dma is not None:
            add(self.last_dma.get(dma))
            self.last_dma[dma] = o
        for k in reads:
            self.res[k][1].append(o)
        for k in writes:
            self.res[k][0] = o
            self.res[k][1] = []
        self.ops[eng].append(o)
        return o

    def barrier(self):
        keys = list(self.res.keys())
        for e in self.ENGS:
            self.op(e, None, reads=keys)
        for e in self.ENGS:
            self.op(e, None, writes=keys)

    def finalize(self, stack):
        nc = self.nc
        def expand(o, out, seen):
            for d in o.deps:
                if id(d) in seen:
                    continue
                seen.add(id(d))
                if d.fn is None:
                    expand(d, out, seen)
                else:
                    out.append(d)
        for e in self.ENGS:
            for o in self.ops[e]:
                out = []
                expand(o, out, set())
                o.deps = out
        for e in self.ENGS:
            for o in self.ops[e]:
                for d in o.deps:
                    d.needs_inc = True
        esem = {}
        for e in self.ENGS:
            esem[e] = stack.enter_context(nc.semaphore("sem_" + e))
        dsem = {}
        dcnt = {}
        for e in self.ENGS:
            cnt = 0
            for o in self.ops[e]:
                if o.fn is None:
                    continue
                if o.dma is not None:
                    if o.dma not in dsem:
                        dsem[o.dma] = stack.enter_context(nc.semaphore("dsem%d" % len(dsem)))
                        dcnt[o.dma] = 0
                    dcnt[o.dma] += 16
                    o.sem, o.val, o.inc = dsem[o.dma], dcnt[o.dma], 16
                    o.needs_inc = True
                elif o.needs_inc:
                    cnt += 1
                    o.sem, o.val, o.inc = esem[e], cnt, 1
        self.nsem = len(dsem) + len(esem)

    def emit(self, block):
        meth = {"pe": block.tensor, "act": block.scalar, "dve": block.vector, "pool": block.gpsimd, "sp": block.sync}
        for ename in self.ENGS:
            ops = self.ops[ename]

            def body(e, ops=ops):
                waited = {}
                for o in ops:
                    need = {}
                    for d in o.deps:
                        k = id(d.sem)
                        if k not in need or need[k][1] < d.val:
                            need[k] = (d.sem, d.val)
                    for k, (sem, val) in need.items():
                        if waited.get(k, 0) >= val:
                            continue
                        e.wait_ge(sem, val)
                        waited[k] = val
                    if o.fn is None:
                        continue
                    ins = o.fn(e)
                    if o.needs_inc:
                        ins.then_inc(o.sem, o.inc)

            meth[ename](body)


def build_program(NT=2048, E=32, HALF=1024):
    NG = NT // TG
    TT = TG // 128
    nc = bass.Bass("TRN2", target_bir_lowering=False)
    S = Sched(nc)

    def din(name, shape):
        return nc.dram_tensor(name, list(shape), F32, kind="ExternalInput").ap()

    x = din("x", [NT, D])
    xhalo = din("xhalo", [HALO, D])
    hmask_d = din("hmask", [128, 1])
    p_d = din("p", [NT, PLE])
    w_in = din("w_in", [D, DIN])
    w_sgu = din("w_sgu_out", [D, D])
    w_cv = din("w_conv_out", [D, D])
    w_o = din("w_o", [D, D])
    w_pg = din("w_pg", [D, D])
    w_ple = din("w_ple", [PLE, D])
    w_rt = din("w_router", [D, E])
    w_e1 = din("w_e1", [E, D, 2 * D])
    w_e2 = din("w_e2", [E, D, D])
    lnv = din("lnv", [8, D])
    bfm_d = din("bfm", [128, 176])
    convw_d = din("convw", [128, KC * CW])
    bv_d = din("bv", [1, D])
    bo_d = din("bo", [1, D])
    bpg_d = din("bpg", [1, D])
    brt_d = din("brt", [1, E])
    wsT_d = din("wsT", [128, 16 * 128])
    bs_d = din("bs", [1, 16 * 128])
    b1_d = din("b1", [128, E * 32])
    b2_d = din("b2", [E, D])
    ident_d = din("ident", [128, 128])
    tri_d = din("tri", [128, 128])
    iota_d = din("iota", [128, 512])
    out = nc.dram_tensor("out", [NT, D], F32, kind="ExternalOutput").ap()
    h1scr = nc.dram_tensor("h1scr", [NT, D], F32, kind="Internal").ap()
    wscr_t = nc.dram_tensor("wscr", [36, 128, KC * 512], BF16, kind="Internal").ap()
    wscr = [wscr_t[i].rearrange("p (a b) -> p a b", a=KC) for i in range(36)]

    def wview(w2d, c0):
        return w2d.rearrange("(kc p) n -> p kc n", p=128)[:, :, c0:c0 + 512]

    with ExitStack() as top:
        def sb(name, shape, dt, stack=top):
            return stack.enter_context(nc.sbuf_tensor("s_" + name, list(shape), dt))

        pst = [top.enter_context(nc.psum_tensor("ps%d" % i, [128, 512], F32)) for i in range(8)]

        def next_ps():
            i = S.nps % 8
            S.nps += 1
            return ("ps", i), pst[i]

        ident = sb("ident", [128, 128], F32)
        ones0 = sb("ones0", [128, 128], BF16)
        wb = [sb("wb%d" % i, [128, KC, 512], BF16) for i in range(NWB)]
        st6 = sb("st6", [128, 4, 6], F32)
        mv = sb("mv", [128, 2], F32)
        rstd = sb("rstd", [128, 1], F32)

        S.op("sp", lambda e: e.dma_start(out=ident[:], in_=ident_d[:, :]), writes=["ident"], dma="c_ident")
        S.op("dve", lambda e: e.memset(ones0[:], 0.0), writes=["ones0"])
        S.op("dve", lambda e: e.memset(ones0[0:1, :], 1.0), writes=["ones0"])

        LNT = {}

        def load_ln_half(i, hf):
            lng, lnb = LNT["g"], LNT["b"]
            S.op("sp", lambda e: e.dma_start(out=lng[:], in_=lnv[2 * i:2 * i + 1, hf * 1024:(hf + 1) * 1024].partition_broadcast(128)),
                 writes=["lng"], dma="lng")
            S.op("sp", lambda e: e.dma_start(out=lnb[:], in_=lnv[2 * i + 1:2 * i + 2, hf * 1024:(hf + 1) * 1024].partition_broadcast(128)),
                 writes=["lnb"], dma="lnb")

        def ln_group(i, tiles):
            lng, lnb = LNT["g"], LNT["b"]
            for (xt, keys, out_ap, out_keys, np_) in tiles:
                ln_tm(xt, keys, out_ap, out_keys, np_=np_, affine=False)
            for hf in range(2):
                load_ln_half(i, hf)
                for (xt, keys, out_ap, out_keys, np_) in tiles:
                    kh = list(keys[2 * hf:2 * hf + 2])
                    xs = xt[:, hf * 1024:(hf + 1) * 1024]
                    os_ = out_ap[:, hf * 1024:(hf + 1) * 1024]
                    S.op("dve", lambda e, xs=xs, np_=np_: e.tensor_tensor(xs, xs, lng[0:np_, :], ALU.mult), reads=kh + ["lng"], writes=kh)
                    okh = kh if list(out_keys) == list(keys) else list(out_keys)
                    S.op("dve", lambda e, xs=xs, os_=os_, np_=np_: e.tensor_tensor(os_, xs, lnb[0:np_, :], ALU.add),
                         reads=kh + ["lnb"], writes=okh)

        def ln_tm(xt, keys, out_ap, out_keys, np_=128, affine=True):
            def f_stats(e):
                ins = None
                for c in range(4):
                    ins = e.bn_stats(st6[0:np_, c, :], xt[:, c * 512:(c + 1) * 512])
                return ins
            S.op("dve", f_stats, reads=keys, writes=["st6"])
            S.op("dve", lambda e: e.bn_aggr(mv[0:np_, :], st6[0:np_, :, :].rearrange("p a b -> p (a b)")),
                 reads=["st6"], writes=["mv"])
            S.op("dve", lambda e: e.tensor_scalar(rstd[0:np_, :], mv[0:np_, 1:2], LN_EPS, None, ALU.add),
                 reads=["mv"], writes=["rstd"])
            S.op("act", lambda e: e.activation(rstd[0:np_, :], rstd[0:np_, :], AF.Sqrt), reads=["rstd"], writes=["rstd"])
            S.op("dve", lambda e: e.reciprocal(rstd[0:np_, :], rstd[0:np_, :]), reads=["rstd"], writes=["rstd"])
            S.op("dve", lambda e: e.tensor_scalar(xt, xt, mv[0:np_, 0:1], rstd[0:np_, 0:1], ALU.subtract, ALU.mult),
                 reads=list(keys) + ["mv", "rstd"], writes=keys)
            if not affine:
                return
            lng, lnb = LNT["g"], LNT["b"]
            S.op("dve", lambda e: e.tensor_tensor(xt, xt, lng[0:np_, :], ALU.mult), reads=list(keys) + ["lng"], writes=keys)
            S.op("dve", lambda e: e.tensor_tensor(out_ap, xt, lnb[0:np_, :], ALU.add),
                 reads=list(keys) + ["lnb"], writes=out_keys)

        def transpose_tile(src, src_keys, dst, dst_key_fn, tcol, nkc=KC, np_=128):
            per = 512 // np_ if np_ < 128 else 4
            q = 0
            kc = 0
            while kc < nkc:
                n = min(per, nkc - kc)
                pk, ps = next_ps()

                def f_t(e, kc=kc, n=n, ps=ps):
                    ins = None
                    for j in range(n):
                        ins = e.transpose(ps[:, j * np_:(j + 1) * np_], src[:, (kc + j) * 128:(kc + j + 1) * 128],
                                          ident[0:np_, 0:np_])
                    return ins
                S.op("pe", f_t, reads=list(src_keys) + ["ident"], writes=[pk])
                S.op("act", lambda e, kc=kc, n=n, ps=ps: e.activation(
                    dst[:, kc:kc + n, tcol:tcol + np_],
                    ps[:, 0:n * np_].rearrange("p (a b) -> p a b", a=n), AF.Identity),
                    reads=[pk], writes=[dst_key_fn(q)])
                kc += n
                q += 1

        with ExitStack() as p1:
            xh0 = sb("xh0", [128, TT, D], F32, p1)
            h0T = sb("h0T", [128, KC, TG], BF16, p1)
            uT = sb("uT", [128, KC, TG], BF16, p1)
            vtco = sb("vtco", [128, TT * D], F32, p1)
            vt = vtco[:, :].rearrange("p (t d) -> p t d", t=TT)
            co = vtco[:, :].rearrange("p (c n) -> p c n", c=KC)
            LNT["g"] = sb("lng", [128, 1024], F32, p1)
            LNT["b"] = sb("lnb", [128, 1024], F32, p1)
            vn = sb("vn", [128, TT, D], BF16, p1)
            cT = sb("cT", [128, KC, HALO + TG], F32, p1)
            cn = sb("cn", [128, KC, TG], BF16, p1)
            gaT = sb("gaT", [128, KC, TG], BF16, p1)
            gbT = sb("gbT", [128, KC, TG], BF16, p1)
            mT = sb("mT", [128, KC, TG], BF16, p1)
            xh = vtco[0:HALO, 0:D]
            xh_keys = [("vc", i) for i in range(4)]
            h0Th = sb("h0Th", [128, KC, HALO], BF16, p1)
            bfm = sb("bfm", [128, 176], F32, p1)
            convw = sb("convw", [128, KC, CW], F32, p1)
            hmask = sb("hmask", [128, 1], F32, p1)
            bvrow = sb("bvrow", [128, D], BF16, p1)
            borow = sb("borow", [128, D], BF16, p1)
            bsrow = sb("bsrow", [128, 16, 128], BF16, p1)
            wsT = sb("wsT", [128, 16, 128], BF16, p1)
            tri = sb("tri", [128, 128], F32, p1)
            ones32 = sb("ones32", [128, 128], F32, p1)
            sgt = [sb("sgt%d" % i, [128, TG], F32, p1) for i in range(2)]
            sqt = [sb("sqt%d" % i, [128, TG], F32, p1) for i in range(2)]
            sgh = sb("sgh", [128, HALO], F32, p1)
            cmean = sb("cmean", [128, TG], F32, p1)
            cmsq = sb("cmsq", [128, TG], F32, p1)
            crstd = sb("crstd", [128, TG], F32, p1)
            cnb = sb("cnb", [128, TG], F32, p1)

            S.op("sp", lambda e: e.dma_start(out=bfm[:], in_=bfm_d[:, :]), writes=["bfm"], dma="c_bfm")
            S.op("sp", lambda e: e.dma_start(out=convw[:], in_=convw_d.rearrange("p (a b) -> p a b", a=KC)),
                 writes=["convw"], dma="c_convw")
            S.op("sp", lambda e: e.dma_start(out=hmask[:], in_=hmask_d[:, :]), writes=["hmask"], dma="c_hmask")
            S.op("pool", lambda e: e.dma_start(out=wsT[:], in_=wsT_d.rearrange("p (a b) -> p a b", a=16)),
                 writes=["wsT"], dma="c_wsT")
            S.op("sp", lambda e: e.dma_start(out=tri[:], in_=tri_d[:, :]), writes=["tri"], dma="c_tri")
            S.op("dve", lambda e: e.memset(ones32[:], 1.0), writes=["ones32"])
            for (t_, d_, nm) in ((bvrow, bv_d, "bvrow"), (borow, bo_d, "borow")):
                S.op("dve", lambda e, t_=t_: e.memset(t_[:], 0.0), writes=[nm])
                S.op("pool", lambda e, t_=t_, d_=d_: e.dma_start(out=t_[0:1, :], in_=d_[:, :]), writes=[nm], dma="c_" + nm)
            S.op("dve", lambda e: e.memset(bsrow[:], 0.0), writes=["bsrow"])
            S.op("pool", lambda e: e.dma_start(out=bsrow[0:1, :, :], in_=bs_d.rearrange("o (a b) -> o a b", a=16)),
                 writes=["bsrow"], dma="c_bsrow")
            for g16 in range(16):
                S.op("dve", lambda e, g16=g16: e.tensor_tensor(wsT[:, g16, :], wsT[:, g16, :], tri[:], ALU.mult),
                     reads=["wsT", "tri"], writes=["wsT"])

            def xkeys(tt):
                return [("xh0", tt, dg) for dg in range(4)]

            def h0T_keys():
                return [("h0T", tt, q) for tt in range(TT) for q in range(4)]

            def prologue(g):
                tok0 = g * TG
                for tt in range(TT):
                    S.op("sp", lambda e, tt=tt: e.dma_start(out=xh0[:, tt, :], in_=x[tok0 + tt * 128: tok0 + (tt + 1) * 128, :]),
                         writes=xkeys(tt), dma=("x", tt))
                tiles0 = [(xh0[:, tt, :], xkeys(tt), xh0[:, tt, :], xkeys(tt), 128) for tt in range(TT)]
                if g == 0:
                    S.op("sp", lambda e: e.dma_start(out=xh, in_=xhalo[:, :]), writes=xh_keys, dma="c_xh")
                    ln_group(0, [(xh, xh_keys, xh, xh_keys, HALO)] + tiles0)
                    transpose_tile(xh, xh_keys, h0Th, lambda q: "h0Th", 0, np_=HALO)
                else:
                    S.op("dve", lambda e: e.tensor_copy(cT[:, :, 0:HALO], cT[:, :, TG:TG + HALO]),
                         reads=[("cT", c) for c in range(KC)], writes=[("cT", c) for c in range(KC)])
                    ln_group(0, tiles0)
                for tt in range(TT):
                    transpose_tile(xh0[:, tt, :], xkeys(tt), h0T, lambda q, tt=tt: ("h0T", tt, q), tt * 128)

            def fform(wbt, wbk, rhs_fn, rhs_keys, N, evac):
                for j in range(4):
                    pk, ps = next_ps()

                    def f_mm(e, j=j, ps=ps):
                        ins = None
                        for kc in range(KC):
                            ins = e.matmul(ps[:, 0:N], lhsT=wbt[:, kc, j * 128:(j + 1) * 128], rhs=rhs_fn(kc),
                                           start=(kc == 0), stop=(kc == KC - 1))
                        return ins
                    S.op("pe", f_mm, reads=[wbk] + list(rhs_keys), writes=[pk])
                    evac(j, ps, pk)

            def tform(wbt, wbk, lhs_fn, lhs_keys_fn, ntt, bias_rhs, bias_key, evac):
                for tt in range(ntt):
                    pk, ps = next_ps()

                    def f_mm(e, tt=tt, ps=ps):
                        ins = None
                        for kc in range(KC):
                            ins = e.matmul(ps[:, :], lhsT=lhs_fn(kc, tt), rhs=wbt[:, kc, :],
                                           start=(kc == 0), stop=(kc == KC - 1 and bias_rhs is None))
                        if bias_rhs is not None:
                            ins = e.matmul(ps[:, :], lhsT=ones0[:, :], rhs=bias_rhs, start=False, stop=True)
                        return ins
                    rk = [wbk, "ones0"] + list(lhs_keys_fn(tt)) + ([bias_key] if bias_key else [])
                    S.op("pe", f_mm, reads=rk, writes=[pk])
                    evac(tt, ps, pk)

            def conv_chunks(cs):
                for c in cs:
                    S.op("dve", lambda e, c=c: e.tensor_scalar(co[:, c, :], cT[:, c, 2:2 + TG], convw[:, c, 0:1], bfm[:, 128 + c:129 + c],
                                                               ALU.mult, ALU.add),
                         reads=[("cT", c), "convw", "bfm"], writes=[("co", c), ("vc", c // 2)])
                for k in range(1, CW):
                    for c in cs:
                        S.op("dve", lambda e, k=k, c=c: e.scalar_tensor_tensor(co[:, c, :], cT[:, c, 2 + k:2 + k + TG], convw[:, c, k:k + 1],
                                                                                co[:, c, :], ALU.mult, ALU.add),
                             reads=[("cT", c), "convw", ("co", c)], writes=[("co", c)])

            def conv_ln():
                pk1, ps1 = next_ps()
                pk2, ps2 = next_ps()
                for c in range(KC):
                    sq = sqt[c % 2]
                    sk = ("sqt", c % 2)
                    S.op("act", lambda e, c=c, sq=sq: e.activation(sq[:], co[:, c, :], AF.Square), reads=[("co", c)], writes=[sk])
                    S.op("pe", lambda e, c=c: e.matmul(ps1[:, 0:TG], lhsT=ones32[:, :], rhs=co[:, c, :], start=(c == 0), stop=(c == KC - 1)),
                         reads=[("co", c), "ones32"], writes=[pk1])
                    S.op("pe", lambda e, c=c, sq=sq: e.matmul(ps2[:, 0:TG], lhsT=ones32[:, :], rhs=sq[:], start=(c == 0), stop=(c == KC - 1)),
                         reads=[sk, "ones32"], writes=[pk2])
                S.op("dve", lambda e: e.tensor_scalar(cmean[:], ps1[:, 0:TG], 1.0 / D, None, ALU.mult), reads=[pk1], writes=["cmean"])
                S.op("dve", lambda e: e.tensor_tensor(cmsq[:], cmean[:], cmean[:], ALU.mult), reads=["cmean"], writes=["cmsq"])
                S.op("dve", lambda e: e.scalar_tensor_tensor(crstd[:], ps2[:, 0:TG], 1.0 / D, cmsq[:], ALU.mult, ALU.subtract),
                     reads=[pk2, "cmsq"], writes=["crstd"])
                S.op("dve", lambda e: e.tensor_scalar(crstd[:], crstd[:], LN_EPS, None, ALU.add), reads=["crstd"], writes=["crstd"])
                S.op("act", lambda e: e.activation(crstd[:], crstd[:], AF.Sqrt), reads=["crstd"], writes=["crstd"])
                S.op("dve", lambda e: e.reciprocal(crstd[:], crstd[:]), reads=["crstd"], writes=["crstd"])
                S.op("dve", lambda e: e.scalar_tensor_tensor(cnb[:], cmean[:], -1.0, crstd[:], ALU.mult, ALU.mult),
                     reads=["cmean", "crstd"], writes=["cnb"])
                for c in range(KC):
                    S.op("dve", lambda e, c=c: e.tensor_tensor(co[:, c, :], co[:, c, :], crstd[:], ALU.mult),
                         reads=[("co", c), "crstd"], writes=[("co", c)])
                    S.op("dve", lambda e, c=c: e.tensor_tensor(co[:, c, :], co[:, c, :], cnb[:], ALU.add),
                         reads=[("co", c), "cnb"], writes=[("co", c)])
                    S.op("act", lambda e, c=c: e.activation(cn[:, c, :], co[:, c, :], AF.Silu, bias=bfm[:, 160 + c:161 + c],
                                                            scale=bfm[:, 144 + c:145 + c]),
                         reads=[("co", c), "bfm"], writes=[("cn", c), ("vc", c // 2)])

            def spatial():
                for tt in range(TT):
                    for q in range(4):
                        pk, ps = next_ps()

                        def f_mm(e, tt=tt, q=q, ps=ps):
                            ins = None
                            for j in range(4):
                                g16 = 4 * q + j
                                e.matmul(ps[:, j * 128:(j + 1) * 128], lhsT=vn[:, tt, g16 * 128:(g16 + 1) * 128], rhs=wsT[:, g16, :],
                                         start=True, stop=False)
                                ins = e.matmul(ps[:, j * 128:(j + 1) * 128], lhsT=ones0[:, :], rhs=bsrow[:, g16, :], start=False, stop=True)
                            return ins
                        S.op("pe", f_mm, reads=[("vn", tt), "wsT", "bsrow", "ones0"], writes=[pk])
                        uk = [("uT", 4 * q + j) for j in range(4)]
                        S.op("dve", lambda e, tt=tt, q=q, ps=ps: e.tensor_tensor(
                            uT[:, 4 * q:4 * q + 4, tt * 128:(tt + 1) * 128], uT[:, 4 * q:4 * q + 4, tt * 128:(tt + 1) * 128],
                            ps[:, :].rearrange("p (a b) -> p a b", a=4), ALU.mult), reads=[pk] + uk, writes=uk)

            def epilogue(g):
                tok0 = g * TG
                ln_group(2, [(xh0[:, tt, :], xkeys(tt), xh0[:, tt, :], xkeys(tt), 128) for tt in range(TT)])
                for tt in range(TT):
                    S.op("sp", lambda e, tt=tt: e.dma_start(out=h1scr[tok0 + tt * 128: tok0 + (tt + 1) * 128, :], in_=xh0[:, tt, :]),
                         reads=xkeys(tt), writes=[("h1scr", g * TT + tt)], dma=("h1st", tt))

            def make_consume(g, bi):
                def consume(wbt, wbk):
                    if bi == P1_ORDER[0]:
                        prologue(g)
                    h0rhs = lambda kc: h0T[:, kc, :]
                    if bi < 4:
                        def ev(j, ps, pk):
                            c = bi * 4 + j
                            S.op("act", lambda e: e.activation(uT[:, c, :], ps[:, 0:TG], AF.Gelu, bias=bfm[:, c:c + 1]),
                                 reads=[pk, "bfm"], writes=[("uT", c)])
                        fform(wbt, wbk, h0rhs, h0T_keys(), TG, ev)
                    elif bi < 8:
                        dg = bi - 4

                        def ev(tt, ps, pk):
                            S.op("act", lambda e: e.activation(vt[:, tt, dg * 512:(dg + 1) * 512], ps[:, :], AF.Gelu),
                                 reads=[pk], writes=[("vc", tt * 4 + dg)])
                        tform(wbt, wbk, lambda kc, tt: h0T[:, kc, tt * 128:(tt + 1) * 128],
                              lambda tt: [("h0T", tt, q) for q in range(4)], TT, bvrow[:, dg * 512:(dg + 1) * 512], "bvrow", ev)
                        if bi == 7:
                            ln_group(1, [(vt[:, tt, :], [("vc", tt * 4 + d_) for d_ in range(4)], vn[:, tt, :], [("vn", tt)], 128)
                                         for tt in range(TT)])
                    elif bi < 12:
                        def ev(j, ps, pk):
                            c = (bi - 8) * 4 + j
                            S.op("act", lambda e: e.activation(cT[:, c, HALO:HALO + TG], ps[:, 0:TG], AF.Identity, bias=bfm[:, 32 + c:33 + c]),
                                 reads=[pk, "bfm"], writes=[("cT", c)])
                            if g == 0:
                                pk2, ps2 = next_ps()

                                def f_mm(e, ps2=ps2, j=j):
                                    ins = None
                                    for kc in range(KC):
                                        ins = e.matmul(ps2[:, 0:HALO], lhsT=wbt[:, kc, j * 128:(j + 1) * 128], rhs=h0Th[:, kc, :],
                                                       start=(kc == 0), stop=(kc == KC - 1))
                                    return ins
                                S.op("pe", f_mm, reads=[wbk, "h0Th"], writes=[pk2])
                                S.op("act", lambda e, ps2=ps2: e.activation(cT[:, c, 0:HALO], ps2[:, 0:HALO], AF.Identity,
                                                                            bias=bfm[:, 32 + c:33 + c]),
                                     reads=[pk2, "bfm"], writes=[("cT", c)])
                        fform(wbt, wbk, h0rhs, h0T_keys(), TG, ev)
                    elif bi < 16:
                        def ev(j, ps, pk):
                            c = (bi - 12) * 4 + j
                            S.op("act", lambda e: e.activation(mT[:, c, :], ps[:, 0:TG], AF.Sigmoid, bias=bfm[:, 48 + c:49 + c]),
                                 reads=[pk, "bfm"], writes=[("mT", c)])
                            if g == 0:
                                pk2, ps2 = next_ps()

                                def f_mm(e, ps2=ps2, j=j):
                                    ins = None
                                    for kc in range(KC):
                                        ins = e.matmul(ps2[:, 0:HALO], lhsT=wbt[:, kc, j * 128:(j + 1) * 128], rhs=h0Th[:, kc, :],
                                                       start=(kc == 0), stop=(kc == KC - 1))
                                    return ins
                                S.op("pe", f_mm, reads=[wbk, "h0Th"], writes=[pk2])
                                S.op("act", lambda e, ps2=ps2: e.activation(sgh[:], ps2[:, 0:HALO], AF.Sigmoid, bias=bfm[:, 48 + c:49 + c]),
                                     reads=[pk2, "bfm"], writes=["sgh"])
                                S.op("dve", lambda e: e.scalar_tensor_tensor(cT[:, c, 0:HALO], cT[:, c, 0:HALO], hmask[:, 0:1], sgh[:],
                                                                             ALU.mult, ALU.mult),
                                     reads=["sgh", "hmask", ("cT", c)], writes=[("cT", c)])
                        fform(wbt, wbk, h0rhs, h0T_keys(), TG, ev)
                        cs = [(bi - 12) * 4 + j for j in range(4)]
                        for c in cs:
                            S.op("dve", lambda e, c=c: e.tensor_tensor(cT[:, c, HALO:HALO + TG], cT[:, c, HALO:HALO + TG], mT[:, c, :], ALU.mult),
                                 reads=[("mT", c), ("cT", c)], writes=[("cT", c)])
                        conv_chunks(cs)
                    elif bi < 24:
                        def ev(j, ps, pk):
                            c = ((bi - 16) % 4) * 4 + j
                            dst, nm, off = (gaT, "gaT", 64) if bi < 20 else (gbT, "gbT", 80)
                            S.op("act", lambda e: e.activation(dst[:, c, :], ps[:, 0:TG], AF.Sigmoid, bias=bfm[:, off + c:off + c + 1]),
                                 reads=[pk, "bfm"], writes=[(nm, c)])
                        fform(wbt, wbk, h0rhs, h0T_keys(), TG, ev)
                    elif bi < 28:
                        if bi == 24:
                            spatial()
                        def ev(j, ps, pk):
                            c = (bi - 24) * 4 + j
                            S.op("dve", lambda e: e.scalar_tensor_tensor(mT[:, c, :], ps[:, 0:TG], bfm[:, 96 + c:97 + c], gaT[:, c, :],
                                                                         ALU.add, ALU.mult),
                                 reads=[pk, "bfm", ("gaT", c)], writes=[("mT", c)])
                        fform(wbt, wbk, lambda kc: uT[:, kc, :], [("uT", c) for c in range(KC)], TG, ev)
                    elif bi < 32:
                        if bi == 28:
                            conv_ln()
                        def ev(j, ps, pk):
                            c = (bi - 28) * 4 + j
                            sg = sgt[c % 2]
                            sk = ("sgt", c % 2)
                            S.op("dve", lambda e: e.scalar_tensor_tensor(sg[:], ps[:, 0:TG], bfm[:, 112 + c:113 + c], gbT[:, c, :],
                                                                         ALU.add, ALU.mult),
                                 reads=[pk, "bfm", ("gbT", c)], writes=[sk])
                            S.op("dve", lambda e: e.tensor_tensor(mT[:, c, :], mT[:, c, :], sg[:], ALU.add),
                                 reads=[sk, ("mT", c)], writes=[("mT", c)])
                        fform(wbt, wbk, lambda kc: cn[:, kc, :], [("cn", c) for c in range(KC)], TG, ev)
                    else:
                        dg = bi - 32

                        def ev(tt, ps, pk):
                            S.op("dve", lambda e: e.scalar_tensor_tensor(xh0[:, tt, dg * 512:(dg + 1) * 512], xh0[:, tt, dg * 512:(dg + 1) * 512],
                                                                         DN_ALPHA, ps[:, :], ALU.mult, ALU.add),
                                 reads=[pk, ("xh0", tt, dg)], writes=[("xh0", tt, dg)])
                        tform(wbt, wbk, lambda kc, tt: mT[:, kc, tt * 128:(tt + 1) * 128],
                              lambda tt: [("mT", c) for c in range(KC)], TT, borow[:, dg * 512:(dg + 1) * 512], "borow", ev)
                        if bi == 35:
                            epilogue(g)
                return consume

            blocks = []
            for g in range(NG):
                for bi in P1_ORDER:
                    if bi < 24:
                        src = wview(w_in, bi * 512)
                    elif bi < 28:
                        src = wview(w_sgu, (bi - 24) * 512)
                    elif bi < 32:
                        src = wview(w_cv, (bi - 28) * 512)
                    else:
                        src = wview(w_o, (bi - 32) * 512)
                    if NG > 1 and g == 0:
                        blocks.append((src, make_consume(g, bi), (wscr[bi], ("wscr", bi))))
                    elif NG > 1:
                        blocks.append(((wscr[bi], ("wscr", bi)), make_consume(g, bi)))
                    else:
                        blocks.append((src, make_consume(g, bi)))

            def run_stream(blocks):
                def issue(i):
                    src = blocks[i][0]
                    b = i % NWB
                    rk = []
                    if isinstance(src, tuple):
                        src, k_ = src
                        rk = [k_]
                    S.op("pool", lambda e: e.dma_start(out=wb[b][:], in_=src), reads=rk, writes=[("wb", b)], dma=("wb", b))
                    if len(blocks[i]) > 2:
                        dst, dk = blocks[i][2]
                        S.op("sp", lambda e: e.dma_start(out=dst, in_=wb[b][:]), reads=[("wb", b)], writes=[dk], dma=("wst", b))
                for i in range(min(NWB, len(blocks))):
                    issue(i)
                for i in range(len(blocks)):
                    blocks[i][1](wb[i % NWB], ("wb", i % NWB))
                    if i + NWB < len(blocks):
                        issue(i + NWB)

            run_stream(blocks)
            S.barrier()

        NTT = HALF // 128
        NH = NT // HALF
        NTA = NH * NTT
        NST = CAP // 128
        CAPA = NH * CAP
        assert CAPA <= 512
        actscr = nc.dram_tensor("actscr", [E, 128, KC * CAPA], BF16, kind="Internal").ap()
        accscr = nc.dram_tensor("accscr", [NT, D], F32, kind="Internal").ap()
        uniq = [0]

        def sbn(name, shape, dt, stack):
            uniq[0] += 1
            return stack.enter_context(nc.sbuf_tensor("s_%s_%d" % (name, uniq[0]), list(shape), dt))

        with ExitStack() as p2:
            Gt = sb("G", [128, NTA, E], F32, p2)
            Mf = sb("Mf", [128, NTA, E], F32, p2)
            Mbf = sb("Mbf", [128, NTA, E], BF16, p2)
            rank = sb("rank", [128, NTA, E], F32, p2)
            iota = sb("iota", [128, CAP], F32, p2)
            identbf = sb("identbf", [128, 128], BF16, p2)
            allones = sb("allones", [128, 128], BF16, p2)
            striu = sb("striu", [128, 128], BF16, p2)
            trif = sb("trif", [128, 128], F32, p2)
            lg = sb("lg", [128, E], F32, p2)
            ex = sb("ex", [128, E], F32, p2)
            m8 = sb("m8", [128, 8], F32, p2)
            nmx = sb("nmx", [128, 1], F32, p2)
            ssum = sb("ssum", [128, 1], F32, p2)
            b1t = sb("b1t", [128, E, 32], F32, p2)

            S.op("sp", lambda e: e.dma_start(out=iota[:], in_=iota_d[:, 0:CAP]), writes=["iota"], dma="c_iota")
            S.op("sp", lambda e: e.dma_start(out=trif[:], in_=tri_d[:, :]), writes=["trif"], dma="c_trif")
            S.op("sp", lambda e: e.dma_start(out=b1t[:], in_=b1_d.rearrange("p (a b) -> p a b", a=E)), writes=["b1t"], dma="c_b1t")
            S.op("dve", lambda e: e.tensor_copy(identbf[:], ident[:]), reads=["ident"], writes=["identbf"])
            S.op("dve", lambda e: e.memset(allones[:], 1.0), writes=["allones"])
            S.op("dve", lambda e: e.tensor_tensor(striu[:], trif[:], ident[:], ALU.subtract), reads=["trif", "ident"], writes=["striu"])

            def akeys(tt):
                return [("acc", tt, dg) for dg in range(4)]

            SA = {}
            SB = {}
            SC = {}

            def open_A():
                st = ExitStack()
                SA.clear()
                SA["stack"] = st
                SA["acc"] = sbn("accA", [128, NTT, D], F32, st)
                SA["h1T"] = sbn("h1T", [128, KC, HALF], BF16, st)
                SA["pt"] = sbn("pt", [128, PLE], F32, st)
                SA["pT"] = sbn("pT", [128, 2, HALF], BF16, st)
                SA["wple"] = sbn("wple", [128, 2, D], BF16, st)
                SA["wr"] = sbn("wr", [128, KC, E], BF16, st)
                SA["brrow"] = sbn("brrow", [128, E], BF16, st)
                SA["bpgrow"] = sbn("bpgrow", [128, D], BF16, st)
                SA["b2bf"] = sbn("b2bf", [128, D], BF16, st)
                SA["GT"] = sbn("GT", [128, HALF], BF16, st)
                SA["tmp"] = [sbn("ptmp", [128, 512], F32, st) for _ in range(2)]
                wple, wr, brrow, bpgrow, b2bf, GT = (SA[k] for k in ("wple", "wr", "brrow", "bpgrow", "b2bf", "GT"))
                S.op("pool", lambda e: e.dma_start(out=wple[:], in_=w_ple.rearrange("(kc p) n -> p kc n", p=128)), writes=["wple"], dma="c_wple")
                S.op("pool", lambda e: e.dma_start(out=wr[:], in_=w_rt.rearrange("(kc p) n -> p kc n", p=128)), writes=["wr"], dma="c_wr")
                S.op("dve", lambda e: e.memset(brrow[:], 0.0), writes=["brrow"])
                S.op("pool", lambda e: e.dma_start(out=brrow[0:1, :], in_=brt_d[:, :]), writes=["brrow"], dma="c_brrow")
                S.op("dve", lambda e: e.memset(bpgrow[:], 0.0), writes=["bpgrow"])
                S.op("pool", lambda e: e.dma_start(out=bpgrow[0:1, :], in_=bpg_d[:, :]), writes=["bpgrow"], dma="c_bpgrow")
                S.op("dve", lambda e: e.memset(b2bf[:], 0.0), writes=["b2bf"])
                S.op("pool", lambda e: e.dma_start(out=b2bf[0:E, :], in_=b2_d[:, :]), writes=["b2bf"], dma="c_b2bf")
                S.op("dve", lambda e: e.memset(GT[:], 0.0), writes=["GT"])

            def close_A():
                S.barrier()
                SA["stack"].close()

            def stageA_prologue(hh):
                t0 = hh * HALF
                open_A()
                acc, h1T, pt, pT, wr, brrow, b2bf, GT = (SA[k] for k in ("acc", "h1T", "pt", "pT", "wr", "brrow", "b2bf", "GT"))
                for tt in range(NTT):
                    S.op("sp", lambda e, tt=tt: e.dma_start(out=acc[:, tt, :], in_=h1scr[t0 + tt * 128:t0 + (tt + 1) * 128, :]),
                         reads=[("h1scr", hh * NTT + tt)], writes=akeys(tt), dma=("h1ld", tt % 2))
                for tt in range(NTT):
                    gt = hh * NTT + tt
                    transpose_tile(acc[:, tt, :], akeys(tt), h1T, lambda q, tt=tt: ("h1T", tt), tt * 128)
                    S.op("sp", lambda e, tt=tt: e.dma_start(out=pt[:], in_=p_d[t0 + tt * 128:t0 + (tt + 1) * 128, :]),
                         writes=["pt"], dma="pt")
                    transpose_tile(pt, ["pt"], pT, lambda q, tt=tt: ("pT", tt), tt * 128, nkc=2)
                    pk, ps = next_ps()

                    def f_mm(e, tt=tt, ps=ps):
                        for kc in range(KC):
                            e.matmul(ps[:, 0:E], lhsT=h1T[:, kc, tt * 128:(tt + 1) * 128], rhs=wr[:, kc, :], start=(kc == 0), stop=False)
                        return e.matmul(ps[:, 0:E], lhsT=ones0[:, :], rhs=brrow[:, :], start=False, stop=True)
                    S.op("pe", f_mm, reads=[("h1T", tt), "wr", "brrow", "ones0"], writes=[pk])
                    S.op("dve", lambda e, ps=ps: e.tensor_copy(lg[:], ps[:, 0:E]), reads=[pk], writes=["lg"])
                    S.op("dve", lambda e: e.max(m8[:], lg[:]), reads=["lg"], writes=["m8"])
                    S.op("dve", lambda e, gt=gt: e.tensor_scalar(Mf[:, gt, :], lg[:], m8[:, 3:4], None, ALU.is_ge), reads=["lg", "m8"], writes=[("Mf", gt)])
                    S.op("dve", lambda e, gt=gt: e.tensor_copy(Mbf[:, gt, :], Mf[:, gt, :]), reads=[("Mf", gt)], writes=[("Mbf", gt)])
                    S.op("dve", lambda e: e.tensor_scalar(nmx[:], m8[:, 0:1], -1.0, None, ALU.mult), reads=["m8"], writes=["nmx"])
                    S.op("act", lambda e: e.activation(ex[:], lg[:], AF.Exp, bias=nmx[:, 0:1]), reads=["lg", "nmx"], writes=["ex"])
                    S.op("dve", lambda e, gt=gt: e.tensor_tensor(ex[:], ex[:], Mf[:, gt, :], ALU.mult), reads=["ex", ("Mf", gt)], writes=["ex"])
                    S.op("dve", lambda e: e.reduce_sum(ssum[:], ex[:], AX.X), reads=["ex"], writes=["ssum"])
                    S.op("dve", lambda e: e.reciprocal(ssum[:], ssum[:]), reads=["ssum"], writes=["ssum"])
                    S.op("dve", lambda e, gt=gt: e.tensor_scalar(Gt[:, gt, :], ex[:], ssum[:, 0:1], None, ALU.mult),
                         reads=["ex", "ssum"], writes=[("G", gt)])
                    pkr, psr = next_ps()

                    def f_rank(e, tt=tt, psr=psr):
                        ins = None
                        for t2 in range(tt + 1):
                            ins = e.matmul(psr[:, 0:E], lhsT=(allones[:, :] if t2 < tt else striu[:, :]), rhs=Mbf[:, hh * NTT + t2, :],
                                           start=(t2 == 0), stop=(t2 == tt))
                        return ins
                    S.op("pe", f_rank, reads=[("Mbf", hh * NTT + t2) for t2 in range(tt + 1)] + ["allones", "striu"], writes=[pkr])
                    S.op("dve", lambda e, gt=gt, psr=psr: e.tensor_copy(rank[:, gt, :], psr[:, 0:E]), reads=[pkr], writes=[("rank", gt)])
                    pk2, ps2 = next_ps()
                    S.op("pe", lambda e, gt=gt, ps2=ps2: e.transpose(ps2[0:E, 0:128], Gt[:, gt, :], ident[:, :]),
                         reads=[("G", gt), "ident"], writes=[pk2])
                    S.op("act", lambda e, tt=tt, ps2=ps2: e.activation(GT[0:E, tt * 128:(tt + 1) * 128], ps2[0:E, 0:128], AF.Identity),
                         reads=[pk2], writes=[("GT", tt)])
                    S.op("dve", lambda e, tt=tt: e.tensor_scalar(acc[:, tt, :], acc[:, tt, :], DN_ALPHA, None, ALU.mult),
                         reads=akeys(tt), writes=akeys(tt))
                    for dg in range(4):
                        pk3, ps3 = next_ps()
                        S.op("pe", lambda e, tt=tt, dg=dg, ps3=ps3: e.matmul(ps3[:, :], lhsT=GT[:, tt * 128:(tt + 1) * 128],
                                                                             rhs=b2bf[:, dg * 512:(dg + 1) * 512], start=True, stop=True),
                             reads=[("GT", tt), "GT", "b2bf"], writes=[pk3])
                        S.op("dve", lambda e, tt=tt, dg=dg, ps3=ps3: e.tensor_tensor(acc[:, tt, dg * 512:(dg + 1) * 512],
                                                                                    acc[:, tt, dg * 512:(dg + 1) * 512], ps3[:, :], ALU.add),
                             reads=[pk3, ("acc", tt, dg)], writes=[("acc", tt, dg)])

            def make_ple(hh, dg):
                def consume(wbt, wbk):
                    if dg == 0:
                        stageA_prologue(hh)
                    acc, h1T, pT, wple, bpgrow = (SA[k] for k in ("acc", "h1T", "pT", "wple", "bpgrow"))
                    tmps = SA["tmp"]

                    def ev(tt, ps, pk):
                        sg = tmps[tt % 2]
                        sk = ("ptmp", tt % 2)
                        S.op("act", lambda e: e.activation(sg[:], ps[:, :], AF.Sigmoid), reads=[pk], writes=[sk])
                        pk2, ps2 = next_ps()

                        def f_mm(e):
                            e.matmul(ps2[:, :], lhsT=pT[:, 0, tt * 128:(tt + 1) * 128], rhs=wple[:, 0, dg * 512:(dg + 1) * 512], start=True, stop=False)
                            return e.matmul(ps2[:, :], lhsT=pT[:, 1, tt * 128:(tt + 1) * 128], rhs=wple[:, 1, dg * 512:(dg + 1) * 512],
                                            start=False, stop=True)
                        S.op("pe", f_mm, reads=[("pT", tt), "wple"], writes=[pk2])
                        S.op("dve", lambda e: e.tensor_tensor(sg[:], sg[:], ps2[:, :], ALU.mult), reads=[sk, pk2], writes=[sk])
                        S.op("dve", lambda e: e.tensor_tensor(acc[:, tt, dg * 512:(dg + 1) * 512], acc[:, tt, dg * 512:(dg + 1) * 512], sg[:], ALU.add),
                             reads=[sk, ("acc", tt, dg)], writes=[("acc", tt, dg)])
                    tform(wbt, wbk, lambda kc, tt: h1T[:, kc, tt * 128:(tt + 1) * 128], lambda tt: [("h1T", tt)], NTT,
                          bpgrow[:, dg * 512:(dg + 1) * 512], "bpgrow", ev)
                    if dg == 3:
                        t0 = hh * HALF
                        for tt in range(NTT):
                            S.op("sp", lambda e, tt=tt: e.dma_start(out=accscr[t0 + tt * 128:t0 + (tt + 1) * 128, :], in_=acc[:, tt, :]),
                                 reads=akeys(tt), writes=[("accscr", hh * NTT + tt)], dma=("accst", tt % 2))
                        close_A()
                        if hh == NH - 1:
                            open_B()
                return consume

            def open_B():
                st = ExitStack()
                SB.clear()
                SB["stack"] = st
                h1tm = sbn("h1tm", [128, NTA, D], BF16, st)
                SB["h1tm"] = h1tm
                SB["xgT"] = sbn("xgT", [128, KC, CAPA], BF16, st)
                SB["actT"] = [sbn("actT", [128, KC, CAPA], BF16, st) for _ in range(2)]
                SB["Pe"] = [sbn("Pe", [128, NTT, CAP], BF16, st) for _ in range(NH)]
                SB["tA"] = [sbn("tA", [128, CAPA], F32, st) for _ in range(2)]
                SB["tB"] = [sbn("tB", [128, CAPA], F32, st) for _ in range(2)]
                SB["rot"] = {"A": 0, "B": 0}
                for gt in range(NTA):
                    S.op("pool", lambda e, gt=gt: e.dma_start(out=h1tm[:, gt, :], in_=h1scr[gt * 128:(gt + 1) * 128, :]),
                         reads=[("h1scr", gt)], writes=[("h1tm", gt)], dma=("h1tm", gt % 2))

            def close_B():
                S.barrier()
                SB["stack"].close()

            def btmp(kind):
                i = SB["rot"][kind] % 2
                SB["rot"][kind] += 1
                return {"A": SB["tA"], "B": SB["tB"]}[kind][i], ("b" + kind, i)

            def expertB_prologue(ex_):
                xgT, h1tm = SB["xgT"], SB["h1tm"]
                npair = 512 // CAP
                for hh in range(NH):
                    Pe = SB["Pe"][hh]
                    for tt in range(NTT):
                        gt = hh * NTT + tt
                        S.op("dve", lambda e, tt=tt, gt=gt, Pe=Pe: e.tensor_scalar(Pe[:, tt, :], iota[:], rank[:, gt, ex_:ex_ + 1], Mf[:, gt, ex_:ex_ + 1],
                                                                                 ALU.is_equal, ALU.mult),
                             reads=["iota", ("rank", gt), ("Mf", gt)], writes=[("Pe", hh, tt)])
                    for q in range(KC // npair):
                        pk, ps = next_ps()

                        def f_mm(e, q=q, ps=ps, hh=hh, Pe=Pe):
                            ins = None
                            for j in range(npair):
                                dc = q * npair + j
                                for tt in range(NTT):
                                    ins = e.matmul(ps[:, j * CAP:(j + 1) * CAP], lhsT=h1tm[:, hh * NTT + tt, dc * 128:(dc + 1) * 128], rhs=Pe[:, tt, :],
                                                   start=(tt == 0), stop=(tt == NTT - 1))
                            return ins
                        S.op("pe", f_mm, reads=[("Pe", hh, tt) for tt in range(NTT)] + [("h1tm", hh * NTT + tt) for tt in range(NTT)], writes=[pk])
                        S.op("act", lambda e, q=q, ps=ps, hh=hh: e.activation(xgT[:, q * npair:(q + 1) * npair, hh * CAP:(hh + 1) * CAP],
                                                                              ps[:, 0:npair * CAP].rearrange("p (a b) -> p a b", a=npair), AF.Identity),
                             reads=[pk], writes=[("xgT", q, hh)])

            def make_w1(ex_, bi):
                def consume(wbt, wbk):
                    if bi == 0:
                        expertB_prologue(ex_)
                    xgT = SB["xgT"]
                    ab = ex_ % 2
                    actT = SB["actT"][ab]
                    for j in range(4):
                        fc = (bi % 4) * 4 + j
                        pk, ps = next_ps()

                        def f_mm(e, j=j, ps=ps):
                            ins = None
                            for kc in range(KC):
                                ins = e.matmul(ps[:, 0:CAPA], lhsT=wbt[:, kc, j * 128:(j + 1) * 128], rhs=xgT[:, kc, :],
                                               start=(kc == 0), stop=(kc == KC - 1))
                            return ins
                        S.op("pe", f_mm, reads=[wbk] + [("xgT", q, hh) for q in range(KC * CAP // 512) for hh in range(NH)], writes=[pk])
                        dst = actT[:, fc, :]
                        ak = ("actT", ab, fc)
                        if bi < 4:
                            gc, gk = btmp("A")
                            sg, sk = btmp("B")
                            S.op("dve", lambda e, ps=ps, gc=gc, fc=fc: e.tensor_scalar(gc[:], ps[:, 0:CAPA], b1t[:, ex_, fc:fc + 1], SW_LIM,
                                                                                       ALU.add, ALU.min),
                                 reads=[pk, "b1t"], writes=[gk])
                            S.op("act", lambda e, gc=gc, sg=sg: e.activation(sg[:], gc[:], AF.Sigmoid, scale=SW_ALPHA),
                                 reads=[gk], writes=[sk])
                            S.op("dve", lambda e, gc=gc, sg=sg, dst=dst: e.tensor_tensor(dst, gc[:], sg[:], ALU.mult),
                                 reads=[gk, sk], writes=[ak])
                        else:
                            ub, uk = btmp("A")
                            S.op("act", lambda e, ps=ps, ub=ub, fc=fc: e.activation(ub[:], ps[:, 0:CAPA], AF.Identity,
                                                                                    bias=b1t[:, ex_, 16 + fc:17 + fc]),
                                 reads=[pk, "b1t"], writes=[uk])
                            S.op("dve", lambda e, ub=ub: e.tensor_scalar(ub[:], ub[:], SW_LIM, -SW_LIM, ALU.min, ALU.max),
                                 reads=[uk], writes=[uk])
                            S.op("dve", lambda e, ub=ub, dst=dst: e.scalar_tensor_tensor(dst, ub[:], 1.0, dst, ALU.add, ALU.mult),
                                 reads=[uk, ak], writes=[ak])
                    if bi == 7:
                        S.op("sp", lambda e: e.dma_start(out=actscr[ex_].rearrange("p (a b) -> p a b", a=KC), in_=actT[:]),
                             reads=[("actT", ab, fc) for fc in range(KC)], writes=[("actscr", ex_)], dma=("actst", ab))
                        if ex_ == E - 1:
                            close_B()
                            open_C()
                return consume

            ND = 2
            DH = D // ND
            NSA = NH * NST

            def load_act(seq):
                ex_ = seq % E
                actH = SC["actH"][seq % 2]
                S.op("sp", lambda e: e.dma_start(out=actH[:], in_=actscr[ex_].rearrange("p (a b) -> p a b", a=KC)),
                     reads=[("actscr", ex_)], writes=[("actH", seq % 2)], dma=("actld", seq % 2))

            def ckeys(gt):
                return [("acc", gt, dgl) for dgl in range(DH // 512)]

            def load_acc(dh):
                acc = SC["acc"]
                for gt in range(NTA):
                    S.op("sp", lambda e, gt=gt: e.dma_start(out=acc[:, gt, :], in_=accscr[gt * 128:(gt + 1) * 128, dh * DH:(dh + 1) * DH]),
                         reads=[("accscr", gt)], writes=ckeys(gt), dma=("accld", gt % 2))

            def open_C():
                st0 = ExitStack()
                SC.clear()
                SC["stack0"] = st0
                SC["acc"] = sbn("accC", [128, NTA, DH], F32, st0)
                st = ExitStack()
                SC["stack"] = st
                SC["actH"] = [sbn("actH", [128, KC, CAPA], BF16, st) for _ in range(2)]
                SC["Pw"] = sbn("Pw", [128, NTA, CAP], BF16, st)
                SC["PTw"] = [sbn("PTw", [128, NSA * NTT, 128], BF16, st) for _ in range(2)]
                SC["ysb"] = [sbn("ysb", [128, NSA, 512], BF16, st) for _ in range(2)]
                SC["rot"] = {"Y": 0}
                load_acc(0)
                load_act(0)

            def expertC_prologue(dh, ex_):
                Pw = SC["Pw"]
                seq = dh * E + ex_
                pb = seq % 2
                PTw = SC["PTw"][pb]
                if seq + 1 < ND * E:
                    load_act(seq + 1)
                for gt in range(NTA):
                    S.op("dve", lambda e, gt=gt: e.tensor_scalar(Pw[:, gt, :], iota[:], rank[:, gt, ex_:ex_ + 1], Gt[:, gt, ex_:ex_ + 1],
                                                                 ALU.is_equal, ALU.mult),
                         reads=["iota", ("rank", gt), ("G", gt)], writes=[("Pw", gt)])
                nblk = NSA * NTT
                for q in range((nblk + 3) // 4):
                    pk, ps = next_ps()
                    n = min(4, nblk - q * 4)

                    def f_mm(e, q=q, ps=ps, n=n):
                        ins = None
                        for j in range(n):
                            blk = q * 4 + j
                            sa, tt = blk // NTT, blk % NTT
                            hh, st_ = sa // NST, sa % NST
                            ins = e.matmul(ps[:, j * 128:(j + 1) * 128], lhsT=Pw[:, hh * NTT + tt, st_ * 128:(st_ + 1) * 128], rhs=identbf[:, :],
                                           start=True, stop=True)
                        return ins
                    S.op("pe", f_mm, reads=[("Pw", gt) for gt in range(NTA)] + ["identbf"], writes=[pk])
                    S.op("act", lambda e, q=q, ps=ps, n=n: e.activation(PTw[:, q * 4:q * 4 + n, :],
                                                                        ps[:, 0:n * 128].rearrange("p (a b) -> p a b", a=n), AF.Identity),
                         reads=[pk], writes=[("PTw", pb)])

            def make_w2(dh, ex_, dgl):
                def consume(wbt, wbk):
                    if dgl == 0:
                        expertC_prologue(dh, ex_)
                    acc = SC["acc"]
                    seq = dh * E + ex_
                    actT = SC["actH"][seq % 2]
                    pb = seq % 2
                    PTw = SC["PTw"][pb]
                    yi = SC["rot"]["Y"] % 2
                    SC["rot"]["Y"] += 1
                    ysb, yk = SC["ysb"][yi], ("ysb", yi)
                    for sa in range(NSA):
                        pk, ps = next_ps()

                        def f_mm(e, sa=sa, ps=ps):
                            ins = None
                            for fc in range(KC):
                                ins = e.matmul(ps[:, :], lhsT=actT[:, fc, sa * 128:(sa + 1) * 128], rhs=wbt[:, fc, :],
                                               start=(fc == 0), stop=(fc == KC - 1))
                            return ins
                        S.op("pe", f_mm, reads=[wbk, ("actH", seq % 2)], writes=[pk])
                        S.op("act", lambda e, sa=sa, ps=ps: e.activation(ysb[:, sa, :], ps[:, :], AF.Identity), reads=[pk], writes=[yk])
                    for gt in range(NTA):
                        hh, tt = gt // NTT, gt % NTT
                        pk, ps = next_ps()

                        def f_sc(e, hh=hh, tt=tt, ps=ps):
                            ins = None
                            for st_ in range(NST):
                                sa = hh * NST + st_
                                ins = e.matmul(ps[:, :], lhsT=PTw[:, sa * NTT + tt, :], rhs=ysb[:, sa, :], start=(st_ == 0), stop=(st_ == NST - 1))
                            return ins
                        S.op("pe", f_sc, reads=[yk, ("PTw", pb)], writes=[pk])
                        S.op("dve", lambda e, gt=gt, ps=ps: e.tensor_tensor(acc[:, gt, dgl * 512:(dgl + 1) * 512], acc[:, gt, dgl * 512:(dgl + 1) * 512],
                                                                            ps[:, :], ALU.add),
                             reads=[pk, ("acc", gt, dgl)], writes=[("acc", gt, dgl)])
                    if ex_ == E - 1 and dgl == DH // 512 - 1:
                        if dh == 0:
                            for gt in range(NTA):
                                S.op("sp", lambda e, gt=gt: e.dma_start(out=accscr[gt * 128:(gt + 1) * 128, 0:DH], in_=acc[:, gt, :]),
                                     reads=ckeys(gt), writes=[("accscrB", gt)], dma=("accst2", gt % 2))
                            load_acc(1)
                        else:
                            stageC_final()
                return consume

            def stageC_final():
                acc = SC["acc"]
                S.barrier()
                SC["stack"].close()
                st = ExitStack()
                SC["stackF"] = st
                lngF = sbn("lngF", [128, D], F32, st)
                lnbF = sbn("lnbF", [128, D], F32, st)
                oth = [sbn("oth", [128, DH], F32, st) for _ in range(2)]
                S.op("sp", lambda e: e.dma_start(out=lngF[:], in_=lnv[6:7, :].partition_broadcast(128)), writes=["lngF"], dma="c_lngF")
                S.op("sp", lambda e: e.dma_start(out=lnbF[:], in_=lnv[7:8, :].partition_broadcast(128)), writes=["lnbF"], dma="c_lnbF")
                for gt in range(NTA):
                    ob = oth[gt % 2]
                    ok = ("oth", gt % 2)
                    a_ = acc[:, gt, :]
                    S.op("sp", lambda e, gt=gt, ob=ob: e.dma_start(out=ob[:], in_=accscr[gt * 128:(gt + 1) * 128, 0:DH]),
                         reads=[("accscrB", gt)], writes=[ok], dma=("othld", gt % 2))

                    def f_stats(e, ob=ob, a_=a_):
                        e.bn_stats(st6[:, 0, :], ob[:, 0:512])
                        e.bn_stats(st6[:, 1, :], ob[:, 512:1024])
                        e.bn_stats(st6[:, 2, :], a_[:, 0:512])
                        return e.bn_stats(st6[:, 3, :], a_[:, 512:1024])
                    S.op("dve", f_stats, reads=[ok] + ckeys(gt), writes=["st6"])
                    S.op("dve", lambda e: e.bn_aggr(mv[:, :], st6[:, :, :].rearrange("p a b -> p (a b)")), reads=["st6"], writes=["mv"])
                    S.op("dve", lambda e: e.tensor_scalar(rstd[:, :], mv[:, 1:2], LN_EPS, None, ALU.add), reads=["mv"], writes=["rstd"])
                    S.op("act", lambda e: e.activation(rstd[:, :], rstd[:, :], AF.Sqrt), reads=["rstd"], writes=["rstd"])
                    S.op("dve", lambda e: e.reciprocal(rstd[:, :], rstd[:, :]), reads=["rstd"], writes=["rstd"])
                    for (buf, bk, c0) in ((ob[:, :], [ok], 0), (a_, ckeys(gt), DH)):
                        S.op("dve", lambda e, buf=buf: e.tensor_scalar(buf, buf, mv[:, 0:1], rstd[:, 0:1], ALU.subtract, ALU.mult),
                             reads=list(bk) + ["mv", "rstd"], writes=bk)
                        S.op("dve", lambda e, buf=buf, c0=c0: e.tensor_tensor(buf, buf, lngF[:, c0:c0 + DH], ALU.mult), reads=list(bk) + ["lngF"], writes=bk)
                        S.op("dve", lambda e, buf=buf, c0=c0: e.tensor_tensor(buf, buf, lnbF[:, c0:c0 + DH], ALU.add), reads=list(bk) + ["lnbF"], writes=bk)
                    S.op("sp", lambda e, gt=gt, ob=ob: e.dma_start(out=out[gt * 128:(gt + 1) * 128, 0:DH], in_=ob[:]),
                         reads=[ok], writes=[("outA", gt)], dma=("ostA", gt % 2))
                    S.op("sp", lambda e, gt=gt, a_=a_: e.dma_start(out=out[gt * 128:(gt + 1) * 128, DH:D], in_=a_),
                         reads=ckeys(gt), writes=[("out", gt)], dma=("ost", gt % 2))
                SC["stackF"].close()
                SC["stack0"].close()

            blocks2 = []
            for hh in range(NH):
                for dg in range(4):
                    blocks2.append((wview(w_pg, dg * 512), make_ple(hh, dg)))
            for ex_ in range(E):
                for bi in range(8):
                    blocks2.append((wview(w_e1[ex_], bi * 512), make_w1(ex_, bi)))
            for dh in range(ND):
                for ex_ in range(E):
                    for dgl in range(DH // 512):
                        blocks2.append((wview(w_e2[ex_], (dh * (DH // 512) + dgl) * 512), make_w2(dh, ex_, dgl)))
            run_stream(blocks2)
            S.op("sp", None, reads=[("out", i) for i in range(NT // 128)] + [("outA", i) for i in range(NT // 128)])

            S.finalize(top)
            with nc.Block() as block:
                S.emit(block)
    return nc


def _core_inputs(c, NT, E, inp, n_per_batch):
    b = c // n_per_batch
    s0 = (c % n_per_batch) * NT
    f = lambda a: np.ascontiguousarray(a, dtype=np.float32)
    x = inp["x"]
    m = {}
    m["x"] = f(x[b, s0:s0 + NT])
    if s0 == 0:
        m["xhalo"] = np.zeros((HALO, D), np.float32)
        m["hmask"] = np.zeros((128, 1), np.float32)
    else:
        m["xhalo"] = f(x[b, s0 - HALO:s0])
        m["hmask"] = np.ones((128, 1), np.float32)
    m["p"] = f(inp["p"][0, b, s0:s0 + NT])
    m["w_in"] = f(inp["w_in"][0])
    m["w_sgu_out"] = f(inp["w_sgu_out"][0])
    m["w_conv_out"] = f(inp["w_conv_out"][0])
    m["w_o"] = f(inp["w_o"][0])
    m["w_pg"] = f(inp["w_pg"][0])
    m["w_ple"] = f(inp["w_ple"][0])
    m["w_router"] = f(inp["w_router"][0])
    m["w_e1"] = f(inp["w_e1"][0])
    m["w_e2"] = f(inp["w_e2"][0])
    m["lnv"] = f(np.stack([inp["ln0_g"], inp["ln0_b"], inp["sgu_ln_g"][0], inp["sgu_ln_b"][0],
                           inp["ln1_g"][0], inp["ln1_b"][0], inp["ln2_g"][0], inp["ln2_b"][0]], 0))
    fm = lambda v: np.asarray(v).reshape(-1, 128).T
    m["bfm"] = f(np.concatenate([fm(inp["b_in"][0]), fm(inp["b_sgu_out"][0]), fm(inp["b_conv_out"][0]), fm(inp["conv_b"][0]),
                                 fm(inp["conv_ln_g"][0]), fm(inp["conv_ln_b"][0])], axis=1))
    m["convw"] = f(np.asarray(inp["conv_w"][0]).T.reshape(KC, 128, CW).transpose(1, 0, 2).reshape(128, KC * CW))
    m["bv"] = f(np.asarray(inp["b_in"][0])[None, D:2 * D])
    m["bo"] = f(np.asarray(inp["b_o"][0])[None])
    m["bpg"] = f(np.asarray(inp["b_pg"][0])[None])
    m["brt"] = f(np.asarray(inp["b_router"][0])[None])
    m["wsT"] = f(np.asarray(inp["w_s"][0]).transpose(2, 0, 1).reshape(128, 16 * 128))
    m["bs"] = f(np.asarray(inp["b_s"][0]).reshape(1, 16 * 128))
    m["b1"] = f(np.asarray(inp["b_e1"][0]).reshape(E, 32, 128).transpose(2, 0, 1).reshape(128, E * 32))
    m["b2"] = f(inp["b_e2"][0])
    m["ident"] = np.eye(128, dtype=np.float32)
    m["tri"] = np.triu(np.ones((128, 128), np.float32))
    m["iota"] = np.ascontiguousarray(np.broadcast_to(np.arange(512, dtype=np.float32), (128, 512)))
    return m


_NC_CACHE = {}


def kernel(**inputs):
    inp = {k: np.asarray(v) for k, v in inputs.items()}
    B, SEQ, _ = inp["x"].shape
    E = inp["w_router"].shape[-1]
    n_cores = 8
    NT = B * SEQ // n_cores
    n_per_batch = SEQ // NT
    key = (NT, E)
    if key not in _NC_CACHE:
        _NC_CACHE[key] = build_program(NT=NT, E=E, HALF=min(1024, NT))
    nc = _NC_CACHE[key]
    in_maps = [_core_inputs(c, NT, E, inp, n_per_batch) for c in range(n_cores)]
    res = run_bass_kernel_spmd(nc, in_maps, core_ids=list(range(n_cores)))
    outs = [np.asarray(r["out"]) for r in res.results]
    return np.stack(outs, 0).reshape(B, SEQ, D).astype(np.float32)
```

```python
import numpy as np
from contextlib import ExitStack
import concourse.bass as bass
import concourse.mybir as mybir
from concourse.bass_utils import run_bass_kernel_spmd

F32 = mybir.dt.float32
BF16 = mybir.dt.bfloat16
ALU = mybir.AluOpType
AF = mybir.ActivationFunctionType
AX = mybir.AxisListType

D = 2048
KC = 16
DIN = 12288
PLE = 256
CW = 31
HALO = 32
TG = 256
LN_EPS = 1e-5
DN_ALPHA = 2.0 ** 0.25
SW_ALPHA = 1.702
SW_LIM = 7.0
NWB = 3
P1_ORDER = list(range(4, 16)) + list(range(0, 4)) + list(range(16, 36))
CAP = 256


class _Op:
    __slots__ = ("eng", "fn", "deps", "needs_inc", "dma", "sem", "val", "inc")

    def __init__(self, eng, fn, dma):
        self.eng, self.fn, self.dma = eng, fn, dma
        self.deps = []
        self.needs_inc = False
        self.sem = None
        self.val = 0
        self.inc = 1


class Sched:
    ENGS = ("pe", "act", "dve", "pool", "sp")

    def __init__(self, nc):
        self.nc = nc
        self.ops = {e: [] for e in self.ENGS}
        self.res = {}
        self.last_dma = {}
        self.nps = 0

    def op(self, eng, fn, reads=(), writes=(), dma=None):
        o = _Op(eng, fn, dma)
        seen = set()

        def add(d):
            if d is None or id(d) in seen:
                return
            if d.eng == "pe" and eng == "pe" and d.dma is None and dma is None:
                return
            seen.add(id(d))
            o.deps.append(d)

        for k in reads:
            st = self.res.setdefault(k, [None, []])
            add(st[0])
        for k in writes:
            st = self.res.setdefault(k, [None, []])
            add(st[0])
            for r in st[1]:
                add(r)
        if dma is not None:
            add(self.last_dma.get(dma))
            self.last_dma[dma] = o
        for k in reads:
            self.res[k][1].append(o)
        for k in writes:
            self.res[k][0] = o
            self.res[k][1] = []
        self.ops[eng].append(o)
        return o

    def barrier(self):
        keys = list(self.res.keys())
        for e in self.ENGS:
            self.op(e, None, reads=keys)
        for e in self.ENGS:
            self.op(e, None, writes=keys)

    def finalize(self, stack):
        nc = self.nc
        def expand(o, out, seen):
            for d in o.deps:
                if id(d) in seen:
                    continue
                seen.add(id(d))
                if d.fn is None:
                    expand(d, out, seen)
                else:
                    out.append(d)
        for e in self.ENGS:
            for o in self.ops[e]:
                out = []
                expand(o, out, set())
                o.deps = out
        for e in self.ENGS:
            for o in self.ops[e]:
                for d in o.deps:
                    d.needs_inc = True
        esem = {}
        for e in self.ENGS:
            esem[e] = stack.enter_context(nc.semaphore("sem_" + e))
        dsem = {}
        dcnt = {}
        for e in self.ENGS:
            cnt = 0
            for o in self.ops[e]:
                if o.fn is None:
                    continue
                if o.dma is not None:
                    if o.dma not in dsem:
                        dsem[o.dma] = stack.enter_context(nc.semaphore("dsem%d" % len(dsem)))
                        dcnt[o.dma] = 0
                    dcnt[o.dma] += 16
                    o.sem, o.val, o.inc = dsem[o.dma], dcnt[o.dma], 16
                    o.needs_inc = True
                elif o.needs_inc:
                    cnt += 1
                    o.sem, o.val, o.inc = esem[e], cnt, 1
        self.nsem = len(dsem) + len(esem)

    def emit(self, block):
        meth = {"pe": block.tensor, "act": block.scalar, "dve": block.vector, "pool": block.gpsimd, "sp": block.sync}
        for ename in self.ENGS:
            ops = self.ops[ename]

            def body(e, ops=ops):
                waited = {}
                for o in ops:
                    need = {}
                    for d in o.deps:
                        k = id(d.sem)
                        if k not in need or need[k][1] < d.val:
                            need[k] = (d.sem, d.val)
                    for k, (sem, val) in need.items():
                        if waited.get(k, 0) >= val:
                            continue
                        e.wait_ge(sem, val)
                        waited[k] = val
                    if o.fn is None:
                        continue
                    ins = o.fn(e)
                    if o.needs_inc:
                        ins.then_inc(o.sem, o.inc)

            meth[ename](body)


def build_program(NT=2048, E=32, HALF=1024):
    NG = NT // TG
    TT = TG // 128
    nc = bass.Bass("TRN2", target_bir_lowering=False)
    S = Sched(nc)

    def din(name, shape):
        return nc.dram_tensor(name, list(shape), F32, kind="ExternalInput").ap()

    x = din("x", [NT, D])
    xhalo = din("xhalo", [HALO, D])
    hmask_d = din("hmask", [128, 1])
    p_d = din("p", [NT, PLE])
    w_in = din("w_in", [D, DIN])
    w_sgu = din("w_sgu_out", [D, D])
    w_cv = din("w_conv_out", [D, D])
    w_o = din("w_o", [D, D])
    w_pg = din("w_pg", [D, D])
    w_ple = din("w_ple", [PLE, D])
    w_rt = din("w_router", [D, E])
    w_e1 = din("w_e1", [E, D, 2 * D])
    w_e2 = din("w_e2", [E, D, D])
    lnv = din("lnv", [8, D])
    bfm_d = din("bfm", [128, 176])
    convw_d = din("convw", [128, KC * CW])
    bv_d = din("bv", [1, D])
    bo_d = din("bo", [1, D])
    bpg_d = din("bpg", [1, D])
    brt_d = din("brt", [1, E])
    wsT_d = din("wsT", [128, 16 * 128])
    bs_d = din("bs", [1, 16 * 128])
    b1_d = din("b1", [128, E * 32])
    b2_d = din("b2", [E, D])
    ident_d = din("ident", [128, 128])
    tri_d = din("tri", [128, 128])
    iota_d = din("iota", [128, 512])
    out = nc.dram_tensor("out", [NT, D], F32, kind="ExternalOutput").ap()
    h1scr = nc.dram_tensor("h1scr", [NT, D], F32, kind="Internal").ap()
    wscr_t = nc.dram_tensor("wscr", [36, 128, KC * 512], BF16, kind="Internal").ap()
    wscr = [wscr_t[i].rearrange("p (a b) -> p a b", a=KC) for i in range(36)]

    def wview(w2d, c0):
        return w2d.rearrange("(kc p) n -> p kc n", p=128)[:, :, c0:c0 + 512]

    with ExitStack() as top:
        def sb(name, shape, dt, stack=top):
            return stack.enter_context(nc.sbuf_tensor("s_" + name, list(shape), dt))

        pst = [top.enter_context(nc.psum_tensor("ps%d" % i, [128, 512], F32)) for i in range(8)]

        def next_ps():
            i = S.nps % 8
            S.nps += 1
            return ("ps", i), pst[i]

        ident = sb("ident", [128, 128], F32)
        ones0 = sb("ones0", [128, 128], BF16)
        wb = [sb("wb%d" % i, [128, KC, 512], BF16) for i in range(NWB)]
        st6 = sb("st6", [128, 4, 6], F32)
        mv = sb("mv", [128, 2], F32)
        rstd = sb("rstd", [128, 1], F32)

        S.op("sp", lambda e: e.dma_start(out=ident[:], in_=ident_d[:, :]), writes=["ident"], dma="c_ident")
        S.op("dve", lambda e: e.memset(ones0[:], 0.0), writes=["ones0"])
        S.op("dve", lambda e: e.memset(ones0[0:1, :], 1.0), writes=["ones0"])

        LNT = {}

        def load_ln_half(i, hf):
            lng, lnb = LNT["g"], LNT["b"]
            S.op("sp", lambda e: e.dma_start(out=lng[:], in_=lnv[2 * i:2 * i + 1, hf * 1024:(hf + 1) * 1024].partition_broadcast(128)),
                 writes=["lng"], dma="lng")
            S.op("sp", lambda e: e.dma_start(out=lnb[:], in_=lnv[2 * i + 1:2 * i + 2, hf * 1024:(hf + 1) * 1024].partition_broadcast(128)),
                 writes=["lnb"], dma="lnb")

        def ln_group(i, tiles):
            lng, lnb = LNT["g"], LNT["b"]
            for (xt, keys, out_ap, out_keys, np_) in tiles:
                ln_tm(xt, keys, out_ap, out_keys, np_=np_, affine=False)
            for hf in range(2):
                load_ln_half(i, hf)
                for (xt, keys, out_ap, out_keys, np_) in tiles:
                    kh = list(keys[2 * hf:2 * hf + 2])
                    xs = xt[:, hf * 1024:(hf + 1) * 1024]
                    os_ = out_ap[:, hf * 1024:(hf + 1) * 1024]
                    S.op("dve", lambda e, xs=xs, np_=np_: e.tensor_tensor(xs, xs, lng[0:np_, :], ALU.mult), reads=kh + ["lng"], writes=kh)
                    okh = kh if list(out_keys) == list(keys) else list(out_keys)
                    S.op("dve", lambda e, xs=xs, os_=os_, np_=np_: e.tensor_tensor(os_, xs, lnb[0:np_, :], ALU.add),
                         reads=kh + ["lnb"], writes=okh)

        def ln_tm(xt, keys, out_ap, out_keys, np_=128, affine=True):
            def f_stats(e):
                ins = None
                for c in range(4):
                    ins = e.bn_stats(st6[0:np_, c, :], xt[:, c * 512:(c + 1) * 512])
                return ins
            S.op("dve", f_stats, reads=keys, writes=["st6"])
            S.op("dve", lambda e: e.bn_aggr(mv[0:np_, :], st6[0:np_, :, :].rearrange("p a b -> p (a b)")),
                 reads=["st6"], writes=["mv"])
            S.op("dve", lambda e: e.tensor_scalar(rstd[0:np_, :], mv[0:np_, 1:2], LN_EPS, None, ALU.add),
                 reads=["mv"], writes=["rstd"])
            S.op("act", lambda e: e.activation(rstd[0:np_, :], rstd[0:np_, :], AF.Sqrt), reads=["rstd"], writes=["rstd"])
            S.op("dve", lambda e: e.reciprocal(rstd[0:np_, :], rstd[0:np_, :]), reads=["rstd"], writes=["rstd"])
            S.op("dve", lambda e: e.tensor_scalar(xt, xt, mv[0:np_, 0:1], rstd[0:np_, 0:1], ALU.subtract, ALU.mult),
                 reads=list(keys) + ["mv", "rstd"], writes=keys)
            if not affine:
                return
            lng, lnb = LNT["g"], LNT["b"]
            S.op("dve", lambda e: e.tensor_tensor(xt, xt, lng[0:np_, :], ALU.mult), reads=list(keys) + ["lng"], writes=keys)
            S.op("dve", lambda e: e.tensor_tensor(out_ap, xt, lnb[0:np_, :], ALU.add),
                 reads=list(keys) + ["lnb"], writes=out_keys)

        def transpose_tile(src, src_keys, dst, dst_key_fn, tcol, nkc=KC, np_=128):
            per = 512 // np_ if np_ < 128 else 4
            q = 0
            kc = 0
            while kc < nkc:
                n = min(per, nkc - kc)
                pk, ps = next_ps()

                def f_t(e, kc=kc, n=n, ps=ps):
                    ins = None
                    for j in range(n):
                        ins = e.transpose(ps[:, j * np_:(j + 1) * np_], src[:, (kc + j) * 128:(kc + j + 1) * 128],
                                          ident[0:np_, 0:np_])
                    return ins
                S.op("pe", f_t, reads=list(src_keys) + ["ident"], writes=[pk])
                S.op("act", lambda e, kc=kc, n=n, ps=ps: e.activation(
                    dst[:, kc:kc + n, tcol:tcol + np_],
                    ps[:, 0:n * np_].rearrange("p (a b) -> p a b", a=n), AF.Identity),
                    reads=[pk], writes=[dst_key_fn(q)])
                kc += n
                q += 1

        with ExitStack() as p1:
            xh0 = sb("xh0", [128, TT, D], F32, p1)
            h0T = sb("h0T", [128, KC, TG], BF16, p1)
            uT = sb("uT", [128, KC, TG], BF16, p1)
            vtco = sb("vtco", [128, TT * D], F32, p1)
            vt = vtco[:, :].rearrange("p (t d) -> p t d", t=TT)
            co = vtco[:, :].rearrange("p (c n) -> p c n", c=KC)
            LNT["g"] = sb("lng", [128, 1024], F32, p1)
            LNT["b"] = sb("lnb", [128, 1024], F32, p1)
            vn = sb("vn", [128, TT, D], BF16, p1)
            cT = sb("cT", [128, KC, HALO + TG], F32, p1)
            cn = sb("cn", [128, KC, TG], BF16, p1)
            gaT = sb("gaT", [128, KC, TG], BF16, p1)
            gbT = sb("gbT", [128, KC, TG], BF16, p1)
            mT = sb("mT", [128, KC, TG], BF16, p1)
            xh = vtco[0:HALO, 0:D]
            xh_keys = [("vc", i) for i in range(4)]
            h0Th = sb("h0Th", [128, KC, HALO], BF16, p1)
            bfm = sb("bfm", [128, 176], F32, p1)
            convw = sb("convw", [128, KC, CW], F32, p1)
            hmask = sb("hmask", [128, 1], F32, p1)
            bvrow = sb("bvrow", [128, D], BF16, p1)
            borow = sb("borow", [128, D], BF16, p1)
            bsrow = sb("bsrow", [128, 16, 128], BF16, p1)
            wsT = sb("wsT", [128, 16, 128], BF16, p1)
            tri = sb("tri", [128, 128], F32, p1)
            ones32 = sb("ones32", [128, 128], F32, p1)
            sgt = [sb("sgt%d" % i, [128, TG], F32, p1) for i in range(2)]
            sqt = [sb("sqt%d" % i, [128, TG], F32, p1) for i in range(2)]
            sgh = sb("sgh", [128, HALO], F32, p1)
            cmean = sb("cmean", [128, TG], F32, p1)
            cmsq = sb("cmsq", [128, TG], F32, p1)
            crstd = sb("crstd", [128, TG], F32, p1)
            cnb = sb("cnb", [128, TG], F32, p1)

            S.op("sp", lambda e: e.dma_start(out=bfm[:], in_=bfm_d[:, :]), writes=["bfm"], dma="c_bfm")
            S.op("sp", lambda e: e.dma_start(out=convw[:], in_=convw_d.rearrange("p (a b) -> p a b", a=KC)),
                 writes=["convw"], dma="c_convw")
            S.op("sp", lambda e: e.dma_start(out=hmask[:], in_=hmask_d[:, :]), writes=["hmask"], dma="c_hmask")
            S.op("pool", lambda e: e.dma_start(out=wsT[:], in_=wsT_d.rearrange("p (a b) -> p a b", a=16)),
                 writes=["wsT"], dma="c_wsT")
            S.op("sp", lambda e: e.dma_start(out=tri[:], in_=tri_d[:, :]), writes=["tri"], dma="c_tri")
            S.op("dve", lambda e: e.memset(ones32[:], 1.0), writes=["ones32"])
            for (t_, d_, nm) in ((bvrow, bv_d, "bvrow"), (borow, bo_d, "borow")):
                S.op("dve", lambda e, t_=t_: e.memset(t_[:], 0.0), writes=[nm])
                S.op("pool", lambda e, t_=t_, d_=d_: e.dma_start(out=t_[0:1, :], in_=d_[:, :]), writes=[nm], dma="c_" + nm)
            S.op("dve", lambda e: e.memset(bsrow[:], 0.0), writes=["bsrow"])
            S.op("pool", lambda e: e.dma_start(out=bsrow[0:1, :, :], in_=bs_d.rearrange("o (a b) -> o a b", a=16)),
                 writes=["bsrow"], dma="c_bsrow")
            for g16 in range(16):
                S.op("dve", lambda e, g16=g16: e.tensor_tensor(wsT[:, g16, :], wsT[:, g16, :], tri[:], ALU.mult),
                     reads=["wsT", "tri"], writes=["wsT"])

            def xkeys(tt):
                return [("xh0", tt, dg) for dg in range(4)]

            def h0T_keys():
                return [("h0T", tt, q) for tt in range(TT) for q in range(4)]

            def prologue(g):
                tok0 = g * TG
                for tt in range(TT):
                    S.op("sp", lambda e, tt=tt: e.dma_start(out=xh0[:, tt, :], in_=x[tok0 + tt * 128: tok0 + (tt + 1) * 128, :]),
                         writes=xkeys(tt), dma=("x", tt))
                tiles0 = [(xh0[:, tt, :], xkeys(tt), xh0[:, tt, :], xkeys(tt), 128) for tt in range(TT)]
                if g == 0:
                    S.op("sp", lambda e: e.dma_start(out=xh, in_=xhalo[:, :]), writes=xh_keys, dma="c_xh")
                    ln_group(0, [(xh, xh_keys, xh, xh_keys, HALO)] + tiles0)
                    transpose_tile(xh, xh_keys, h0Th, lambda q: "h0Th", 0, np_=HALO)
                else:
                    S.op("dve", lambda e: e.tensor_copy(cT[:, :, 0:HALO], cT[:, :, TG:TG + HALO]),
                         reads=[("cT", c) for c in range(KC)], writes=[("cT", c) for c in range(KC)])
                    ln_group(0, tiles0)
                for tt in range(TT):
                    transpose_tile(xh0[:, tt, :], xkeys(tt), h0T, lambda q, tt=tt: ("h0T", tt, q), tt * 128)

            def fform(wbt, wbk, rhs_fn, rhs_keys, N, evac):
                for j in range(4):
                    pk, ps = next_ps()

                    def f_mm(e, j=j, ps=ps):
                        ins = None
                        for kc in range(KC):
                            ins = e.matmul(ps[:, 0:N], lhsT=wbt[:, kc, j * 128:(j + 1) * 128], rhs=rhs_fn(kc),
                                           start=(kc == 0), stop=(kc == KC - 1))
                        return ins
                    S.op("pe", f_mm, reads=[wbk] + list(rhs_keys), writes=[pk])
                    evac(j, ps, pk)

            def tform(wbt, wbk, lhs_fn, lhs_keys_fn, ntt, bias_rhs, bias_key, evac):
                for tt in range(ntt):
                    pk, ps = next_ps()

                    def f_mm(e, tt=tt, ps=ps):
                        ins = None
                        for kc in range(KC):
                            ins = e.matmul(ps[:, :], lhsT=lhs_fn(kc, tt), rhs=wbt[:, kc, :],
                                           start=(kc == 0), stop=(kc == KC - 1 and bias_rhs is None))
                        if bias_rhs is not None:
                            ins = e.matmul(ps[:, :], lhsT=ones0[:, :], rhs=bias_rhs, start=False, stop=True)
                        return ins
                    rk = [wbk, "ones0"] + list(lhs_keys_fn(tt)) + ([bias_key] if bias_key else [])
                    S.op("pe", f_mm, reads=rk, writes=[pk])
                    evac(tt, ps, pk)

            def conv_chunks(cs):
                for c in cs:
                    S.op("dve", lambda e, c=c: e.tensor_scalar(co[:, c, :], cT[:, c, 2:2 + TG], convw[:, c, 0:1], bfm[:, 128 + c:129 + c],
                                                               ALU.mult, ALU.add),
                         reads=[("cT", c), "convw", "bfm"], writes=[("co", c), ("vc", c // 2)])
                for k in range(1, CW):
                    for c in cs:
                        S.op("dve", lambda e, k=k, c=c: e.scalar_tensor_tensor(co[:, c, :], cT[:, c, 2 + k:2 + k + TG], convw[:, c, k:k + 1],
                                                                                co[:, c, :], ALU.mult, ALU.add),
                             reads=[("cT", c), "convw", ("co", c)], writes=[("co", c)])

            def conv_ln():
                pk1, ps1 = next_ps()
                pk2, ps2 = next_ps()
                for c in range(KC):
                    sq = sqt[c % 2]
                    sk = ("sqt", c % 2)
                    S.op("act", lambda e, c=c, sq=sq: e.activation(sq[:], co[:, c, :], AF.Square), reads=[("co", c)], writes=[sk])
                    S.op("pe", lambda e, c=c: e.matmul(ps1[:, 0:TG], lhsT=ones32[:, :], rhs=co[:, c, :], start=(c == 0), stop=(c == KC - 1)),
                         reads=[("co", c), "ones32"], writes=[pk1])
                    S.op("pe", lambda e, c=c, sq=sq: e.matmul(ps2[:, 0:TG], lhsT=ones32[:, :], rhs=sq[:], start=(c == 0), stop=(c == KC - 1)),
                         reads=[sk, "ones32"], writes=[pk2])
                S.op("dve", lambda e: e.tensor_scalar(cmean[:], ps1[:, 0:TG], 1.0 / D, None, ALU.mult), reads=[pk1], writes=["cmean"])
                S.op("dve", lambda e: e.tensor_tensor(cmsq[:], cmean[:], cmean[:], ALU.mult), reads=["cmean"], writes=["cmsq"])
                S.op("dve", lambda e: e.scalar_tensor_tensor(crstd[:], ps2[:, 0:TG], 1.0 / D, cmsq[:], ALU.mult, ALU.subtract),
                     reads=[pk2, "cmsq"], writes=["crstd"])
                S.op("dve", lambda e: e.tensor_scalar(crstd[:], crstd[:], LN_EPS, None, ALU.add), reads=["crstd"], writes=["crstd"])
                S.op("act", lambda e: e.activation(crstd[:], crstd[:], AF.Sqrt), reads=["crstd"], writes=["crstd"])
                S.op("dve", lambda e: e.reciprocal(crstd[:], crstd[:]), reads=["crstd"], writes=["crstd"])
                S.op("dve", lambda e: e.scalar_tensor_tensor(cnb[:], cmean[:], -1.0, crstd[:], ALU.mult, ALU.mult),
                     reads=["cmean", "crstd"], writes=["cnb"])
                for c in range(KC):
                    S.op("dve", lambda e, c=c: e.tensor_tensor(co[:, c, :], co[:, c, :], crstd[:], ALU.mult),
                         reads=[("co", c), "crstd"], writes=[("co", c)])
                    S.op("dve", lambda e, c=c: e.tensor_tensor(co[:, c, :], co[:, c, :], cnb[:], ALU.add),
                         reads=[("co", c), "cnb"], writes=[("co", c)])
                    S.op("act", lambda e, c=c: e.activation(cn[:, c, :], co[:, c, :], AF.Silu, bias=bfm[:, 160 + c:161 + c],
                                                            scale=bfm[:, 144 + c:145 + c]),
                         reads=[("co", c), "bfm"], writes=[("cn", c), ("vc", c // 2)])

            def spatial():
                for tt in range(TT):
                    for q in range(4):
                        pk, ps = next_ps()

                        def f_mm(e, tt=tt, q=q, ps=ps):
                            ins = None
                            for j in range(4):
                                g16 = 4 * q + j
                                e.matmul(ps[:, j * 128:(j + 1) * 128], lhsT=vn[:, tt, g16 * 128:(g16 + 1) * 128], rhs=wsT[:, g16, :],
                                         start=True, stop=False)
                                ins = e.matmul(ps[:, j * 128:(j + 1) * 128], lhsT=ones0[:, :], rhs=bsrow[:, g16, :], start=False, stop=True)
                            return ins
                        S.op("pe", f_mm, reads=[("vn", tt), "wsT", "bsrow", "ones0"], writes=[pk])
                        uk = [("uT", 4 * q + j) for j in range(4)]
                        S.op("dve", lambda e, tt=tt, q=q, ps=ps: e.tensor_tensor(
                            uT[:, 4 * q:4 * q + 4, tt * 128:(tt + 1) * 128], uT[:, 4 * q:4 * q + 4, tt * 128:(tt + 1) * 128],
                            ps[:, :].rearrange("p (a b) -> p a b", a=4), ALU.mult), reads=[pk] + uk, writes=uk)

            def epilogue(g):
                tok0 = g * TG
                ln_group(2, [(xh0[:, tt, :], xkeys(tt), xh0[:, tt, :], xkeys(tt), 128) for tt in range(TT)])
                for tt in range(TT):
                    S.op("sp", lambda e, tt=tt: e.dma_start(out=h1scr[tok0 + tt * 128: tok0 + (tt + 1) * 128, :], in_=xh0[:, tt, :]),
                         reads=xkeys(tt), writes=[("h1scr", g * TT + tt)], dma=("h1st", tt))

            def make_consume(g, bi):
                def consume(wbt, wbk):
                    if bi == P1_ORDER[0]:
                        prologue(g)
                    h0rhs = lambda kc: h0T[:, kc, :]
                    if bi < 4:
                        def ev(j, ps, pk):
                            c = bi * 4 + j
                            S.op("act", lambda e: e.activation(uT[:, c, :], ps[:, 0:TG], AF.Gelu, bias=bfm[:, c:c + 1]),
                                 reads=[pk, "bfm"], writes=[("uT", c)])
                        fform(wbt, wbk, h0rhs, h0T_keys(), TG, ev)
                    elif bi < 8:
                        dg = bi - 4

                        def ev(tt, ps, pk):
                            S.op("act", lambda e: e.activation(vt[:, tt, dg * 512:(dg + 1) * 512], ps[:, :], AF.Gelu),
                                 reads=[pk], writes=[("vc", tt * 4 + dg)])
                        tform(wbt, wbk, lambda kc, tt: h0T[:, kc, tt * 128:(tt + 1) * 128],
                              lambda tt: [("h0T", tt, q) for q in range(4)], TT, bvrow[:, dg * 512:(dg + 1) * 512], "bvrow", ev)
                        if bi == 7:
                            ln_group(1, [(vt[:, tt, :], [("vc", tt * 4 + d_) for d_ in range(4)], vn[:, tt, :], [("vn", tt)], 128)
                                         for tt in range(TT)])
                    elif bi < 12:
                        def ev(j, ps, pk):
                            c = (bi - 8) * 4 + j
                            S.op("act", lambda e: e.activation(cT[:, c, HALO:HALO + TG], ps[:, 0:TG], AF.Identity, bias=bfm[:, 32 + c:33 + c]),
                                 reads=[pk, "bfm"], writes=[("cT", c)])
                            if g == 0:
                                pk2, ps2 = next_ps()

                                def f_mm(e, ps2=ps2, j=j):
                                    ins = None
                                    for kc in range(KC):
                                        ins = e.matmul(ps2[:, 0:HALO], lhsT=wbt[:, kc, j * 128:(j + 1) * 128], rhs=h0Th[:, kc, :],
                                                       start=(kc == 0), stop=(kc == KC - 1))
                                    return ins
                                S.op("pe", f_mm, reads=[wbk, "h0Th"], writes=[pk2])
                                S.op("act", lambda e, ps2=ps2: e.activation(cT[:, c, 0:HALO], ps2[:, 0:HALO], AF.Identity,
                                                                            bias=bfm[:, 32 + c:33 + c]),
                                     reads=[pk2, "bfm"], writes=[("cT", c)])
                        fform(wbt, wbk, h0rhs, h0T_keys(), TG, ev)
                    elif bi < 16:
                        def ev(j, ps, pk):
                            c = (bi - 12) * 4 + j
                            S.op("act", lambda e: e.activation(mT[:, c, :], ps[:, 0:TG], AF.Sigmoid, bias=bfm[:, 48 + c:49 + c]),
                                 reads=[pk, "bfm"], writes=[("mT", c)])
                            if g == 0:
                                pk2, ps2 = next_ps()

                                def f_mm(e, ps2=ps2, j=j):
                                    ins = None
                                    for kc in range(KC):
                                        ins = e.matmul(ps2[:, 0:HALO], lhsT=wbt[:, kc, j * 128:(j + 1) * 128], rhs=h0Th[:, kc, :],
                                                       start=(kc == 0), stop=(kc == KC - 1))
                                    return ins
                                S.op("pe", f_mm, reads=[wbk, "h0Th"], writes=[pk2])
                                S.op("act", lambda e, ps2=ps2: e.activation(sgh[:], ps2[:, 0:HALO], AF.Sigmoid, bias=bfm[:, 48 + c:49 + c]),
                                     reads=[pk2, "bfm"], writes=["sgh"])
                                S.op("dve", lambda e: e.scalar_tensor_tensor(cT[:, c, 0:HALO], cT[:, c, 0:HALO], hmask[:, 0:1], sgh[:],
                                                                             ALU.mult, ALU.mult),
                                     reads=["sgh", "hmask", ("cT", c)], writes=[("cT", c)])
                        fform(wbt, wbk, h0rhs, h0T_keys(), TG, ev)
                        cs = [(bi - 12) * 4 + j for j in range(4)]
                        for c in cs:
                            S.op("dve", lambda e, c=c: e.tensor_tensor(cT[:, c, HALO:HALO + TG], cT[:, c, HALO:HALO + TG], mT[:, c, :], ALU.mult),
                                 reads=[("mT", c), ("cT", c)], writes=[("cT", c)])
                        conv_chunks(cs)
                    elif bi < 24:
                        def ev(j, ps, pk):
                            c = ((bi - 16) % 4) * 4 + j
                            dst, nm, off = (gaT, "gaT", 64) if bi < 20 else (gbT, "gbT", 80)
                            S.op("act", lambda e: e.activation(dst[:, c, :], ps[:, 0:TG], AF.Sigmoid, bias=bfm[:, off + c:off + c + 1]),
                                 reads=[pk, "bfm"], writes=[(nm, c)])
                        fform(wbt, wbk, h0rhs, h0T_keys(), TG, ev)
                    elif bi < 28:
                        if bi == 24:
                            spatial()
                        def ev(j, ps, pk):
                            c = (bi - 24) * 4 + j
                            S.op("dve", lambda e: e.scalar_tensor_tensor(mT[:, c, :], ps[:, 0:TG], bfm[:, 96 + c:97 + c], gaT[:, c, :],
                                                                         ALU.add, ALU.mult),
                                 reads=[pk, "bfm", ("gaT", c)], writes=[("mT", c)])
                        fform(wbt, wbk, lambda kc: uT[:, kc, :], [("uT", c) for c in range(KC)], TG, ev)
                    elif bi < 32:
                        if bi == 28:
                            conv_ln()
                        def ev(j, ps, pk):
                            c = (bi - 28) * 4 + j
                            sg = sgt[c % 2]
                            sk = ("sgt", c % 2)
                            S.op("dve", lambda e: e.scalar_tensor_tensor(sg[:], ps[:, 0:TG], bfm[:, 112 + c:113 + c], gbT[:, c, :],
                                                                         ALU.add, ALU.mult),
                                 reads=[pk, "bfm", ("gbT", c)], writes=[sk])
                            S.op("dve", lambda e: e.tensor_tensor(mT[:, c, :], mT[:, c, :], sg[:], ALU.add),
                                 reads=[sk, ("mT", c)], writes=[("mT", c)])
                        fform(wbt, wbk, lambda kc: cn[:, kc, :], [("cn", c) for c in range(KC)], TG, ev)
                    else:
                        dg = bi - 32

                        def ev(tt, ps, pk):
                            S.op("dve", lambda e: e.scalar_tensor_tensor(xh0[:, tt, dg * 512:(dg + 1) * 512], xh0[:, tt, dg * 512:(dg + 1) * 512],
                                                                         DN_ALPHA, ps[:, :], ALU.mult, ALU.add),
                                 reads=[pk, ("xh0", tt, dg)], writes=[("xh0", tt, dg)])
                        tform(wbt, wbk, lambda kc, tt: mT[:, kc, tt * 128:(tt + 1) * 128],
                              lambda tt: [("mT", c) for c in range(KC)], TT, borow[:, dg * 512:(dg + 1) * 512], "borow", ev)
                        if bi == 35:
                            epilogue(g)
                return consume

            blocks = []
            for g in range(NG):
                for bi in P1_ORDER:
                    if bi < 24:
                        src = wview(w_in, bi * 512)
                    elif bi < 28:
                        src = wview(w_sgu, (bi - 24) * 512)
                    elif bi < 32:
                        src = wview(w_cv, (bi - 28) * 512)
                    else:
                        src = wview(w_o, (bi - 32) * 512)
                    if NG > 1 and g == 0:
                        blocks.append((src, make_consume(g, bi), (wscr[bi], ("wscr", bi))))
                    elif NG > 1:
                        blocks.append(((wscr[bi], ("wscr", bi)), make_consume(g, bi)))
                    else:
                        blocks.append((src, make_consume(g, bi)))

            def run_stream(blocks):
                def issue(i):
                    src = blocks[i][0]
                    b = i % NWB
                    rk = []
                    if isinstance(src, tuple):
                        src, k_ = src
                        rk = [k_]
                    S.op("pool", lambda e: e.dma_start(out=wb[b][:], in_=src), reads=rk, writes=[("wb", b)], dma=("wb", b))
                    if len(blocks[i]) > 2:
                        dst, dk = blocks[i][2]
                        S.op("sp", lambda e: e.dma_start(out=dst, in_=wb[b][:]), reads=[("wb", b)], writes=[dk], dma=("wst", b))
                for i in range(min(NWB, len(blocks))):
                    issue(i)
                for i in range(len(blocks)):
                    blocks[i][1](wb[i % NWB], ("wb", i % NWB))
                    if i + NWB < len(blocks):
                        issue(i + NWB)

            run_stream(blocks)
            S.barrier()

        NTT = HALF // 128
        NH = NT // HALF
        NTA = NH * NTT
        NST = CAP // 128
        CAPB = 192
        CAPA = NH * CAPB
        assert CAPA <= 512
        actscr = nc.dram_tensor("actscr", [E, 128, KC * CAPA], BF16, kind="Internal").ap()
        accscr = nc.dram_tensor("accscr", [NT, D], F32, kind="Internal").ap()
        uniq = [0]

        def sbn(name, shape, dt, stack):
            uniq[0] += 1
            return stack.enter_context(nc.sbuf_tensor("s_%s_%d" % (name, uniq[0]), list(shape), dt))

        with ExitStack() as p2:
            Gt = sb("G", [128, NTA, E], F32, p2)
            Mf = sb("Mf", [128, NTA, E], F32, p2)
            Mbf = sb("Mbf", [128, NTA, E], BF16, p2)
            rank = sb("rank", [128, NTA, E], F32, p2)
            iota = sb("iota", [128, CAP], F32, p2)
            identbf = sb("identbf", [128, 128], BF16, p2)
            allones = sb("allones", [128, 128], BF16, p2)
            striu = sb("striu", [128, 128], BF16, p2)
            trif = sb("trif", [128, 128], F32, p2)
            lg = sb("lg", [128, E], F32, p2)
            ex = sb("ex", [128, E], F32, p2)
            m8 = sb("m8", [128, 8], F32, p2)
            nmx = sb("nmx", [128, 1], F32, p2)
            ssum = sb("ssum", [128, 1], F32, p2)
            b1t = sb("b1t", [128, E, 32], F32, p2)

            S.op("sp", lambda e: e.dma_start(out=iota[:], in_=iota_d[:, 0:CAP]), writes=["iota"], dma="c_iota")
            S.op("sp", lambda e: e.dma_start(out=trif[:], in_=tri_d[:, :]), writes=["trif"], dma="c_trif")
            S.op("sp", lambda e: e.dma_start(out=b1t[:], in_=b1_d.rearrange("p (a b) -> p a b", a=E)), writes=["b1t"], dma="c_b1t")
            S.op("dve", lambda e: e.tensor_copy(identbf[:], ident[:]), reads=["ident"], writes=["identbf"])
            S.op("dve", lambda e: e.memset(allones[:], 1.0), writes=["allones"])
            S.op("dve", lambda e: e.tensor_tensor(striu[:], trif[:], ident[:], ALU.subtract), reads=["trif", "ident"], writes=["striu"])

            def akeys(tt):
                return [("acc", tt, dg) for dg in range(4)]

            SA = {}
            SB = {}
            SC = {}

            def open_A():
                st = ExitStack()
                SA.clear()
                SA["stack"] = st
                SA["acc"] = sbn("accA", [128, NTT, D], F32, st)
                SA["h1T"] = sbn("h1T", [128, KC, HALF], BF16, st)
                SA["pt"] = sbn("pt", [128, PLE], F32, st)
                SA["pT"] = sbn("pT", [128, 2, HALF], BF16, st)
                SA["wple"] = sbn("wple", [128, 2, D], BF16, st)
                SA["wr"] = sbn("wr", [128, KC, E], BF16, st)
                SA["brrow"] = sbn("brrow", [128, E], BF16, st)
                SA["bpgrow"] = sbn("bpgrow", [128, D], BF16, st)
                SA["b2bf"] = sbn("b2bf", [128, D], BF16, st)
                SA["GT"] = sbn("GT", [128, HALF], BF16, st)
                SA["tmp"] = [sbn("ptmp", [128, 512], F32, st) for _ in range(2)]
                wple, wr, brrow, bpgrow, b2bf, GT = (SA[k] for k in ("wple", "wr", "brrow", "bpgrow", "b2bf", "GT"))
                S.op("pool", lambda e: e.dma_start(out=wple[:], in_=w_ple.rearrange("(kc p) n -> p kc n", p=128)), writes=["wple"], dma="c_wple")
                S.op("pool", lambda e: e.dma_start(out=wr[:], in_=w_rt.rearrange("(kc p) n -> p kc n", p=128)), writes=["wr"], dma="c_wr")
                S.op("dve", lambda e: e.memset(brrow[:], 0.0), writes=["brrow"])
                S.op("pool", lambda e: e.dma_start(out=brrow[0:1, :], in_=brt_d[:, :]), writes=["brrow"], dma="c_brrow")
                S.op("dve", lambda e: e.memset(bpgrow[:], 0.0), writes=["bpgrow"])
                S.op("pool", lambda e: e.dma_start(out=bpgrow[0:1, :], in_=bpg_d[:, :]), writes=["bpgrow"], dma="c_bpgrow")
                S.op("dve", lambda e: e.memset(b2bf[:], 0.0), writes=["b2bf"])
                S.op("pool", lambda e: e.dma_start(out=b2bf[0:E, :], in_=b2_d[:, :]), writes=["b2bf"], dma="c_b2bf")
                S.op("dve", lambda e: e.memset(GT[:], 0.0), writes=["GT"])

            def close_A():
                S.barrier()
                SA["stack"].close()

            def stageA_prologue(hh):
                t0 = hh * HALF
                open_A()
                acc, h1T, pt, pT, wr, brrow, b2bf, GT = (SA[k] for k in ("acc", "h1T", "pt", "pT", "wr", "brrow", "b2bf", "GT"))
                for tt in range(NTT):
                    S.op("sp", lambda e, tt=tt: e.dma_start(out=acc[:, tt, :], in_=h1scr[t0 + tt * 128:t0 + (tt + 1) * 128, :]),
                         reads=[("h1scr", hh * NTT + tt)], writes=akeys(tt), dma=("h1ld", tt % 2))
                for tt in range(NTT):
                    gt = hh * NTT + tt
                    transpose_tile(acc[:, tt, :], akeys(tt), h1T, lambda q, tt=tt: ("h1T", tt), tt * 128)
                    S.op("sp", lambda e, tt=tt: e.dma_start(out=pt[:], in_=p_d[t0 + tt * 128:t0 + (tt + 1) * 128, :]),
                         writes=["pt"], dma="pt")
                    transpose_tile(pt, ["pt"], pT, lambda q, tt=tt: ("pT", tt), tt * 128, nkc=2)
                    pk, ps = next_ps()

                    def f_mm(e, tt=tt, ps=ps):
                        for kc in range(KC):
                            e.matmul(ps[:, 0:E], lhsT=h1T[:, kc, tt * 128:(tt + 1) * 128], rhs=wr[:, kc, :], start=(kc == 0), stop=False)
                        return e.matmul(ps[:, 0:E], lhsT=ones0[:, :], rhs=brrow[:, :], start=False, stop=True)
                    S.op("pe", f_mm, reads=[("h1T", tt), "wr", "brrow", "ones0"], writes=[pk])
                    S.op("dve", lambda e, ps=ps: e.tensor_copy(lg[:], ps[:, 0:E]), reads=[pk], writes=["lg"])
                    S.op("dve", lambda e: e.max(m8[:], lg[:]), reads=["lg"], writes=["m8"])
                    S.op("dve", lambda e, gt=gt: e.tensor_scalar(Mf[:, gt, :], lg[:], m8[:, 3:4], None, ALU.is_ge), reads=["lg", "m8"], writes=[("Mf", gt)])
                    S.op("dve", lambda e, gt=gt: e.tensor_copy(Mbf[:, gt, :], Mf[:, gt, :]), reads=[("Mf", gt)], writes=[("Mbf", gt)])
                    S.op("dve", lambda e: e.tensor_scalar(nmx[:], m8[:, 0:1], -1.0, None, ALU.mult), reads=["m8"], writes=["nmx"])
                    S.op("act", lambda e: e.activation(ex[:], lg[:], AF.Exp, bias=nmx[:, 0:1]), reads=["lg", "nmx"], writes=["ex"])
                    S.op("dve", lambda e, gt=gt: e.tensor_tensor(ex[:], ex[:], Mf[:, gt, :], ALU.mult), reads=["ex", ("Mf", gt)], writes=["ex"])
                    S.op("dve", lambda e: e.reduce_sum(ssum[:], ex[:], AX.X), reads=["ex"], writes=["ssum"])
                    S.op("dve", lambda e: e.reciprocal(ssum[:], ssum[:]), reads=["ssum"], writes=["ssum"])
                    S.op("dve", lambda e, gt=gt: e.tensor_scalar(Gt[:, gt, :], ex[:], ssum[:, 0:1], None, ALU.mult),
                         reads=["ex", "ssum"], writes=[("G", gt)])
                    pkr, psr = next_ps()

                    def f_rank(e, tt=tt, psr=psr):
                        ins = None
                        for t2 in range(tt + 1):
                            ins = e.matmul(psr[:, 0:E], lhsT=(allones[:, :] if t2 < tt else striu[:, :]), rhs=Mbf[:, hh * NTT + t2, :],
                                           start=(t2 == 0), stop=(t2 == tt))
                        return ins
                    S.op("pe", f_rank, reads=[("Mbf", hh * NTT + t2) for t2 in range(tt + 1)] + ["allones", "striu"], writes=[pkr])
                    S.op("dve", lambda e, gt=gt, psr=psr: e.tensor_copy(rank[:, gt, :], psr[:, 0:E]), reads=[pkr], writes=[("rank", gt)])
                    pk2, ps2 = next_ps()
                    S.op("pe", lambda e, gt=gt, ps2=ps2: e.transpose(ps2[0:E, 0:128], Gt[:, gt, :], ident[:, :]),
                         reads=[("G", gt), "ident"], writes=[pk2])
                    S.op("act", lambda e, tt=tt, ps2=ps2: e.activation(GT[0:E, tt * 128:(tt + 1) * 128], ps2[0:E, 0:128], AF.Identity),
                         reads=[pk2], writes=[("GT", tt)])
                    S.op("dve", lambda e, tt=tt: e.tensor_scalar(acc[:, tt, :], acc[:, tt, :], DN_ALPHA, None, ALU.mult),
                         reads=akeys(tt), writes=akeys(tt))
                    for dg in range(4):
                        pk3, ps3 = next_ps()
                        S.op("pe", lambda e, tt=tt, dg=dg, ps3=ps3: e.matmul(ps3[:, :], lhsT=GT[:, tt * 128:(tt + 1) * 128],
                                                                             rhs=b2bf[:, dg * 512:(dg + 1) * 512], start=True, stop=True),
                             reads=[("GT", tt), "GT", "b2bf"], writes=[pk3])
                        S.op("dve", lambda e, tt=tt, dg=dg, ps3=ps3: e.tensor_tensor(acc[:, tt, dg * 512:(dg + 1) * 512],
                                                                                    acc[:, tt, dg * 512:(dg + 1) * 512], ps3[:, :], ALU.add),
                             reads=[pk3, ("acc", tt, dg)], writes=[("acc", tt, dg)])

            def make_ple(hh, dg):
                def consume(wbt, wbk):
                    if dg == 0:
                        stageA_prologue(hh)
                    acc, h1T, pT, wple, bpgrow = (SA[k] for k in ("acc", "h1T", "pT", "wple", "bpgrow"))
                    tmps = SA["tmp"]

                    def ev(tt, ps, pk):
                        sg = tmps[tt % 2]
                        sk = ("ptmp", tt % 2)
                        S.op("act", lambda e: e.activation(sg[:], ps[:, :], AF.Sigmoid), reads=[pk], writes=[sk])
                        pk2, ps2 = next_ps()

                        def f_mm(e):
                            e.matmul(ps2[:, :], lhsT=pT[:, 0, tt * 128:(tt + 1) * 128], rhs=wple[:, 0, dg * 512:(dg + 1) * 512], start=True, stop=False)
                            return e.matmul(ps2[:, :], lhsT=pT[:, 1, tt * 128:(tt + 1) * 128], rhs=wple[:, 1, dg * 512:(dg + 1) * 512],
                                            start=False, stop=True)
                        S.op("pe", f_mm, reads=[("pT", tt), "wple"], writes=[pk2])
                        S.op("dve", lambda e: e.tensor_tensor(sg[:], sg[:], ps2[:, :], ALU.mult), reads=[sk, pk2], writes=[sk])
                        S.op("dve", lambda e: e.tensor_tensor(acc[:, tt, dg * 512:(dg + 1) * 512], acc[:, tt, dg * 512:(dg + 1) * 512], sg[:], ALU.add),
                             reads=[sk, ("acc", tt, dg)], writes=[("acc", tt, dg)])
                    tform(wbt, wbk, lambda kc, tt: h1T[:, kc, tt * 128:(tt + 1) * 128], lambda tt: [("h1T", tt)], NTT,
                          bpgrow[:, dg * 512:(dg + 1) * 512], "bpgrow", ev)
                    if dg == 3:
                        t0 = hh * HALF
                        for tt in range(NTT):
                            S.op("sp", lambda e, tt=tt: e.dma_start(out=accscr[t0 + tt * 128:t0 + (tt + 1) * 128, :], in_=acc[:, tt, :]),
                                 reads=akeys(tt), writes=[("accscr", hh * NTT + tt)], dma=("accst", tt % 2))
                        close_A()
                        if hh == NH - 1:
                            open_B()
                return consume

            def open_B():
                st = ExitStack()
                SB.clear()
                SB["stack"] = st
                h1tm = sbn("h1tm", [128, NTA, D], BF16, st)
                SB["h1tm"] = h1tm
                SB["xgT"] = sbn("xgT", [128, KC, CAPA], BF16, st)
                SB["actT"] = [sbn("actT", [128, KC, CAPA], BF16, st) for _ in range(2)]
                SB["Pe"] = [sbn("Pe", [128, NTT, CAPB], BF16, st) for _ in range(NH)]
                SB["tA"] = [sbn("tA", [128, CAPA], F32, st) for _ in range(2)]
                SB["tB"] = [sbn("tB", [128, CAPA], F32, st) for _ in range(2)]
                SB["rot"] = {"A": 0, "B": 0}
                for gt in range(NTA):
                    S.op("pool", lambda e, gt=gt: e.dma_start(out=h1tm[:, gt, :], in_=h1scr[gt * 128:(gt + 1) * 128, :]),
                         reads=[("h1scr", gt)], writes=[("h1tm", gt)], dma=("h1tm", gt % 2))

            def close_B():
                S.barrier()
                SB["stack"].close()

            def btmp(kind):
                i = SB["rot"][kind] % 2
                SB["rot"][kind] += 1
                return {"A": SB["tA"], "B": SB["tB"]}[kind][i], ("b" + kind, i)

            def expertB_prologue(ex_):
                xgT, h1tm = SB["xgT"], SB["h1tm"]
                npair = 512 // CAPB
                for hh in range(NH):
                    Pe = SB["Pe"][hh]
                    for tt in range(NTT):
                        gt = hh * NTT + tt
                        S.op("dve", lambda e, tt=tt, gt=gt, Pe=Pe: e.tensor_scalar(Pe[:, tt, :], iota[:, 0:CAPB], rank[:, gt, ex_:ex_ + 1], Mf[:, gt, ex_:ex_ + 1],
                                                                                 ALU.is_equal, ALU.mult),
                             reads=["iota", ("rank", gt), ("Mf", gt)], writes=[("Pe", hh, tt)])
                    for q in range(KC // npair):
                        pk, ps = next_ps()

                        def f_mm(e, q=q, ps=ps, hh=hh, Pe=Pe):
                            ins = None
                            for j in range(npair):
                                dc = q * npair + j
                                for tt in range(NTT):
                                    ins = e.matmul(ps[:, j * CAPB:(j + 1) * CAPB], lhsT=h1tm[:, hh * NTT + tt, dc * 128:(dc + 1) * 128], rhs=Pe[:, tt, :],
                                                   start=(tt == 0), stop=(tt == NTT - 1))
                            return ins
                        S.op("pe", f_mm, reads=[("Pe", hh, tt) for tt in range(NTT)] + [("h1tm", hh * NTT + tt) for tt in range(NTT)], writes=[pk])
                        S.op("act", lambda e, q=q, ps=ps, hh=hh: e.activation(xgT[:, q * npair:(q + 1) * npair, hh * CAPB:(hh + 1) * CAPB],
                                                                              ps[:, 0:npair * CAPB].rearrange("p (a b) -> p a b", a=npair), AF.Identity),
                             reads=[pk], writes=[("xgT", q, hh)])

            def make_w1(ex_, bi):
                def consume(wbt, wbk):
                    if bi == 0:
                        expertB_prologue(ex_)
                    xgT = SB["xgT"]
                    ab = ex_ % 2
                    actT = SB["actT"][ab]
                    for j in range(4):
                        fc = (bi % 4) * 4 + j
                        pk, ps = next_ps()

                        def f_mm(e, j=j, ps=ps):
                            ins = None
                            for kc in range(KC):
                                ins = e.matmul(ps[:, 0:CAPA], lhsT=wbt[:, kc, j * 128:(j + 1) * 128], rhs=xgT[:, kc, :],
                                               start=(kc == 0), stop=(kc == KC - 1))
                            return ins
                        S.op("pe", f_mm, reads=[wbk] + [("xgT", q, hh) for q in range(KC // (512 // CAPB)) for hh in range(NH)], writes=[pk])
                        dst = actT[:, fc, :]
                        ak = ("actT", ab, fc)
                        if bi < 4:
                            gc, gk = btmp("A")
                            sg, sk = btmp("B")
                            S.op("dve", lambda e, ps=ps, gc=gc, fc=fc: e.tensor_scalar(gc[:], ps[:, 0:CAPA], b1t[:, ex_, fc:fc + 1], SW_LIM,
                                                                                       ALU.add, ALU.min),
                                 reads=[pk, "b1t"], writes=[gk])
                            S.op("act", lambda e, gc=gc, sg=sg: e.activation(sg[:], gc[:], AF.Sigmoid, scale=SW_ALPHA),
                                 reads=[gk], writes=[sk])
                            S.op("dve", lambda e, gc=gc, sg=sg, dst=dst: e.tensor_tensor(dst, gc[:], sg[:], ALU.mult),
                                 reads=[gk, sk], writes=[ak])
                        else:
                            ub, uk = btmp("A")
                            S.op("act", lambda e, ps=ps, ub=ub, fc=fc: e.activation(ub[:], ps[:, 0:CAPA], AF.Identity,
                                                                                    bias=b1t[:, ex_, 16 + fc:17 + fc]),
                                 reads=[pk, "b1t"], writes=[uk])
                            S.op("dve", lambda e, ub=ub: e.tensor_scalar(ub[:], ub[:], SW_LIM, -SW_LIM, ALU.min, ALU.max),
                                 reads=[uk], writes=[uk])
                            S.op("dve", lambda e, ub=ub, dst=dst: e.scalar_tensor_tensor(dst, ub[:], 1.0, dst, ALU.add, ALU.mult),
                                 reads=[uk, ak], writes=[ak])
                    if bi == 7:
                        S.op("sp", lambda e: e.dma_start(out=actscr[ex_].rearrange("p (a b) -> p a b", a=KC), in_=actT[:]),
                             reads=[("actT", ab, fc) for fc in range(KC)], writes=[("actscr", ex_)], dma=("actst", ab))
                        if ex_ == E - 1:
                            close_B()
                            open_C(0)
                return consume

            def load_act(hh, ex_):
                actH = SC["actH"][ex_ % 2]
                S.op("sp", lambda e: e.dma_start(out=actH[:, :, 0:CAPB], in_=actscr[ex_].rearrange("p (a b) -> p a b", a=KC)[:, :, hh * CAPB:(hh + 1) * CAPB]),
                     reads=[("actscr", ex_)], writes=[("actH", ex_ % 2)], dma=("actld", ex_ % 2))

            def open_C(hh):
                st = ExitStack()
                SC.clear()
                SC["stack"] = st
                acc = sbn("accC", [128, NTT, D], F32, st)
                SC["acc"] = acc
                SC["actH"] = [sbn("actH", [128, KC, CAP], BF16, st) for _ in range(2)]
                for i_, a_h in enumerate(SC["actH"]):
                    S.op("dve", lambda e, a_h=a_h: e.memset(a_h[:], 0.0), writes=[("actH", i_)])
                SC["Pw"] = sbn("Pw", [128, NTT, CAP], BF16, st)
                SC["PTw"] = [sbn("PTw", [128, NST * NTT, 128], BF16, st) for _ in range(2)]
                SC["ysb"] = [sbn("ysb", [128, NST, 512], BF16, st) for _ in range(2)]
                SC["lng2"] = sbn("lng2", [128, 512], F32, st)
                SC["lnb2"] = sbn("lnb2", [128, 512], F32, st)
                SC["rot"] = {"Y": 0}
                t0 = hh * HALF
                for tt in range(NTT):
                    S.op("sp", lambda e, tt=tt: e.dma_start(out=acc[:, tt, :], in_=accscr[t0 + tt * 128:t0 + (tt + 1) * 128, :]),
                         reads=[("accscr", hh * NTT + tt)], writes=akeys(tt), dma=("accld", tt % 2))
                load_act(hh, 0)

            def close_C():
                S.barrier()
                SC["stack"].close()

            def expertC_prologue(hh, ex_):
                Pw = SC["Pw"]
                pb = ex_ % 2
                PTw = SC["PTw"][pb]
                if ex_ + 1 < E:
                    load_act(hh, ex_ + 1)
                for tt in range(NTT):
                    gt = hh * NTT + tt
                    S.op("dve", lambda e, tt=tt, gt=gt: e.tensor_scalar(Pw[:, tt, :], iota[:], rank[:, gt, ex_:ex_ + 1], Gt[:, gt, ex_:ex_ + 1],
                                                                       ALU.is_equal, ALU.mult),
                         reads=["iota", ("rank", gt), ("G", gt)], writes=[("Pw", tt)])
                nblk = NST * NTT
                for q in range((nblk + 3) // 4):
                    pk, ps = next_ps()
                    n = min(4, nblk - q * 4)

                    def f_mm(e, q=q, ps=ps, n=n):
                        ins = None
                        for j in range(n):
                            blk = q * 4 + j
                            st_, tt = blk // NTT, blk % NTT
                            ins = e.matmul(ps[:, j * 128:(j + 1) * 128], lhsT=Pw[:, tt, st_ * 128:(st_ + 1) * 128], rhs=identbf[:, :],
                                           start=True, stop=True)
                        return ins
                    S.op("pe", f_mm, reads=[("Pw", tt) for tt in range(NTT)] + ["identbf"], writes=[pk])
                    S.op("act", lambda e, q=q, ps=ps, n=n: e.activation(PTw[:, q * 4:q * 4 + n, :],
                                                                        ps[:, 0:n * 128].rearrange("p (a b) -> p a b", a=n), AF.Identity),
                         reads=[pk], writes=[("PTw", pb)])

            def make_w2(hh, ex_, dg):
                def consume(wbt, wbk):
                    if dg == 0:
                        expertC_prologue(hh, ex_)
                    acc = SC["acc"]
                    actT = SC["actH"][ex_ % 2]
                    pb = ex_ % 2
                    PTw = SC["PTw"][pb]
                    yi = SC["rot"]["Y"] % 2
                    SC["rot"]["Y"] += 1
                    ysb, yk = SC["ysb"][yi], ("ysb", yi)
                    for st_ in range(NST):
                        pk, ps = next_ps()

                        def f_mm(e, st_=st_, ps=ps):
                            ins = None
                            for fc in range(KC):
                                ins = e.matmul(ps[:, :], lhsT=actT[:, fc, st_ * 128:(st_ + 1) * 128], rhs=wbt[:, fc, :],
                                               start=(fc == 0), stop=(fc == KC - 1))
                            return ins
                        S.op("pe", f_mm, reads=[wbk, ("actH", ex_ % 2)], writes=[pk])
                        S.op("act", lambda e, st_=st_, ps=ps: e.activation(ysb[:, st_, :], ps[:, :], AF.Identity), reads=[pk], writes=[yk])
                    for tt in range(NTT):
                        pk, ps = next_ps()

                        def f_sc(e, tt=tt, ps=ps):
                            ins = None
                            for st_ in range(NST):
                                ins = e.matmul(ps[:, :], lhsT=PTw[:, st_ * NTT + tt, :], rhs=ysb[:, st_, :], start=(st_ == 0), stop=(st_ == NST - 1))
                            return ins
                        S.op("pe", f_sc, reads=[yk, ("PTw", pb)], writes=[pk])
                        S.op("dve", lambda e, tt=tt, ps=ps: e.tensor_tensor(acc[:, tt, dg * 512:(dg + 1) * 512], acc[:, tt, dg * 512:(dg + 1) * 512],
                                                                            ps[:, :], ALU.add),
                             reads=[pk, ("acc", tt, dg)], writes=[("acc", tt, dg)])
                    if ex_ == E - 1 and dg == 3:
                        stageC_epilogue(hh)
                return consume

            def stageC_epilogue(hh):
                t0 = hh * HALF
                acc, lng2, lnb2 = SC["acc"], SC["lng2"], SC["lnb2"]
                for tt in range(NTT):
                    ln_tm(acc[:, tt, :], akeys(tt), acc[:, tt, :], akeys(tt), affine=False)
                for dg in range(4):
                    S.op("sp", lambda e, dg=dg: e.dma_start(out=lng2[:], in_=lnv[6:7, dg * 512:(dg + 1) * 512].partition_broadcast(128)),
                         writes=["lng2"], dma="lng2")
                    S.op("sp", lambda e, dg=dg: e.dma_start(out=lnb2[:], in_=lnv[7:8, dg * 512:(dg + 1) * 512].partition_broadcast(128)),
                         writes=["lnb2"], dma="lnb2")
                    for tt in range(NTT):
                        a_ = acc[:, tt, dg * 512:(dg + 1) * 512]
                        S.op("dve", lambda e, a_=a_: e.tensor_tensor(a_, a_, lng2[:], ALU.mult), reads=[("acc", tt, dg), "lng2"], writes=[("acc", tt, dg)])
                        S.op("dve", lambda e, a_=a_: e.tensor_tensor(a_, a_, lnb2[:], ALU.add), reads=[("acc", tt, dg), "lnb2"], writes=[("acc", tt, dg)])
                for tt in range(NTT):
                    S.op("sp", lambda e, tt=tt: e.dma_start(out=out[t0 + tt * 128:t0 + (tt + 1) * 128, :], in_=acc[:, tt, :]),
                         reads=akeys(tt), writes=[("out", hh * NTT + tt)], dma=("ost", tt % 2))
                close_C()
                if hh + 1 < NH:
                    open_C(hh + 1)

            blocks2 = []
            for hh in range(NH):
                for dg in range(4):
                    blocks2.append((wview(w_pg, dg * 512), make_ple(hh, dg)))
            for ex_ in range(E):
                for bi in range(8):
                    blocks2.append((wview(w_e1[ex_], bi * 512), make_w1(ex_, bi)))
            for hh in range(NH):
                for ex_ in range(E):
                    for dg in range(4):
                        blocks2.append((wview(w_e2[ex_], dg * 512), make_w2(hh, ex_, dg)))
            run_stream(blocks2)
            S.op("sp", None, reads=[("out", i) for i in range(NT // 128)])

            S.finalize(top)
            with nc.Block() as block:
                S.emit(block)
    return nc


def _core_inputs(c, NT, E, inp, n_per_batch):
    b = c // n_per_batch
    s0 = (c % n_per_batch) * NT
    f = lambda a: np.ascontiguousarray(a, dtype=np.float32)
    x = inp["x"]
    m = {}
    m["x"] = f(x[b, s0:s0 + NT])
    if s0 == 0:
        m["xhalo"] = np.zeros((HALO, D), np.float32)
        m["hmask"] = np.zeros((128, 1), np.float32)
    else:
        m["xhalo"] = f(x[b, s0 - HALO:s0])
        m["hmask"] = np.ones((128, 1), np.float32)
    m["p"] = f(inp["p"][0, b, s0:s0 + NT])
    m["w_in"] = f(inp["w_in"][0])
    m["w_sgu_out"] = f(inp["w_sgu_out"][0])
    m["w_conv_out"] = f(inp["w_conv_out"][0])
    m["w_o"] = f(inp["w_o"][0])
    m["w_pg"] = f(inp["w_pg"][0])
    m["w_ple"] = f(inp["w_ple"][0])
    m["w_router"] = f(inp["w_router"][0])
    m["w_e1"] = f(inp["w_e1"][0])
    m["w_e2"] = f(inp["w_e2"][0])
    m["lnv"] = f(np.stack([inp["ln0_g"], inp["ln0_b"], inp["sgu_ln_g"][0], inp["sgu_ln_b"][0],
                           inp["ln1_g"][0], inp["ln1_b"][0], inp["ln2_g"][0], inp["ln2_b"][0]], 0))
    fm = lambda v: np.asarray(v).reshape(-1, 128).T
    m["bfm"] = f(np.concatenate([fm(inp["b_in"][0]), fm(inp["b_sgu_out"][0]), fm(inp["b_conv_out"][0]), fm(inp["conv_b"][0]),
                                 fm(inp["conv_ln_g"][0]), fm(inp["conv_ln_b"][0])], axis=1))
    m["convw"] = f(np.asarray(inp["conv_w"][0]).T.reshape(KC, 128, CW).transpose(1, 0, 2).reshape(128, KC * CW))
    m["bv"] = f(np.asarray(inp["b_in"][0])[None, D:2 * D])
    m["bo"] = f(np.asarray(inp["b_o"][0])[None])
    m["bpg"] = f(np.asarray(inp["b_pg"][0])[None])
    m["brt"] = f(np.asarray(inp["b_router"][0])[None])
    m["wsT"] = f(np.asarray(inp["w_s"][0]).transpose(2, 0, 1).reshape(128, 16 * 128))
    m["bs"] = f(np.asarray(inp["b_s"][0]).reshape(1, 16 * 128))
    m["b1"] = f(np.asarray(inp["b_e1"][0]).reshape(E, 32, 128).transpose(2, 0, 1).reshape(128, E * 32))
    m["b2"] = f(inp["b_e2"][0])
    m["ident"] = np.eye(128, dtype=np.float32)
    m["tri"] = np.triu(np.ones((128, 128), np.float32))
    m["iota"] = np.ascontiguousarray(np.broadcast_to(np.arange(512, dtype=np.float32), (128, 512)))
    return m


_NC_CACHE = {}


def kernel(**inputs):
    inp = {k: np.asarray(v) for k, v in inputs.items()}
    B, SEQ, _ = inp["x"].shape
    E = inp["w_router"].shape[-1]
    n_cores = 8
    NT = B * SEQ // n_cores
    n_per_batch = SEQ // NT
    key = (NT, E)
    if key not in _NC_CACHE:
        _NC_CACHE[key] = build_program(NT=NT, E=E, HALF=min(1024, NT))
    nc = _NC_CACHE[key]
    in_maps = [_core_inputs(c, NT, E, inp, n_per_batch) for c in range(n_cores)]
    res = run_bass_kernel_spmd(nc, in_maps, core_ids=list(range(n_cores)))
    outs = [np.asarray(r["out"]) for r in res.results]
    return np.stack(outs, 0).reshape(B, SEQ, D).astype(np.float32)
```
